# Optimizing a Trainium2 kernel written in Bass

```python
import jax, jax.numpy as jnp
from jax import lax
import numpy as np

D_MODEL = 1024
BATCH = 8
SEQ = 4096
DEPTH = 2

GRID_W = 64
CTX_LEN = 256
HEAD_DIM = 64
D_MIX = D_MODEL
D_RWKV = D_MIX // 2
D_CONV = D_MIX // 4
D_FOURIER = D_MIX - D_RWKV - D_CONV
N_RWKV_HEADS = D_RWKV // HEAD_DIM
N_CONV_GROUPS = D_CONV // HEAD_DIM
N_FOURIER_GROUPS = D_FOURIER // HEAD_DIM
D_DECAY_LORA = 64
D_AAA_LORA = 64
D_GATE_LORA = 128
D_FF = 2816
N_EXPERTS = 8
TOP_K = 2
D_FF_EXPERT = 3584
N_DENSE = (DEPTH + 1) // 2
N_MOE = DEPTH // 2
NORM_EPS = 1e-6
GN_EPS = 64e-5
POS_BASE = 10000.0
COL_SIZES = (D_RWKV, D_RWKV, D_DECAY_LORA, D_DECAY_LORA, D_AAA_LORA, D_AAA_LORA, D_RWKV, D_GATE_LORA,
             D_CONV, D_CONV, D_CONV, D_FOURIER)
D_STATE_COLS = 2 * D_RWKV + 2 * D_DECAY_LORA + 2 * D_AAA_LORA
D_IN = 4 * D_RWKV // 2 * 1 + D_RWKV + 2 * D_DECAY_LORA + 2 * D_AAA_LORA + D_GATE_LORA + 3 * D_CONV + D_FOURIER

kernel_name = "hybrid_rwkv7_shortconv_fourier_moe_dit"


def split_cols(z, sizes):
    out, o = [], 0
    for s in sizes:
        out.append(z[..., o:o + s])
        o += s
    return out


def rms_norm(x, g):
    xf = x.astype(jnp.float32)
    y = xf * lax.rsqrt(jnp.mean(xf * xf, axis=-1, keepdims=True) + NORM_EPS)
    return (y * g.astype(jnp.float32)).astype(x.dtype)


def adaln(silu_c, w, b):
    return jnp.split(silu_c @ w + b, 6, axis=-1)


def sincos_2d(rows, cols, dim):
    quarter = dim // 4
    omega = 1.0 / (POS_BASE ** (jnp.arange(quarter, dtype=jnp.float32) / quarter))

    def axis_emb(n):
        ang = jnp.arange(n, dtype=jnp.float32)[:, None] * omega[None, :]
        return jnp.concatenate([jnp.sin(ang), jnp.cos(ang)], axis=-1)

    er, ec = axis_emb(rows), axis_emb(cols)
    emb = jnp.concatenate([jnp.broadcast_to(er[:, None, :], (rows, cols, dim // 2)),
                           jnp.broadcast_to(ec[None, :, :], (rows, cols, dim // 2))], axis=-1)
    return emb.reshape(rows * cols, dim)


def token_shift(z, reverse):
    if reverse:
        return jnp.concatenate([z[:, 1:], jnp.zeros_like(z[:, :1])], axis=1)
    return jnp.concatenate([jnp.zeros_like(z[:, :1]), z[:, :-1]], axis=1)


def to_heads(z):
    return z.reshape(z.shape[0], z.shape[1], -1, HEAD_DIM)


def rwkv_direction(k, v, wlo, alo, r, mu, mu_w, mu_a, w0, w2, a0, a2, k_k, k_a, reverse):
    f32 = jnp.float32
    lerp = lambda z, m: z + (token_shift(z, reverse) - z) * m.astype(f32)
    k = lerp(k.astype(f32), mu[0])
    v = lerp(v.astype(f32), mu[1])
    wlo = lerp(wlo.astype(f32), mu_w)
    alo = lerp(alo.astype(f32), mu_a)
    w_log = -jax.nn.softplus(-(w0.astype(f32) + jnp.tanh(wlo) @ w2.astype(f32))) - 0.5
    decay = jnp.exp(-jnp.exp(w_log))
    a = jax.nn.sigmoid(a0.astype(f32) + alo @ a2.astype(f32))
    kk = to_heads(k * k_k.astype(f32))
    kk = kk / jnp.maximum(jnp.linalg.norm(kk, axis=-1, keepdims=True), 1e-12)
    k = k * (1.0 + (a - 1.0) * k_a.astype(f32))
    rh = None if r is None else to_heads(lerp(r.astype(f32), mu[2]))
    return rh, to_heads(decay), to_heads(k), to_heads(v), -kk, kk * to_heads(a)


def wkv_scan(r, w, k, v, a, b, state0, reverse):
    def update(S, w_t, k_t, v_t, a_t, b_t):
        sa = jnp.einsum("bhvk,bhk->bhv", S, a_t)
        return S * w_t[:, :, None, :] + sa[..., None] * b_t[:, :, None, :] + v_t[..., None] * k_t[:, :, None, :]

    tm = lambda t: jnp.moveaxis(t, 1, 0)
    if r is None:
        def step_state(S, inp):
            return update(S, *inp), None
        S, _ = lax.scan(step_state, state0, tuple(map(tm, (w, k, v, a, b))), reverse=reverse)
        return None, S

    def step(S, inp):
        S = update(S, *inp[1:])
        return S, jnp.einsum("bhvk,bhk->bhv", S, inp[0])

    S, y = lax.scan(step, state0, tuple(map(tm, (r, w, k, v, a, b))), reverse=reverse)
    return jnp.moveaxis(y, 0, 1), S


def rwkv_readout(y, r, k, v, r_k, gn_w, gn_b):
    mean = jnp.mean(y, axis=-1, keepdims=True)
    var = jnp.mean(jnp.square(y - mean), axis=-1, keepdims=True)
    yn = ((y - mean) * lax.rsqrt(var + GN_EPS)).reshape(y.shape[0], y.shape[1], D_RWKV)
    yn = yn * gn_w.astype(jnp.float32) + gn_b.astype(jnp.float32)
    bonus = jnp.sum(r * k * r_k.astype(jnp.float32), axis=-1, keepdims=True) * v
    return yn + bonus.reshape(yn.shape)


def rwkv_mixer(k, v, wlos, alos, r, glo, rp, states0):
    ys, states = [], []
    for d, reverse in enumerate((False, True)):
        rh, wh, kh, vh, ah, bh = rwkv_direction(k, v, wlos[d], alos[d], r, rp["mu"][d], rp["mu_w"][d],
                                                rp["mu_a"][d], rp["w0"][d], rp["w2"][d], rp["a0"][d],
                                                rp["a2"][d], rp["k_k"], rp["k_a"], reverse)
        y, S = wkv_scan(rh, wh, kh, vh, ah, bh, states0[d], reverse)
        states.append(S)
        if r is not None:
            ys.append(rwkv_readout(y, rh, kh, vh, rp["r_k"], rp["gn_w"], rp["gn_b"]))
    if r is None:
        return None, states
    gate = jax.nn.sigmoid(glo.astype(jnp.float32)) @ rp["g2"].astype(jnp.float32)
    return ((ys[0] + ys[1]) * gate).astype(k.dtype), states


def conv3_centred(h, w):
    hp = jnp.pad(h, ((0, 0), (1, 1), (0, 0)))
    return hp[:, :-2] * w[0] + hp[:, 1:-1] * w[1] + hp[:, 2:] * w[2]


def fourier_mix(f):
    bsz, length, _ = f.shape
    fg = f.astype(jnp.float32).reshape(bsz, length, N_FOURIER_GROUPS, HEAD_DIM)
    out = jnp.fft.fft2(fg, axes=(1, 3), norm="ortho").real
    return out.reshape(bsz, length, D_FOURIER).astype(f.dtype)


def token_mixers(z, rp, conv_w, states0, on_grid):
    k, v, wlo_f, wlo_b, alo_f, alo_b, r, glo, u, gate_b, gate_c, f = split_cols(z, COL_SIZES)
    y_rwkv, states = rwkv_mixer(k, v, (wlo_f, wlo_b), (alo_f, alo_b), r, glo, rp, states0)
    hc = gate_c * u
    if on_grid:
        bsz, length, ch = hc.shape
        rows = length // GRID_W
        conv = conv3_centred(hc.reshape(bsz * rows, GRID_W, ch), conv_w).reshape(bsz, length, ch)
    else:
        conv = conv3_centred(hc, conv_w)
    y_conv = gate_b * conv
    y_four = fourier_mix(f)
    return jnp.concatenate([y_rwkv, y_conv, y_four], axis=-1), states


def swiglu(h, wg, wu, wd):
    return (jax.nn.silu(h @ wg) * (h @ wu)) @ wd


def moe_swiglu(h, router_w, router_b, wg, wu, wd):
    logits = h.astype(jnp.float32) @ router_w.astype(jnp.float32) + router_b.astype(jnp.float32)
    top_val, top_idx = lax.top_k(logits, TOP_K)
    top_p = jax.nn.softmax(top_val, axis=-1)
    gates = jnp.sum(jax.nn.one_hot(top_idx, N_EXPERTS, dtype=jnp.float32) * top_p[..., None], axis=-2)
    gates = gates.astype(h.dtype)
    out = jnp.zeros_like(h)
    for e in range(N_EXPERTS):
        out = out + gates[..., e:e + 1] * swiglu(h, wg[e], wu[e], wd[e])
    return out


def setup_inputs(seed: int = 0) -> dict:
    key = jax.random.key(seed)
    ks = iter(jax.random.split(key, 40))
    nrm = lambda shape, s: jax.random.normal(next(ks), shape, jnp.float32) * s
    uni = lambda shape, lo, hi: jax.random.uniform(next(ks), shape, jnp.float32, lo, hi)
    L = DEPTH
    return {
        "x": nrm((BATCH, SEQ, D_MODEL), 1.0),
        "c": nrm((BATCH, D_MODEL), 1.0),
        "ctx": nrm((BATCH, CTX_LEN, D_MODEL), 1.0),
        "c_ctx": nrm((D_MODEL,), 1.0),
        "ada_w": nrm((L, D_MODEL, 6 * D_MODEL), 0.5 * D_MODEL ** -0.5),
        "ada_b": nrm((L, 6 * D_MODEL), 0.02),
        "norm_g": 1.0 + nrm((L, 4, D_MODEL), 0.02),
        "w_in": nrm((L, D_MODEL, D_IN), D_MODEL ** -0.5),
        "w_out": nrm((L, D_MIX, D_MODEL), D_MIX ** -0.5),
        "rwkv_mu": uni((L, 2, 3, D_RWKV), 0.0, 1.0),
        "rwkv_mu_w": uni((L, 2, D_DECAY_LORA), 0.0, 1.0),
        "rwkv_mu_a": uni((L, 2, D_AAA_LORA), 0.0, 1.0),
        "rwkv_w0": uni((L, 2, D_RWKV), -6.0, 1.0),
        "rwkv_w2": nrm((L, 2, D_DECAY_LORA, D_RWKV), 0.1),
        "rwkv_a0": nrm((L, 2, D_RWKV), 0.5),
        "rwkv_a2": nrm((L, 2, D_AAA_LORA, D_RWKV), 0.1),
        "rwkv_g2": nrm((L, D_GATE_LORA, D_RWKV), D_GATE_LORA ** -0.5),
        "rwkv_k_k": 0.85 + nrm((L, D_RWKV), 0.05),
        "rwkv_k_a": 1.0 + nrm((L, D_RWKV), 0.05),
        "rwkv_r_k": nrm((L, N_RWKV_HEADS, HEAD_DIM), 0.1),
        "rwkv_gn_w": 1.0 + nrm((L, D_RWKV), 0.02),
        "rwkv_gn_b": nrm((L, D_RWKV), 0.02),
        "conv_w": nrm((L, 3, D_CONV), 3.0 ** -0.5),
        "ffn_w_gate": nrm((N_DENSE, D_MODEL, D_FF), D_MODEL ** -0.5),
        "ffn_w_up": nrm((N_DENSE, D_MODEL, D_FF), D_MODEL ** -0.5),
        "ffn_w_down": nrm((N_DENSE, D_FF, D_MODEL), D_FF ** -0.5),
        "router_w": nrm((N_MOE, D_MODEL, N_EXPERTS), D_MODEL ** -0.5),
        "router_b": nrm((N_MOE, N_EXPERTS), 0.01),
        "moe_w_gate": nrm((N_MOE, N_EXPERTS, D_MODEL, D_FF_EXPERT), D_MODEL ** -0.5),
        "moe_w_up": nrm((N_MOE, N_EXPERTS, D_MODEL, D_FF_EXPERT), D_MODEL ** -0.5),
        "moe_w_down": nrm((N_MOE, N_EXPERTS, D_FF_EXPERT, D_MODEL), D_FF_EXPERT ** -0.5),
    }


def reference(x, c, ctx, c_ctx, ada_w, ada_b, norm_g, w_in, w_out, rwkv_mu, rwkv_mu_w, rwkv_mu_a,
              rwkv_w0, rwkv_w2, rwkv_a0, rwkv_a2, rwkv_g2, rwkv_k_k, rwkv_k_a, rwkv_r_k, rwkv_gn_w,
              rwkv_gn_b, conv_w, ffn_w_gate, ffn_w_up, ffn_w_down, router_w, router_b, moe_w_gate,
              moe_w_up, moe_w_down):
    bsz, length, dim = x.shape
    rows = length // GRID_W
    x = x + sincos_2d(rows, GRID_W, dim).astype(x.dtype)[None]
    xc = ctx
    silu_c, silu_cc = jax.nn.silu(c), jax.nn.silu(c_ctx)
    zero_state = jnp.zeros((bsz, N_RWKV_HEADS, HEAD_DIM, HEAD_DIM), jnp.float32)

    def channel_mixer(l, h):
        if l % 2 == 0:
            i = l // 2
            return swiglu(h, ffn_w_gate[i], ffn_w_up[i], ffn_w_down[i])
        i = l // 2
        return moe_swiglu(h, router_w[i], router_b[i], moe_w_gate[i], moe_w_up[i], moe_w_down[i])

    for l in range(DEPTH):
        last = l == DEPTH - 1
        rp = {"mu": rwkv_mu[l], "mu_w": rwkv_mu_w[l], "mu_a": rwkv_mu_a[l], "w0": rwkv_w0[l],
              "w2": rwkv_w2[l], "a0": rwkv_a0[l], "a2": rwkv_a2[l], "g2": rwkv_g2[l],
              "k_k": rwkv_k_k[l], "k_a": rwkv_k_a[l], "r_k": rwkv_r_k[l], "gn_w": rwkv_gn_w[l],
              "gn_b": rwkv_gn_b[l]}
        sh1, sc1, g1, sh2, sc2, g2 = adaln(silu_c[:, None, :], ada_w[l], ada_b[l])
        csh1, csc1, cg1, csh2, csc2, cg2 = adaln(silu_cc, ada_w[l], ada_b[l])

        hc = rms_norm(xc, norm_g[l, 0]) * (1.0 + csc1) + csh1
        if last:
            kc, vc, wfc, wbc, afc, abc = split_cols(hc @ w_in[l][:, :D_STATE_COLS], COL_SIZES[:6])
            _, ctx_states = rwkv_mixer(kc, vc, (wfc, wbc), (afc, abc), None, None, rp,
                                       (zero_state, zero_state))
        else:
            yc, ctx_states = token_mixers(hc @ w_in[l], rp, conv_w[l], (zero_state, zero_state), False)
            xc = xc + cg1 * rms_norm(yc @ w_out[l], norm_g[l, 1])
            hc2 = rms_norm(xc, norm_g[l, 2]) * (1.0 + csc2) + csh2
            xc = xc + cg2 * rms_norm(channel_mixer(l, hc2), norm_g[l, 3])

        h = rms_norm(x, norm_g[l, 0]) * (1.0 + sc1) + sh1
        y, _ = token_mixers(h @ w_in[l], rp, conv_w[l], ctx_states, True)
        x = x + g1 * rms_norm(y @ w_out[l], norm_g[l, 1])
        h2 = rms_norm(x, norm_g[l, 2]) * (1.0 + sc2) + sh2
        x = x + g2 * rms_norm(channel_mixer(l, h2), norm_g[l, 3])
    return x
```

```python
import numpy as np
import concourse.bass as bass
import concourse.mybir as mybir
from concourse.bass_utils import run_bass_kernel_spmd
from contextlib import ExitStack

F32 = mybir.dt.float32
BF16 = mybir.dt.bfloat16
AF = mybir.ActivationFunctionType
ALU = mybir.AluOpType
AX = mybir.AxisListType
NDS = 40
NPL = 8

D = 1024
SEQ = 4096
CTX = 256
TT = CTX + SEQ
DIN = 2944
DFF = 2816
DFE = 3584
NE = 8
EPS = 1e-6
GN_EPS = 64e-5
O_K, O_V, O_WF, O_WB, O_AF, O_AB, O_R, O_G, O_U, O_B, O_C, O_F = (
    0, 512, 1024, 1088, 1152, 1216, 1280, 1792, 1920, 2176, 2432, 2688)


class Res:
    __slots__ = ("w", "r")

    def __init__(self):
        self.w = None
        self.r = []


class T:
    def __init__(self, h, res=None):
        self.h = h
        self.res = res if res is not None else Res()

    def __getitem__(self, idx):
        return self.h[idx]


def _res(x):
    return x.res if isinstance(x, T) else x


class KB:
    ENG = ("pe", "act", "dve", "pool", "sp")

    def __init__(self, nc, es):
        self.nc = nc
        self.eng = dict(pe=nc.tensor, act=nc.scalar, dve=nc.vector, pool=nc.gpsimd, sp=nc.sync)
        self.sems = [{e: es.enter_context(nc.semaphore("s%d_%s" % (i, e))) for e in self.ENG} for i in range(2)]
        self.dsemss = [[es.enter_context(nc.semaphore("d%d_%d" % (j, i))) for i in range(NDS - NPL)] for j in range(2)]
        self.psems = [es.enter_context(nc.semaphore("p%d" % i)) for i in range(NPL)]
        self.pcnt = [0] * NPL
        self.pnext = 0
        self.cur = 0
        self.ep = 0
        self._reset()
        self.nwait = 0
        self.nins = 0
        self.hook = None

    def _reset(self):
        self.sem = self.sems[self.cur]
        self.dsems = self.dsemss[self.cur]
        self.cnt = {e: 0 for e in self.ENG}
        self.seen = {e: {} for e in self.ENG}
        self.dcnt = [0] * (NDS - NPL)
        self.dnext = 0

    def _wait(self, eng, tok):
        if tok is None or tok[3] != self.ep:
            return
        kind, a, n, _ = tok
        if kind == "c":
            if a == "pe" and eng == "pe":
                return
            sem, val, key = self.sem[a], n, a
        elif kind == "p":
            sem, val, key = self.psems[a], 16 * n, "p%d" % a
        else:
            sem, val, key = self.dsems[a], 16 * n, "d%d" % a
        if self.seen[eng].get(key, 0) >= val:
            return
        self.eng[eng].wait_ge(sem, val)
        self.seen[eng][key] = val
        self.nwait += 1

    def _deps(self, eng, r, w):
        toks = []
        for x in r:
            toks.append(_res(x).w)
        for x in w:
            rs = _res(x)
            toks.append(rs.w)
            toks.extend(rs.r)
        for t in toks:
            self._wait(eng, t)

    def _mark(self, tok, r, w):
        for x in r:
            rs = _res(x)
            rs.r = [t for t in rs.r if t[3] == self.ep and not (t[0] == tok[0] and t[1] == tok[1])]
            rs.r.append(tok)
        for x in w:
            rs = _res(x)
            rs.w = tok
            rs.r = []

    def op(self, eng, fn, r=(), w=()):
        self._deps(eng, r, w)
        ins = fn(self.eng[eng])
        self.cnt[eng] += 1
        ins.then_inc(self.sem[eng], 1)
        self.nins += 1
        tok = ("c", eng, self.cnt[eng], self.ep)
        self._mark(tok, r, w)
        if self.hook:
            self.hook()
        return tok

    def dma(self, q, out, in_, r=(), w=(), **kw):
        if q == "pool":
            i = self.pnext
            self.pnext = (i + 1) % NPL
            if self.pcnt[i] > 0:
                self.seen[q].pop("p%d" % i, None)
                self.eng[q].wait_ge(self.psems[i], 16 * self.pcnt[i])
            self._deps(q, r, w)
            ins = self.eng[q].dma_start(out=out, in_=in_, **kw)
            self.pcnt[i] += 1
            ins.then_inc(self.psems[i], 16)
            self.nins += 1
            tok = ("p", i, self.pcnt[i], self.ep)
            self._mark(tok, r, w)
            return tok
        i = self.dnext
        self.dnext = (i + 1) % (NDS - NPL)
        if self.dcnt[i] > 0:
            self._wait(q, ("d", i, self.dcnt[i], self.ep))
        self._deps(q, r, w)
        ins = self.eng[q].dma_start(out=out, in_=in_, **kw)
        self.dcnt[i] += 1
        ins.then_inc(self.dsems[i], 16)
        self.nins += 1
        tok = ("d", i, self.dcnt[i], self.ep)
        self._mark(tok, r, w)
        return tok

    def barrier(self, engines=None):
        engines = engines or self.ENG
        for e in engines:
            for f in self.ENG:
                if self.cnt[f] > 0 and f != e:
                    self._wait(e, ("c", f, self.cnt[f], self.ep))
            if e != "pe" and self.cnt[e] > 0:
                self._wait(e, ("c", e, self.cnt[e], self.ep))
            for i in range(NDS - NPL):
                if self.dcnt[i] > 0:
                    self._wait(e, ("d", i, self.dcnt[i], self.ep))
            for i in range(NPL):
                if self.pcnt[i] > 0:
                    self.seen[e].pop("p%d" % i, None)
                    self._wait(e, ("p", i, self.pcnt[i], self.ep))

    def epoch(self):
        self.barrier()
        old_sem, old_d = self.sem, self.dsems
        self.cur = 1 - self.cur
        self.ep += 1
        self._reset()
        for e in self.ENG:
            ins = self.eng[e].nop()
            self.cnt[e] = 1
            ins.then_inc(self.sem[e], 1)
        for e in self.ENG:
            if e != "sp":
                self._wait("sp", ("c", e, 1, self.ep))
        for s_ in list(old_sem.values()) + list(old_d):
            self.eng["sp"].sem_clear(s_)
        ins = self.eng["sp"].nop()
        self.cnt["sp"] += 1
        ins.then_inc(self.sem["sp"], 1)


class Ctx:
    pass


def weave(k, fns, quota):
    import threading
    n = len(fns)
    if n == 1:
        fns[0]()
        return
    sems = [threading.Semaphore(0) for _ in fns]
    main = threading.Semaphore(0)
    done = [False] * n
    state = {"cur": 0, "left": quota[0], "err": None}

    def nxt(i):
        for j in range(1, n + 1):
            t = (i + j) % n
            if not done[t]:
                return t
        return None

    def hook():
        i = state["cur"]
        state["left"] -= 1
        if state["left"] <= 0:
            t = nxt(i)
            if t is not None and t != i:
                state["cur"] = t
                state["left"] = quota[t]
                sems[t].release()
                sems[i].acquire()
            else:
                state["left"] = quota[i]

    def runner(i):
        sems[i].acquire()
        try:
            fns[i]()
        except BaseException as ex:
            state["err"] = ex
        finally:
            done[i] = True
            t = nxt(i)
            if t is not None:
                state["cur"] = t
                state["left"] = quota[t]
                sems[t].release()
            else:
                main.release()

    k.hook = hook
    threads = [threading.Thread(target=runner, args=(i,)) for i in range(n)]
    for t in threads:
        t.start()
    sems[0].release()
    main.acquire()
    for t in threads:
        t.join()
    k.hook = None
    if state["err"] is not None:
        raise state["err"]


_UID = [0]


def uname(n):
    _UID[0] += 1
    return "%s_%d" % (n, _UID[0])


def token_tiles():
    tiles = [(0, CTX, 1)]
    for i in range(SEQ // 512):
        tiles.append((CTX + i * 512, 512, 0))
    return tiles


def phase_mod(g, l):
    nc, k = g.nc, g.k
    with ExitStack() as ph:
        sb = lambda n, s, d: T(ph.enter_context(nc.sbuf_tensor(uname(n), s, d)))
        cv = sb("cv", [128, 8, 2], F32)
        scv = sb("scv", [128, 8, 2], F32)
        adab = sb("adab", [128, 48], F32)
        ng = sb("ng", [128, 4, 8], F32)
        raw = sb("raw", [128, 48, 2], F32)
        wbuf = [sb("adaw%d" % i, [128, 8, 512], F32) for i in range(2)]
        ps = g.ps[0]
        k.dma("sp", cv[:], g.i["cvec"], w=[cv])
        k.dma("sp", adab[:], g.i["ada_b"][l], w=[adab])
        k.dma("sp", ng[:], g.i["norm_g"][l], w=[ng])
        k.op("act", lambda e: e.activation(out=scv[:], in_=cv[:], func=AF.Silu), r=[cv], w=[scv])
        wsrc = g.i["ada_w"][l].rearrange("(kc p) n -> p kc n", p=128)
        psv = ps[:, 0:96].rearrange("p (n w) -> p n w", w=2)
        for nb in range(12):
            wb = wbuf[nb % 2]
            k.dma("sp", wb[:], wsrc[:, :, nb * 512:(nb + 1) * 512], w=[wb])
            for j in range(4):
                n = nb * 4 + j
                for kc in range(8):
                    k.op("pe", lambda e, n=n, j=j, kc=kc, wb=wb: e.matmul(
                        psv[:, n, :], lhsT=wb[:, kc, j * 128:(j + 1) * 128], rhs=scv[:, kc, :],
                        start=(kc == 0), stop=(kc == 7)), r=[wb, scv], w=[ps])
        for w_ in range(2):
            k.op("dve", lambda e, w_=w_: e.tensor_tensor(out=raw[:, :, w_], in0=psv[:, :, w_], in1=adab[:],
                                                  op=ALU.add), r=[ps, adab], w=[raw])
        mv = g.modv
        for w_ in range(2):
            for (slot, jsc, jsh, jg, n0, n1) in ((0, 1, 0, 2, 0, 1), (3, 4, 3, 5, 2, 3)):
                k.op("dve", lambda e, w_=w_, slot=slot, jsc=jsc, n0=n0: e.scalar_tensor_tensor(
                    out=mv[:, slot, :, w_], in0=raw[:, jsc * 8:(jsc + 1) * 8, w_], scalar=1.0, in1=ng[:, n0, :],
                    op0=ALU.add, op1=ALU.mult), r=[raw, ng], w=[mv])
                k.op("dve", lambda e, w_=w_, slot=slot, jsh=jsh: e.tensor_copy(
                    out=mv[:, slot + 1, :, w_], in_=raw[:, jsh * 8:(jsh + 1) * 8, w_]), r=[raw], w=[mv])
                k.op("dve", lambda e, w_=w_, slot=slot, jg=jg, n1=n1: e.tensor_tensor(
                    out=mv[:, slot + 2, :, w_], in0=raw[:, jg * 8:(jg + 1) * 8, w_], in1=ng[:, n1, :],
                    op=ALU.mult), r=[raw, ng], w=[mv])
        k.epoch()


def rms_rstd(g, xt, tn, sq, ps, rstd, tmp):
    k = g.k
    k.op("act", lambda e: e.activation(out=sq[:, :, :tn], in_=xt[:, :, :tn], func=AF.Square), r=[xt], w=[sq])
    for dc in range(8):
        k.op("pe", lambda e, dc=dc: e.matmul(ps[:, :tn], lhsT=g.ones_bf[:], rhs=sq[:, dc, :tn],
                                             start=(dc == 0), stop=(dc == 7)), r=[sq, g.ones_bf], w=[ps])
    k.op("act", lambda e: e.activation(out=tmp[:, :tn], in_=ps[:, :tn], func=AF.Sqrt, scale=1.0 / D, bias=g.eps_t[:]),
         r=[ps, g.eps_t], w=[tmp])
    k.op("dve", lambda e: e.reciprocal(out=rstd[:, :tn], in_=tmp[:, :tn]), r=[tmp], w=[rstd])


def phase_win(g, l):
    nc, k = g.nc, g.k
    with ExitStack() as ph:
        sb = lambda n, s, d: T(ph.enter_context(nc.sbuf_tensor(uname(n), s, d)))
        win = sb("win", [128, 8, DIN], BF16)
        wsrc = g.i["w_in"][l].rearrange("(kc p) n -> p kc n", p=128)
        for kc in range(8):
            k.dma("pool", win[:, kc, :], wsrc[:, kc, :], w=[win])
        xts = [sb("xt%d" % i, [128, 8, 512], F32) for i in range(2)]
        pts = [sb("pt%d" % i, [128, 8, 512], F32) for i in range(2)]
        sq = sb("sq", [128, 8, 512], BF16)
        hT = [sb("hT%d" % i, [128, 8, 512], BF16) for i in range(2)]
        rstd = sb("rstd", [128, 512], F32)
        tmp = sb("tmp", [128, 512], F32)
        tmp2 = [sb("tmp2_%d" % i, [128, 512], F32) for i in range(2)]
        zsb = [sb("zsb%d" % i, [128, 512], F32) for i in range(4)]
        xsrc = (g.i["xT"] if l == 0 else g.xres).rearrange("(dc p) t -> p dc t", p=128)
        xdst = g.xres.rearrange("(dc p) t -> p dc t", p=128)
        psrc = g.i["posT"].rearrange("(dc p) t -> p dc t", p=128)
        zi = 0
        for ti, (c0, tn, wh) in enumerate(token_tiles()):
            xt = xts[ti % 2]
            h = hT[ti % 2]
            k.dma("sp", xt[:, :, :tn], xsrc[:, :, c0:c0 + tn], r=[g.xres_r] if l else [], w=[xt])
            if l == 0:
                if wh == 0:
                    pt = pts[ti % 2]
                    k.dma("sp", pt[:, :, :tn], psrc[:, :, c0 - CTX:c0 - CTX + tn], w=[pt])
                    k.op("pool", lambda e, xt=xt, pt=pt: e.tensor_tensor(out=xt[:], in0=xt[:], in1=pt[:], op=ALU.add),
                         r=[pt, xt], w=[xt])
                k.dma("sp", xdst[:, :, c0:c0 + tn], xt[:, :, :tn], r=[xt], w=[g.xres_r])
            rms_rstd(g, xt, tn, sq, g.ps[1], rstd, tmp)
            for dc in range(8):
                t2 = tmp2[dc % 2]
                k.op("dve", lambda e, dc=dc, t2=t2, xt=xt: e.scalar_tensor_tensor(
                    out=t2[:, :tn], in0=xt[:, dc, :tn], scalar=g.modv[:, 0, dc, wh:wh + 1], in1=rstd[:, :tn],
                    op0=ALU.mult, op1=ALU.mult), r=[xt, rstd, g.modv], w=[t2])
                k.op("act", lambda e, dc=dc, t2=t2, h=h: e.activation(
                    out=h[:, dc, :tn], in_=t2[:, :tn], func=AF.Identity, bias=g.modv[:, 1, dc, wh:wh + 1]),
                    r=[t2, g.modv], w=[h])
            for n in range(DIN // 128):
                ps = g.ps[2 + (zi % 4)]
                zs = zsb[zi % 4]
                for dc in range(8):
                    k.op("pe", lambda e, n=n, dc=dc, ps=ps, h=h: e.matmul(
                        ps[:, :tn], lhsT=win[:, dc, n * 128:(n + 1) * 128], rhs=h[:, dc, :tn],
                        start=(dc == 0), stop=(dc == 7)), r=[win, h], w=[ps])
                if zi % 2 == 0:
                    k.op("act", lambda e, ps=ps, zs=zs: e.activation(out=zs[:, :tn], in_=ps[:, :tn], func=AF.Copy),
                         r=[ps], w=[zs])
                else:
                    k.op("dve", lambda e, ps=ps, zs=zs: e.tensor_copy(out=zs[:, :tn], in_=ps[:, :tn]), r=[ps], w=[zs])
                k.dma("sp", g.zT[n * 128:(n + 1) * 128, c0:c0 + tn], zs[:, :tn], r=[zs], w=[g.zT_r])
                zi += 1
        k.epoch()


def phase_conv(g, l):
    nc, k = g.nc, g.k
    with ExitStack() as ph:
        sb = lambda n, s, d: T(ph.enter_context(nc.sbuf_tensor(uname(n), s, d)))
        for cc in range(2):
            u = sb("cu%d" % cc, [128, TT], F32)
            bg = sb("cb%d" % cc, [128, TT], F32)
            cg = sb("cc%d" % cc, [128, TT], F32)
            y = sb("cy%d" % cc, [128, TT], F32)
            yo = sb("cyo%d" % cc, [128, TT], BF16)
            cw = sb("cw%d" % cc, [128, 3], F32)
            k.dma("sp", cw[:], g.i["conv_w"][l, cc], w=[cw])
            k.dma("sp", u[:], g.zT[O_U + cc * 128:O_U + (cc + 1) * 128, :], r=[g.zT_r], w=[u])
            k.dma("sp", bg[:], g.zT[O_B + cc * 128:O_B + (cc + 1) * 128, :], r=[g.zT_r], w=[bg])
            k.dma("sp", cg[:], g.zT[O_C + cc * 128:O_C + (cc + 1) * 128, :], r=[g.zT_r], w=[cg])
            k.op("dve", lambda e: e.tensor_tensor(out=u[:], in0=u[:], in1=cg[:], op=ALU.mult), r=[cg, u], w=[u])
            k.op("dve", lambda e: e.tensor_scalar(out=y[:], in0=u[:], scalar1=cw[:, 1:2], scalar2=None, op0=ALU.mult),
                 r=[u, cw], w=[y])
            yl = y[:, CTX:].rearrange("p (r c) -> p r c", c=64)
            ul = u[:, CTX:].rearrange("p (r c) -> p r c", c=64)
            for (o_, i_, wi) in ((y[:, 1:CTX], u[:, 0:CTX - 1], 0), (y[:, 0:CTX - 1], u[:, 1:CTX], 2),
                                 (yl[:, :, 1:64], ul[:, :, 0:63], 0), (yl[:, :, 0:63], ul[:, :, 1:64], 2)):
                k.op("dve", lambda e, o_=o_, i_=i_, wi=wi: e.scalar_tensor_tensor(
                    out=o_, in0=i_, scalar=cw[:, wi:wi + 1], in1=o_, op0=ALU.mult, op1=ALU.add), r=[u, cw, y], w=[y])
            k.op("dve", lambda e: e.tensor_tensor(out=yo[:], in0=y[:], in1=bg[:], op=ALU.mult), r=[y, bg], w=[yo])
            k.dma("sp", g.ymix[512 + cc * 128:512 + (cc + 1) * 128, :], yo[:], r=[yo], w=[g.ymix_r])
        k.epoch()


def phase_fourier(g, l):
    nc, k = g.nc, g.k
    with ExitStack() as ph:
        sb = lambda n, s, d: T(ph.enter_context(nc.sbuf_tensor(uname(n), s, d)))
        bd = sb("bd", [128, 2, 128], BF16)
        k.dma("sp", bd[:], g.i["bd64"].rearrange("a p n -> p a n"), w=[bd])
        fst = sb("fst", [128, SEQ], F32)
        fb = sb("fb", [128, 2, SEQ], BF16)
        G = sb("G", [128, 32, 512], BF16)
        cb = [sb("dc%d" % i, [128, 32, 256], BF16) for i in range(2)]
        sbf = [sb("ds%d" % i, [128, 32, 256], BF16) for i in range(2)]
        yo = [sb("fyo%d" % i, [128, 256], BF16) for i in range(2)]
        it = 0
        for (c0, L, csrc) in ((0, CTX, g.i["dftC"]), (CTX, SEQ, g.i["dftL"])):
            if l == 1 and c0 == 0:
                continue
            ntt = L // 128
            for cc in range(2):
                k.dma("sp", fst[:, :L], g.zT[O_F + cc * 128:O_F + (cc + 1) * 128, c0:c0 + L], r=[g.zT_r], w=[fst])
                k.op("act", lambda e, cc=cc: e.activation(out=fb[:, cc, :L], in_=fst[:, :L], func=AF.Copy), r=[fst], w=[fb])
            for tt in range(ntt):
                ps = g.ps[tt % 2]
                for cs in range(2):
                    for cc in range(2):
                        k.op("pe", lambda e, ps=ps, cs=cs, cc=cc, tt=tt: e.matmul(
                            ps[:, (cs * 2 + cc) * 128:(cs * 2 + cc + 1) * 128], lhsT=fb[:, cc, tt * 128:(tt + 1) * 128],
                            rhs=bd[:, cs, :], start=True, stop=True), r=[fb, bd], w=[ps])
                if tt % 2 == 0:
                    k.op("act", lambda e, ps=ps, tt=tt: e.activation(out=G[:, tt, :], in_=ps[:, :], func=AF.Copy), r=[ps], w=[G])
                else:
                    k.op("dve", lambda e, ps=ps, tt=tt: e.tensor_copy(out=G[:, tt, :], in_=ps[:, :]), r=[ps], w=[G])
            for tp in range(L // 256):
                cbt, sbt = cb[tp % 2], sbf[tp % 2]
                k.dma("sp", cbt[:, :ntt, :], csrc[0, tp].rearrange("p (tt n) -> p tt n", n=256), w=[cbt])
                k.dma("sp", sbt[:, :ntt, :], csrc[1, tp].rearrange("p (tt n) -> p tt n", n=256), w=[sbt])
                for cc in range(2):
                    ps = g.ps[2 + it % 2]
                    y_ = yo[it % 2]
                    for tt in range(ntt):
                        k.op("pe", lambda e, ps=ps, cc=cc, tt=tt, cbt=cbt: e.matmul(
                            ps[:, :256], lhsT=G[:, tt, cc * 128:(cc + 1) * 128], rhs=cbt[:, tt, :],
                            start=(tt == 0), stop=False), r=[G, cbt], w=[ps])
                        k.op("pe", lambda e, ps=ps, cc=cc, tt=tt, sbt=sbt: e.matmul(
                            ps[:, :256], lhsT=G[:, tt, (2 + cc) * 128:(3 + cc) * 128], rhs=sbt[:, tt, :],
                            start=False, stop=(tt == ntt - 1)), r=[G, sbt], w=[ps])
                    k.op("act", lambda e, ps=ps, y_=y_: e.activation(out=y_[:], in_=ps[:, :256], func=AF.Copy), r=[ps], w=[y_])
                    k.dma("sp", g.ymix[768 + cc * 128:768 + (cc + 1) * 128, c0 + tp * 256:c0 + (tp + 1) * 256], y_[:],
                          r=[y_], w=[g.ymix_r])
                    it += 1
        k.epoch()


SEG = 512
NCH = TT // 64
DEC = -0.6065306597126334


def rwkv_segments(d):
    segs = [(0, CTX)] + [(CTX + i * SEG, SEG) for i in range(SEQ // SEG)]
    if d == 1:
        segs = [segs[0]] + segs[:0:-1]
    return segs


def phase_rwkv(g, l, last):
    nc, k = g.nc, g.k
    with ExitStack() as ph:
        sb = lambda n, s, d: T(ph.enter_context(nc.sbuf_tensor(uname(n), s, d)))
        par = sb("rpar", [64, 8, 2, 8], F32)
        omka = sb("omka", [64, 8], F32)
        lmu = sb("lmu", [64, 2, 2], F32)
        lora = sb("lora", [64, 2, 2, 512], F32)
        g2 = sb("g2w", [128, 512], F32)
        gn = sb("gnw", [64, 2, 512], F32)
        mask = sb("mask", [64, 2, 320], F32)
        ident = sb("ident", [64, 64], F32)
        ones64 = sb("ones64", [64, 64], F32)
        rmask = sb("rmask", [64, SEG], F32)
        sgl = sb("sgl", [128, TT], F32)
        k.dma("sp", par[:], g.i["rw_par"][l], w=[par])
        k.dma("sp", lmu[:], g.i["rw_lmu"][l], w=[lmu])
        k.dma("sp", lora[:], g.i["rw_lora"][l], w=[lora])
        k.dma("sp", g2[:], g.i["rw_g2"][l], w=[g2])
        k.dma("sp", gn[:], g.i["rw_gn"][l], w=[gn])
        k.dma("sp", mask[:], g.i["masks"], w=[mask])
        k.dma("sp", ident[:], g.i["ident"][0:64, 0:64], w=[ident])
        k.dma("sp", rmask[:], g.i["rmask"], w=[rmask])
        k.op("pool", lambda e: e.memset(ones64[:], 1.0), w=[ones64])
        k.op("dve", lambda e: e.tensor_scalar(out=omka[:], in0=par[:, :, 0, 6], scalar1=-1.0, scalar2=1.0,
                                              op0=ALU.mult, op1=ALU.add), r=[par], w=[omka])
        k.dma("sp", sgl[:], g.zT[O_G:O_G + 128, :], r=[g.zT_r], w=[sgl])
        k.op("act", lambda e: e.activation(out=sgl[:], in_=sgl[:], func=AF.Sigmoid), r=[sgl], w=[sgl])
        hz = {n: sb("hz_" + n, [64, SEG + 2], F32) for n in ("k", "v", "r", "w", "a")}
        wt0 = {n: sb("wt_" + n, [64, SEG], F32) for n in
               ("kl", "vl", "rl", "wl", "al", "sw", "sa", "kk", "t0", "t1", "kkn", "km", "bv", "Lp", "Ex", "En",
                "BT", "KT", "rk")}
        wt = wt0
        NC8 = SEG // 64
        Eis = [sb("Ei%d" % i, [64, SEG], F32) for i in range(3)]
        ARs = [sb("AR%d" % i, [64, NC8 * 128], F32) for i in range(3)]
        BTs = [wt0["BT"], sb("BTb", [64, SEG], F32)]
        KTs = [wt0["KT"], sb("KTb", [64, SEG], F32)]
        VLs = [wt0["vl"], sb("vlb", [64, SEG], F32)]
        RKs = [wt0["rk"], sb("rkb", [64, SEG], F32)]
        T3s = [sb("T3_%d" % i, [64, NC8, 4, 64], F32) for i in range(2)]
        ABKs = [sb("ABK%d" % i, [64, NC8, 256], F32) for i in range(2)]
        Wbs = [sb("Wb%d" % i, [64, NC8, 64], F32) for i in range(2)]
        N0 = sb("N0", [64, NC8, 64], F32)
        MNb = [sb("MNb%d" % i, [64, NC8, 128], F32) for i in range(2)]
        X = sb("Xs", [64, 64], F32)
        U = sb("Us", [64, 64], F32)
        Hs = [sb("Hs%d" % i, [64, 64], F32) for i in range(2)]
        Yd = sb("Yd", [64, NCH, 64], F32)
        Ysum = sb("Ysum", [64, NCH, 64], F32)
        st1 = sb("st1", [64, SEG // 64], F32)
        st2 = sb("st2", [64, SEG // 64], F32)
        ysq = sb("ysq", [64, SEG // 64, 64], F32)
        yfm = sb("yfm", [64, TT], BF16)
        pL = [g.ps[0], g.ps[1]]
        pN = g.ps[2]
        pB = [g.ps[3], g.ps[4]]
        pC = g.ps[5]
        pCh = g.ps[6]
        pH = g.ps[7]
        RX = RU = RH = RY = pCh
        RG = pN
        hi_ = 0

        def lerp(dst, src, S, d, mu_ap):
            cur = src[:, 1:S + 1]
            sh = src[:, 0:S] if d == 0 else src[:, 2:S + 2]
            k.op("pool", lambda e: e.tensor_tensor(out=wt["t0"][:, :S], in0=sh, in1=cur, op=ALU.subtract),
                 r=[src], w=[wt["t0"]])
            k.op("dve", lambda e: e.scalar_tensor_tensor(out=dst[:, :S], in0=wt["t0"][:, :S], scalar=mu_ap, in1=cur,
                                                         op0=ALU.mult, op1=ALU.add), r=[wt["t0"], src, lmu, par], w=[dst])

        tot = sb("tot", [64, SEG // 64], F32)
        st = {"hcur": 0}

        def stageB1(h, d, c0, S, b2, b3):
            wt = dict(wt0)
            wt.update(BT=BTs[b2], KT=KTs[b2], vl=VLs[b2], rk=RKs[b2])
            wt["Ei"] = Eis[b3]
            AR = ARs[b3]
            lo, hi = (0, CTX) if c0 < CTX else (CTX, TT)
            need_y = not (last and c0 < CTX)
            nch = S // 64
            rows = {"k": O_K + h * 64, "v": O_V + h * 64, "r": O_R + h * 64,
                    "w": (O_WF, O_WB)[d], "a": (O_AF, O_AB)[d]}
            for n_, t_ in hz.items():
                k.op("pool", lambda e, t_=t_: e.memset(t_[:], 0.0), w=[t_])
                a_, b_ = max(c0 - 1, lo), min(c0 + S + 1, hi)
                k.dma("sp", t_[:, a_ - (c0 - 1):b_ - (c0 - 1)], g.zT[rows[n_]:rows[n_] + 64, a_:b_],
                      r=[g.zT_r], w=[t_])
            P = lambda j: par[:, h, d, j:j + 1]
            lerp(wt["kl"], hz["k"], S, d, P(0))
            lerp(wt["vl"], hz["v"], S, d, P(1))
            lerp(wt["rl"], hz["r"], S, d, P(2))
            lerp(wt["wl"], hz["w"], S, d, lmu[:, d, 0:1])
            lerp(wt["al"], hz["a"], S, d, lmu[:, d, 1:2])
            k.op("act", lambda e: e.activation(out=wt["wl"][:, :S], in_=wt["wl"][:, :S], func=AF.Tanh),
                 r=[wt["wl"]], w=[wt["wl"]])
            for c5 in range(0, S, 512):
                n5 = min(512, S - c5)
                for (src, dst, li, bj) in ((wt["wl"], wt["sw"], 0, 3), (wt["al"], wt["sa"], 1, 4)):
                    k.op("pe", lambda e, src=src, li=li, c5=c5, n5=n5: e.matmul(
                        pH[0:64, :n5], lhsT=lora[:, d, li, h * 64:(h + 1) * 64], rhs=src[:, c5:c5 + n5],
                        start=True, stop=True), r=[lora, src], w=[pH])
                    k.op("act", lambda e, dst=dst, bj=bj, c5=c5, n5=n5: e.activation(
                        out=dst[:, c5:c5 + n5], in_=pH[0:64, :n5], func=AF.Sigmoid, bias=P(bj)), r=[pH, par], w=[dst])
            k.op("dve", lambda e: e.tensor_scalar(out=wt["kk"][:, :S], in0=wt["kl"][:, :S], scalar1=P(5), scalar2=None,
                                                  op0=ALU.mult), r=[wt["kl"], par], w=[wt["kk"]])
            k.op("pool", lambda e: e.tensor_tensor(out=wt["t0"][:, :S], in0=wt["kk"][:, :S], in1=wt["kk"][:, :S],
                                                   op=ALU.mult), r=[wt["kk"]], w=[wt["t0"]])
            for c5 in range(0, S, 512):
                n5 = min(512, S - c5)
                k.op("pe", lambda e, c5=c5, n5=n5: e.matmul(pH[0:64, :n5], lhsT=ones64[:], rhs=wt["t0"][:, c5:c5 + n5],
                                                           start=True, stop=True), r=[ones64, wt["t0"]], w=[pH])
                k.op("act", lambda e, c5=c5, n5=n5: e.activation(out=wt["t1"][:, c5:c5 + n5], in_=pH[0:64, :n5],
                                                                 func=AF.Sqrt), r=[pH], w=[wt["t1"]])
            k.op("dve", lambda e: e.tensor_scalar(out=wt["t1"][:, :S], in0=wt["t1"][:, :S], scalar1=1e-12, scalar2=None,
                                                  op0=ALU.max), r=[wt["t1"]], w=[wt["t1"]])
            k.op("dve", lambda e: e.reciprocal(out=wt["t1"][:, :S], in_=wt["t1"][:, :S]), r=[wt["t1"]], w=[wt["t1"]])
            k.op("pool", lambda e: e.tensor_tensor(out=wt["kkn"][:, :S], in0=wt["kk"][:, :S], in1=wt["t1"][:, :S],
                                                   op=ALU.mult), r=[wt["kk"], wt["t1"]], w=[wt["kkn"]])
            k.op("dve", lambda e: e.tensor_scalar(out=wt["t1"][:, :S], in0=wt["sa"][:, :S], scalar1=P(6),
                                                  scalar2=omka[:, h:h + 1], op0=ALU.mult, op1=ALU.add),
                 r=[wt["sa"], par, omka], w=[wt["t1"]])
            k.op("pool", lambda e: e.tensor_tensor(out=wt["km"][:, :S], in0=wt["kl"][:, :S], in1=wt["t1"][:, :S],
                                                   op=ALU.mult), r=[wt["kl"], wt["t1"]], w=[wt["km"]])
            k.op("pool", lambda e: e.tensor_tensor(out=wt["bv"][:, :S], in0=wt["kkn"][:, :S], in1=wt["sa"][:, :S],
                                                   op=ALU.mult), r=[wt["kkn"], wt["sa"]], w=[wt["bv"]])
            k.op("dve", lambda e: e.tensor_tensor_scan(out=wt["Lp"][:, :S], data0=rmask[:, :S], data1=wt["sw"][:, :S],
                                                       initial=0.0, op0=ALU.mult, op1=ALU.add),
                 r=[rmask, wt["sw"]], w=[wt["Lp"]])
            if d == 1:
                lp3 = wt["Lp"][:, :S].rearrange("p (c t) -> p c t", t=64)
                sw3 = wt["sw"][:, :S].rearrange("p (c t) -> p c t", t=64)
                t03 = wt["t0"][:, :S].rearrange("p (c t) -> p c t", t=64)
                k.op("pool", lambda e: e.tensor_tensor(out=wt["t0"][:, :S], in0=wt["sw"][:, :S], in1=wt["Lp"][:, :S],
                                                       op=ALU.subtract), r=[wt["sw"], wt["Lp"]], w=[wt["t0"]])
                k.op("dve", lambda e: e.tensor_copy(out=tot[:, :nch], in_=wt["Lp"][:, :S].rearrange("p (c t) -> p c t", t=64)[:, :, 63]),
                     r=[wt["Lp"]], w=[tot])
                k.op("dve", lambda e: e.tensor_tensor(out=lp3, in0=t03, in1=tot[:, :nch].unsqueeze(2).to_broadcast([64, nch, 64]),
                                                      op=ALU.add), r=[wt["t0"], tot], w=[wt["Lp"]])
            k.op("act", lambda e: e.activation(out=wt["Ei"][:, :S], in_=wt["Lp"][:, :S], func=AF.Exp, scale=DEC),
                 r=[wt["Lp"]], w=[wt["Ei"]])
            k.op("act", lambda e: e.activation(out=wt["En"][:, :S], in_=wt["Lp"][:, :S], func=AF.Exp, scale=-DEC),
                 r=[wt["Lp"]], w=[wt["En"]])
            k.op("pool", lambda e: e.tensor_tensor(out=wt["t0"][:, :S], in0=wt["Lp"][:, :S], in1=wt["sw"][:, :S],
                                                   op=ALU.subtract), r=[wt["sw"], wt["Lp"]], w=[wt["t0"]])
            k.op("act", lambda e: e.activation(out=wt["Ex"][:, :S], in_=wt["t0"][:, :S], func=AF.Exp, scale=DEC),
                 r=[wt["t0"]], w=[wt["Ex"]])
            ar4 = AR[:, :nch * 128].rearrange("p (c j t) -> p c j t", j=2, t=64)
            v3 = lambda t_: t_[:, :S].rearrange("p (c t) -> p c t", t=64)
            k.op("dve", lambda e: e.scalar_tensor_tensor(out=ar4[:, :, 0, :], in0=v3(wt["kkn"]), scalar=-1.0,
                                                         in1=v3(wt["Ex"]), op0=ALU.mult, op1=ALU.mult),
                 r=[wt["kkn"], wt["Ex"]], w=[AR])
            k.op("pool", lambda e: e.tensor_tensor(out=ar4[:, :, 1, :], in0=v3(wt["rl"]), in1=v3(wt["Ei"]), op=ALU.mult),
                 r=[wt["rl"], wt["Ei"]], w=[AR])
            k.op("dve", lambda e: e.tensor_tensor(out=wt["BT"][:, :S], in0=wt["bv"][:, :S], in1=wt["En"][:, :S], op=ALU.mult),
                 r=[wt["bv"], wt["En"]], w=[wt["BT"]])
            k.op("pool", lambda e: e.tensor_tensor(out=wt["KT"][:, :S], in0=wt["km"][:, :S], in1=wt["En"][:, :S], op=ALU.mult),
                 r=[wt["km"], wt["En"]], w=[wt["KT"]])
            if need_y:
                k.op("dve", lambda e: e.scalar_tensor_tensor(out=wt["rk"][:, :S], in0=wt["rl"][:, :S], scalar=P(7),
                                                             in1=wt["km"][:, :S], op0=ALU.mult, op1=ALU.mult),
                     r=[wt["rl"], wt["km"], par], w=[wt["rk"]])

        def stageB2(h, d, c0, S, b2, b3, buf):
            wt = dict(wt0)
            wt.update(BT=BTs[b2], KT=KTs[b2], vl=VLs[b2], rk=RKs[b2])
            wt["Ei"] = Eis[b3]
            AR = ARs[b3]
            T3, ABK, Wb = T3s[buf], ABKs[buf], Wbs[buf]
            need_y = not (last and c0 < CTX)
            nch = S // 64
            for c in range(nch if g.rw_stage >= 1 else 0):
                cs = slice(c * 64, (c + 1) * 64)
                for j, src in enumerate((wt["BT"], wt["KT"], wt["vl"])):
                    k.op("pe", lambda e, j=j, src=src, cs=cs: e.matmul(pN[0:64, j * 64:(j + 1) * 64], lhsT=src[:, cs], rhs=ident[:], start=True, stop=True),
                         r=[src, ident], w=[RG])
                if need_y:
                    k.op("pe", lambda e, cs=cs: e.matmul(pN[0:64, 192:256], lhsT=wt["rk"][:, cs], rhs=ones64[:],
                                                         start=True, stop=True), r=[wt["rk"], ones64], w=[RG])
                nj = 4 if need_y else 3
                k.op("dve", lambda e, c=c, nj=nj: e.tensor_copy(out=T3[:, c, 0:nj, :], in_=pN[0:64, 0:nj * 64].rearrange("p (j t) -> p j t", j=nj)),
                     r=[RG], w=[T3])
            if g.rw_stage >= 2:
                ar_of = lambda c: (AR[:, c * 128:c * 128 + 64], AR[:, c * 128 + 64:c * 128 + 128], AR[:, c * 128:c * 128 + 128])
                for h0 in range(0, nch, 4):
                    for c in range(h0, min(h0 + 4, nch)):
                        cs = slice(c * 64, (c + 1) * 64)
                        aT, rT, arT = ar_of(c)
                        pl = pL[(c % 4) // 2]
                        o0 = (c % 2) * 256
                        k.op("pe", lambda e, pl=pl, o0=o0, cs=cs, arT=arT: e.matmul(pl[0:64, o0:o0 + 128], lhsT=wt["BT"][:, cs], rhs=arT, start=True, stop=True),
                             r=[wt["BT"], AR], w=[pl])
                        k.op("pe", lambda e, pl=pl, o0=o0, cs=cs, arT=arT: e.matmul(pl[0:64, o0 + 128:o0 + 256], lhsT=wt["KT"][:, cs], rhs=arT, start=True, stop=True),
                             r=[wt["KT"], AR], w=[pl])
                        k.op("pe", lambda e, c=c, cs=cs, aT=aT: e.matmul(pN[0:64, c * 64:(c + 1) * 64], lhsT=aT, rhs=wt["BT"][:, cs], start=True, stop=True),
                             r=[wt["BT"], AR], w=[pN])
                    for j2 in range(2):
                        cA = h0 + j2 * 2
                        if cA >= nch:
                            continue
                        pl = pL[j2]
                        k.op("dve", lambda e, pl=pl, cA=cA: e.tensor_tensor(
                            out=ABK[:, cA:cA + 2, :], in0=pl[0:64, 0:512].rearrange("p (c x) -> p c x", c=2),
                            in1=mask[:, d, 0:256].unsqueeze(1).to_broadcast([64, 2, 256]), op=ALU.mult), r=[pl, mask], w=[ABK])
                k.op("dve", lambda e: e.tensor_tensor(out=N0[:, :nch, :], in0=pN[0:64, 0:nch * 64].rearrange("p (c x) -> p c x", x=64),
                                                      in1=mask[:, d, 256:320].unsqueeze(1).to_broadcast([64, nch, 64]), op=ALU.mult),
                     r=[pN, mask], w=[N0])
                k.op("pool", lambda e: e.tensor_tensor(out=Wb[:, :nch, :], in0=ABK[:, :nch, 0:64],
                                                       in1=ident[:].unsqueeze(1).to_broadcast([64, nch, 64]), op=ALU.add),
                     r=[ABK, ident], w=[Wb])
                for j in range(5):
                    lastj = j == 4
                    mn = MNb[j % 2]
                    for c in range(nch):
                        if j == 0:
                            Mj, Nj, Mr, Nr = ABK[:, c, 0:64], N0[:, c, :], ABK, N0
                        else:
                            pm = MNb[(j - 1) % 2]
                            Mj, Nj, Mr, Nr = pm[:, c, 0:64], pm[:, c, 64:128], pm, pm
                        pb = pB[c // 4]
                        o0 = (c % 4) * 128
                        if not lastj:
                            k.op("pe", lambda e, Mj=Mj, Nj=Nj, pb=pb, o0=o0: e.matmul(pb[0:64, o0:o0 + 64], lhsT=Nj, rhs=Mj, start=True, stop=True),
                                 r=[Mr, Nr], w=[pb])
                        k.op("pe", lambda e, Mj=Mj, Nj=Nj, pb=pb, o0=o0: e.matmul(pb[0:64, o0 + 64:o0 + 128], lhsT=Mj, rhs=Nj, start=True, stop=True),
                             r=[Mr, Nr], w=[pb])
                    for b4 in range((nch + 3) // 4):
                        n4 = min(4, nch - b4 * 4)
                        k.op("act", lambda e, b4=b4, n4=n4, mn=mn: e.activation(
                            out=mn[:, b4 * 4:b4 * 4 + n4, :], in_=pB[b4][0:64, 0:n4 * 128].rearrange("p (c x) -> p c x", x=128), func=AF.Copy),
                            r=[pB[b4]], w=[mn])
                    for c in range(nch):
                        k.op("pe", lambda e, c=c, mn=mn: e.matmul(pC[0:64, c * 64:(c + 1) * 64], lhsT=mn[:, c, 64:128], rhs=Wb[:, c, :], start=True, stop=True),
                             r=[mn, Wb], w=[pC])
                    wo_ = Wb
                    k.op("dve", lambda e, wo_=wo_: e.tensor_tensor(out=wo_[:, :nch, :], in0=pC[0:64, 0:nch * 64].rearrange("p (c x) -> p c x", x=64),
                                                                   in1=Wb[:, :nch, :], op=ALU.add), r=[pC, Wb], w=[wo_])

        def stageA(h, d, c0, S, buf, b3, first, last_ser, last_head):
            AR, T3, ABK, Wb = ARs[b3], T3s[buf], ABKs[buf], Wbs[buf]
            wt = dict(wt0)
            wt["Ei"] = Eis[b3]
            need_y = not (last and c0 < CTX)
            nch = S // 64
            ar_of = lambda c: (AR[:, c * 128:c * 128 + 64], AR[:, c * 128 + 64:c * 128 + 128], AR[:, c * 128:c * 128 + 128])
            if first:
                k.op("pool", lambda e: e.memset(Hs[0][:], 0.0), w=[Hs[0]])
                st["hcur"] = 0
            hcur = st["hcur"]
            if g.rw_stage >= 2:
                pass
            corder = range(nch) if d == 0 else range(nch - 1, -1, -1)
            for c in corder:
                aT, rT, arT = ar_of(c)
                Hn = Hs[1 - hcur]
                H = Hs[hcur]
                gc = c0 // 64 + c
                k.op("pe", lambda e, H=H, aT=aT: e.matmul(pCh[0:64, 0:64], lhsT=aT, rhs=H[:], start=True, stop=False),
                     r=[AR, H], w=[RX])
                k.op("pe", lambda e, c=c: e.matmul(pCh[0:64, 0:64], lhsT=ABK[:, c, 128:192], rhs=T3[:, c, 2, :], start=False, stop=True),
                     r=[ABK, T3], w=[RX])
                k.op("act", lambda e: e.activation(out=X[:], in_=pCh[0:64, 0:64], func=AF.Copy), r=[RX], w=[X])
                k.op("pe", lambda e, c=c: e.matmul(pCh[0:64, 64:128], lhsT=Wb[:, c, :], rhs=X[:], start=True, stop=True),
                     r=[Wb, X], w=[RU])
                k.op("dve", lambda e: e.tensor_copy(out=U[:], in_=pCh[0:64, 64:128]), r=[RU], w=[U])
                k.op("pe", lambda e, H=H: e.matmul(pCh[0:64, 192:256], lhsT=ident[:], rhs=H[:], start=True, stop=False),
                     r=[ident, H], w=[RH])
                k.op("pe", lambda e, c=c: e.matmul(pCh[0:64, 192:256], lhsT=T3[:, c, 0, :], rhs=U[:], start=False, stop=False),
                     r=[T3, U], w=[RH])
                k.op("pe", lambda e, c=c: e.matmul(pCh[0:64, 192:256], lhsT=T3[:, c, 1, :], rhs=T3[:, c, 2, :], start=False, stop=True),
                     r=[T3], w=[RH])
                pcol = c * 64 + (63 if d == 0 else 0)
                k.op("dve", lambda e, Hn=Hn, pcol=pcol: e.tensor_scalar(out=Hn[:], in0=pCh[0:64, 192:256],
                                                                         scalar1=wt["Ei"][:, pcol:pcol + 1], scalar2=None, op0=ALU.mult),
                     r=[RH, wt["Ei"]], w=[Hn])
                if need_y:
                    k.op("pe", lambda e, H=H, rT=rT: e.matmul(pCh[0:64, 128:192], lhsT=rT, rhs=H[:], start=True, stop=False),
                         r=[AR, H], w=[RY])
                    k.op("pe", lambda e, c=c: e.matmul(pCh[0:64, 128:192], lhsT=ABK[:, c, 64:128], rhs=U[:], start=False, stop=False),
                         r=[ABK, U], w=[RY])
                    k.op("pe", lambda e, c=c: e.matmul(pCh[0:64, 128:192], lhsT=ABK[:, c, 192:256], rhs=T3[:, c, 2, :], start=False, stop=True),
                         r=[ABK, T3], w=[RY])
                    k.op("act", lambda e, gc=gc: e.activation(out=Yd[:, gc, :], in_=pCh[0:64, 128:192], func=AF.Copy),
                         r=[RY], w=[Yd])
                hcur = 1 - hcur
            st["hcur"] = hcur
            if need_y and g.rw_stage >= 3:
                g0 = c0 // 64
                yv = Yd[:, g0:g0 + nch, :]
                bc = lambda ap: ap.unsqueeze(2).to_broadcast([64, nch, 64])
                k.op("dve", lambda e: e.tensor_reduce(out=st1[:, :nch], in_=yv, axis=AX.X, op=ALU.add), r=[Yd], w=[st1])
                k.op("pool", lambda e: e.tensor_tensor(out=ysq[:, :nch, :], in0=yv, in1=yv, op=ALU.mult), r=[Yd], w=[ysq])
                k.op("dve", lambda e: e.tensor_reduce(out=st2[:, :nch], in_=ysq[:, :nch, :], axis=AX.X, op=ALU.add), r=[ysq], w=[st2])
                k.op("dve", lambda e: e.tensor_scalar(out=st1[:, :nch], in0=st1[:, :nch], scalar1=1.0 / 64, scalar2=None, op0=ALU.mult),
                     r=[st1], w=[st1])
                k.op("dve", lambda e: e.scalar_tensor_tensor(out=ysq[:, :nch, 0], in0=st1[:, :nch], scalar=-1.0, in1=st1[:, :nch],
                                                             op0=ALU.mult, op1=ALU.mult), r=[st1], w=[ysq])
                k.op("dve", lambda e: e.scalar_tensor_tensor(out=st2[:, :nch], in0=st2[:, :nch], scalar=1.0 / 64, in1=ysq[:, :nch, 0],
                                                             op0=ALU.mult, op1=ALU.add), r=[st2, ysq], w=[st2])
                k.op("act", lambda e: e.activation(out=st2[:, :nch], in_=st2[:, :nch], func=AF.Sqrt, bias=g.gneps_t[0:64, :]),
                     r=[st2, g.gneps_t], w=[st2])
                k.op("dve", lambda e: e.reciprocal(out=st2[:, :nch], in_=st2[:, :nch]), r=[st2], w=[st2])
                k.op("dve", lambda e: e.tensor_tensor(out=yv, in0=yv, in1=bc(st1[:, :nch]), op=ALU.subtract), r=[Yd, st1], w=[Yd])
                k.op("dve", lambda e: e.tensor_tensor(out=yv, in0=yv, in1=bc(st2[:, :nch]), op=ALU.mult), r=[Yd, st2], w=[Yd])
                gb = lambda j: gn[:, j, h * 64:(h + 1) * 64].unsqueeze(1).to_broadcast([64, nch, 64])
                k.op("pool", lambda e: e.tensor_tensor(out=yv, in0=yv, in1=gb(0), op=ALU.mult), r=[Yd, gn], w=[Yd])
                k.op("pool", lambda e: e.tensor_tensor(out=yv, in0=yv, in1=gb(1), op=ALU.add), r=[Yd, gn], w=[Yd])
                k.op("pool", lambda e: e.tensor_tensor(out=ysq[:, :nch, :], in0=T3[:, :nch, 2, :], in1=T3[:, :nch, 3, :], op=ALU.mult),
                     r=[T3], w=[ysq])
                ys = Ysum[:, g0:g0 + nch, :]
                if d == 0:
                    k.op("dve", lambda e: e.tensor_tensor(out=ys, in0=yv, in1=ysq[:, :nch, :], op=ALU.add), r=[Yd, ysq], w=[Ysum])
                else:
                    k.op("dve", lambda e: e.tensor_tensor(out=yv, in0=yv, in1=ysq[:, :nch, :], op=ALU.add), r=[Yd, ysq], w=[Yd])
                    k.op("pool", lambda e: e.tensor_tensor(out=ys, in0=ys, in1=yv, op=ALU.add), r=[Yd, Ysum], w=[Ysum])
            if last_ser and g.dbg_state is not None:
                k.dma("sp", g.dbg_state[l, d, h], Hs[hcur][:], r=[Hs[hcur]])
            if last_head:
                pass
            if last_head and g.rw_stage >= 4:
                for c8 in range(0, NCH, 8):
                    n8 = min(8, NCH - c8)
                    for c in range(n8):
                        gc = c8 + c
                        k.op("pe", lambda e, c=c, gc=gc: e.matmul(pCh[0:64, c * 64:(c + 1) * 64], lhsT=sgl[:, gc * 64:(gc + 1) * 64],
                                                                  rhs=g2[:, h * 64:(h + 1) * 64], start=True, stop=True),
                             r=[sgl, g2], w=[pCh])
                    ysl = Ysum[:, c8:c8 + n8, :]
                    k.op("dve", lambda e, ysl=ysl, n8=n8: e.tensor_tensor(out=ysl, in0=ysl, in1=pCh[0:64, 0:n8 * 64].rearrange("p (c t) -> p c t", t=64),
                                                                          op=ALU.mult), r=[Ysum, pCh], w=[Ysum])
                    for c in range(n8):
                        gc = c8 + c
                        k.op("pe", lambda e, c=c, gc=gc: e.matmul(pCh[0:64, c * 64:(c + 1) * 64], lhsT=Ysum[:, gc, :], rhs=ident[:], start=True, stop=True),
                             r=[Ysum, ident], w=[pCh])
                    k.op("act", lambda e, c8=c8, n8=n8: e.activation(out=yfm[:, c8 * 64:(c8 + n8) * 64], in_=pCh[0:64, 0:n8 * 64], func=AF.Copy),
                         r=[pCh], w=[yfm])
                k.dma("sp", g.ymix[h * 64:(h + 1) * 64, :], yfm[:], r=[yfm], w=[g.ymix_r])

        jobs = []
        for h in g.heads:
            for d in range(2):
                segs = rwkv_segments(d)[:g.rw_maxseg]
                for si, (c0, S) in enumerate(segs):
                    n_ = len(jobs)
                    jobs.append(dict(h=h, d=d, c0=c0, S=S, buf=n_ % 2, b3=n_ % 3, first=(si == 0),
                                     last_ser=(si == len(segs) - 1), last_head=(d == 1 and si == len(segs) - 1)))
        J = len(jobs)
        for r_ in range(J + 2):
            fns, quota = [], []
            ja = jobs[r_ - 2] if 0 <= r_ - 2 < J else None
            jb = jobs[r_ - 1] if 0 <= r_ - 1 < J else None
            jc = jobs[r_] if r_ < J else None
            if ja is not None:
                fns.append(lambda p_=ja: stageA(p_["h"], p_["d"], p_["c0"], p_["S"], p_["buf"], p_["b3"], p_["first"],
                                                 p_["last_ser"], p_["last_head"]))
                quota.append(1)
            if jb is not None:
                fns.append(lambda p_=jb: stageB2(p_["h"], p_["d"], p_["c0"], p_["S"], p_["buf"], p_["b3"], p_["buf"]))
                quota.append(2)
            if jc is not None:
                fns.append(lambda p_=jc: stageB1(p_["h"], p_["d"], p_["c0"], p_["S"], p_["buf"], p_["b3"]))
                quota.append(1)
            weave(k, fns, quota)
            if ja is not None and ja["last_head"]:
                k.epoch()


def phase_wout(g, l):
    nc, k = g.nc, g.k
    with ExitStack() as ph:
        sb = lambda n, s, d: T(ph.enter_context(nc.sbuf_tensor(uname(n), s, d)))
        wo = sb("wo", [128, 8, D], BF16)
        wsrc = g.i["w_out"][l].rearrange("(kc p) n -> p kc n", p=128)
        for kc in range(8):
            k.dma("pool", wo[:, kc, :], wsrc[:, kc, :], w=[wo])
        yms = [sb("ym%d" % i, [128, 8, 512], BF16) for i in range(2)]
        xts = [sb("fx%d" % i, [128, 8, 512], F32) for i in range(2)]
        o = sb("fo", [128, 8, 512], F32)
        sq = sb("fsq", [128, 8, 512], BF16)
        h2f = sb("h2f", [128, 8, 512], F32)
        h2b = sb("h2b", [128, 8, 512], BF16)
        rstd = sb("frstd", [128, 512], F32)
        tmp = sb("ftmp", [128, 512], F32)
        tmp2 = [sb("ftmp2_%d" % i, [128, 512], F32) for i in range(2)]
        xv = g.xres.rearrange("(dc p) t -> p dc t", p=128)
        ymv = g.ymix.rearrange("(dc p) t -> p dc t", p=128)
        h2v = g.h2T.rearrange("(dc p) t -> p dc t", p=128)
        h2fv = g.h2F.rearrange("(dc p) t -> p dc t", p=128)
        zi = 0
        for ti, (c0, tn, wh) in enumerate(token_tiles()):
            if l == 1 and wh == 1:
                continue
            ym, xt = yms[ti % 2], xts[ti % 2]
            k.dma("sp", ym[:, :, :tn], ymv[:, :, c0:c0 + tn], r=[g.ymix_r], w=[ym])
            k.dma("sp", xt[:, :, :tn], xv[:, :, c0:c0 + tn], r=[g.xres_r], w=[xt])
            for dc in range(8):
                ps = g.ps[2 + (zi % 4)]
                zi += 1
                for kc in range(8):
                    k.op("pe", lambda e, ps=ps, kc=kc, dc=dc, ym=ym: e.matmul(
                        ps[:, :tn], lhsT=wo[:, kc, dc * 128:(dc + 1) * 128], rhs=ym[:, kc, :tn],
                        start=(kc == 0), stop=(kc == 7)), r=[wo, ym], w=[ps])
                if dc % 2 == 0:
                    k.op("act", lambda e, ps=ps, dc=dc: e.activation(out=o[:, dc, :tn], in_=ps[:, :tn], func=AF.Copy), r=[ps], w=[o])
                else:
                    k.op("dve", lambda e, ps=ps, dc=dc: e.tensor_copy(out=o[:, dc, :tn], in_=ps[:, :tn]), r=[ps], w=[o])
            rms_rstd(g, o, tn, sq, g.ps[1], rstd, tmp)
            for dc in range(8):
                t2 = tmp2[dc % 2]
                k.op("dve", lambda e, dc=dc, t2=t2: e.scalar_tensor_tensor(
                    out=t2[:, :tn], in0=o[:, dc, :tn], scalar=g.modv[:, 2, dc, wh:wh + 1], in1=rstd[:, :tn],
                    op0=ALU.mult, op1=ALU.mult), r=[o, rstd, g.modv], w=[t2])
                k.op("pool", lambda e, dc=dc, t2=t2, xt=xt: e.tensor_tensor(out=xt[:, dc, :tn], in0=xt[:, dc, :tn], in1=t2[:, :tn],
                                                                           op=ALU.add), r=[t2, xt], w=[xt])
            k.dma("sp", xv[:, :, c0:c0 + tn], xt[:, :, :tn], r=[xt], w=[g.xres_r])
            rms_rstd(g, xt, tn, sq, g.ps[1], rstd, tmp)
            for dc in range(8):
                t2 = tmp2[dc % 2]
                k.op("dve", lambda e, dc=dc, t2=t2, xt=xt: e.scalar_tensor_tensor(
                    out=t2[:, :tn], in0=xt[:, dc, :tn], scalar=g.modv[:, 3, dc, wh:wh + 1], in1=rstd[:, :tn],
                    op0=ALU.mult, op1=ALU.mult), r=[xt, rstd, g.modv], w=[t2])
                k.op("act", lambda e, dc=dc, t2=t2: e.activation(
                    out=h2f[:, dc, :tn], in_=t2[:, :tn], func=AF.Identity, bias=g.modv[:, 4, dc, wh:wh + 1]),
                    r=[t2, g.modv], w=[h2f])
            k.op("pool", lambda e: e.tensor_copy(out=h2b[:, :, :tn], in_=h2f[:, :, :tn]), r=[h2f], w=[h2b])
            k.dma("sp", h2v[:, :, c0:c0 + tn], h2b[:, :, :tn], r=[h2b], w=[g.h2T_r])
            if l == 1:
                k.dma("sp", h2fv[:, :, c0:c0 + tn], h2f[:, :, :tn], r=[h2f], w=[g.h2F_r])
        k.epoch()


def phase_router(g, l):
    nc, k = g.nc, g.k
    with ExitStack() as ph:
        sb = lambda n, s, d: T(ph.enter_context(nc.sbuf_tensor(uname(n), s, d)))
        rw = sb("rw", [128, 8, 8], F32)
        rb = sb("rb", [128, 4, 8], F32)
        idn = sb("idn", [128, 128], F32)
        k.dma("sp", rw[:], g.i["router_w"], w=[rw])
        k.dma("sp", rb[:], g.i["router_b"], w=[rb])
        k.dma("sp", idn[:], g.i["ident"], w=[idn])
        hf = [sb("rhf%d" % i, [128, 8, 512], F32) for i in range(2)]
        lg = sb("lg", [128, 4, 8], F32)
        lg2 = sb("lg2", [128, 4, 8], F32)
        eq = sb("eq", [128, 4, 8], F32)
        m1 = sb("m1", [128, 4], F32)
        m2 = sb("m2", [128, 4], F32)
        gT = sb("gTs", [8, 512], F32)
        h2fv = g.h2F.rearrange("(dc p) t -> p dc t", p=128)
        pX, pY = g.ps[0], g.ps[1]
        bc = lambda ap: ap.unsqueeze(2).to_broadcast([128, 4, 8])
        for ti in range(SEQ // 512):
            c0 = CTX + ti * 512
            h = hf[ti % 2]
            k.dma("sp", h[:], h2fv[:, :, c0:c0 + 512], r=[g.h2F_r], w=[h])
            for ch in range(4):
                for dc in range(8):
                    k.op("pe", lambda e, ch=ch, dc=dc, h=h: e.matmul(pX[:, ch * 8:(ch + 1) * 8], lhsT=h[:, dc, ch * 128:(ch + 1) * 128],
                                                                   rhs=rw[:, dc, :], start=(dc == 0), stop=(dc == 7)), r=[h, rw], w=[pX])
            k.op("dve", lambda e: e.tensor_tensor(out=lg[:], in0=pX[:, 0:32].rearrange("p (c e) -> p c e", e=8), in1=rb[:], op=ALU.add),
                 r=[pX, rb], w=[lg])
            k.op("dve", lambda e: e.tensor_reduce(out=m1[:], in_=lg[:], axis=AX.X, op=ALU.max), r=[lg], w=[m1])
            k.op("dve", lambda e: e.tensor_tensor(out=eq[:], in0=lg[:], in1=bc(m1[:]), op=ALU.is_equal), r=[lg, m1], w=[eq])
            k.op("dve", lambda e: e.scalar_tensor_tensor(out=lg2[:], in0=eq[:], scalar=-1e30, in1=lg[:], op0=ALU.mult, op1=ALU.add),
                 r=[eq, lg], w=[lg2])
            k.op("dve", lambda e: e.tensor_reduce(out=m2[:], in_=lg2[:], axis=AX.X, op=ALU.max), r=[lg2], w=[m2])
            k.op("dve", lambda e: e.tensor_tensor(out=eq[:], in0=lg[:], in1=bc(m2[:]), op=ALU.is_ge), r=[lg, m2], w=[eq])
            k.op("dve", lambda e: e.tensor_tensor(out=lg2[:], in0=lg[:], in1=bc(m1[:]), op=ALU.subtract), r=[lg, m1], w=[lg2])
            k.op("act", lambda e: e.activation(out=lg2[:], in_=lg2[:], func=AF.Exp), r=[lg2], w=[lg2])
            k.op("dve", lambda e: e.tensor_tensor(out=lg2[:], in0=lg2[:], in1=eq[:], op=ALU.mult), r=[lg2, eq], w=[lg2])
            k.op("dve", lambda e: e.tensor_reduce(out=m2[:], in_=lg2[:], axis=AX.X, op=ALU.add), r=[lg2], w=[m2])
            k.op("dve", lambda e: e.reciprocal(out=m2[:], in_=m2[:]), r=[m2], w=[m2])
            k.op("dve", lambda e: e.tensor_tensor(out=lg2[:], in0=lg2[:], in1=bc(m2[:]), op=ALU.mult), r=[lg2, m2], w=[lg2])
            for ch in range(4):
                k.op("pe", lambda e, ch=ch: e.matmul(pY[0:8, ch * 128:(ch + 1) * 128], lhsT=lg2[:, ch, :], rhs=idn[:], start=True, stop=True),
                     r=[lg2, idn], w=[pY])
            k.op("act", lambda e: e.activation(out=gT[:], in_=pY[0:8, :], func=AF.Copy), r=[pY], w=[gT])
            k.dma("sp", g.gT[:, c0:c0 + 512], gT[:], r=[gT], w=[g.gT_r])
        k.epoch()


def phase_ffn(g, l):
    nc, k = g.nc, g.k
    moe = (l % 2 == 1)
    NEXP = NE if moe else 1
    dff = DFE if moe else DFF
    groups = [(f0, min(512, dff - f0)) for f0 in range(0, dff, 512)]
    TB = 1024
    blocks = ([] if l == 1 else [(0, CTX, 1)]) + [(CTX + i * TB, TB, 0) for i in range(SEQ // TB)]
    with ExitStack() as ph:
        sb = lambda n, s, d: T(ph.enter_context(nc.sbuf_tensor(uname(n), s, d)))
        hb = sb("hb", [128, 8, TB], BF16)
        acc = sb("acc", [128, 8, TB], F32)
        wg = [sb("wg%d" % i, [128, 8, 512], BF16) for i in range(2)]
        wu = [sb("wu%d" % i, [128, 8, 512], BF16) for i in range(2)]
        wd = [sb("wd%d" % i, [128, 4, D], BF16) for i in range(2)]
        at = [sb("at%d" % i, [128, 4, 512], BF16) for i in range(2)]
        sg = [sb("sg%d" % i, [128, 512], F32) for i in range(2)]
        a1 = [sb("a1%d" % i, [128, 512], F32) for i in range(2)]
        xt = sb("gx", [128, 8, 512], F32)
        sq = sb("gsq", [128, 8, 512], BF16)
        rstd = sb("grstd", [128, 512], F32)
        tmp = sb("gtmp", [128, 512], F32)
        tmp2 = [sb("gtmp2_%d" % i, [128, 512], F32) for i in range(2)]
        if moe:
            gb = sb("gb", [128, NE, TB], BF16)
            gTs = sb("gTl", [8, TB], F32)
            sel = sb("sel", [8, NE, 128], F32)
            k.dma("sp", sel[:], g.i["sel"], w=[sel])
        h2v = g.h2T.rearrange("(dc p) t -> p dc t", p=128)
        xv = g.xres.rearrange("(dc p) t -> p dc t", p=128)
        ov = g.out.rearrange("(dc p) t -> p dc t", p=128)
        wi = 0
        ai = 0
        pi = 0
        import os
        bsel = os.environ.get("FFN_BLOCKS")
        if bsel:
            blocks = [blocks[int(x)] for x in bsel.split(",")]
        for (c0, tb, wh) in blocks:
            ntt = (tb + 511) // 512
            k.dma("sp", hb[:, :, :tb], h2v[:, :, c0:c0 + tb], r=[g.h2T_r], w=[hb])
            if moe:
                k.dma("sp", gTs[:, :tb], g.gT[:, c0:c0 + tb], r=[g.gT_r], w=[gTs])
                for e_ in range(NE):
                    for tt in range(ntt):
                        ps = g.ps[6 + (pi % 2)]
                        pi += 1
                        k.op("pe", lambda e, e_=e_, tt=tt, ps=ps: e.matmul(ps[:, :], lhsT=sel[:, e_, :], rhs=gTs[:, tt * 512:(tt + 1) * 512],
                                                                          start=True, stop=True), r=[sel, gTs], w=[ps])
                        k.op("act", lambda e, e_=e_, tt=tt, ps=ps: e.activation(out=gb[:, e_, tt * 512:(tt + 1) * 512], in_=ps[:, :], func=AF.Copy),
                             r=[ps], w=[gb])
            first = True
            for e_ in range(NEXP):
                if moe:
                    sg_, su_, sd_ = g.i["moe_w_gate"][e_], g.i["moe_w_up"][e_], g.i["moe_w_down"][e_]
                else:
                    sg_, su_, sd_ = g.i["ffn_w_gate"], g.i["ffn_w_up"], g.i["ffn_w_down"]
                for (f0, fw) in groups:
                    nfc = fw // 128
                    wgt, wut, wdt = wg[wi % 2], wu[wi % 2], wd[wi % 2]
                    wi += 1
                    gi = f0 // 512
                    bi = blocks.index((c0, tb, wh))
                    if moe and bi > 0:
                        rr = g.wscr_r[(e_, gi)]
                        k.dma("sp", wgt[:].rearrange("p a b -> p (a b)"), g.wscr[0][e_, gi], r=[rr], w=[wgt])
                        k.dma("sp", wut[:].rearrange("p a b -> p (a b)"), g.wscr[1][e_, gi], r=[rr], w=[wut])
                        k.dma("sp", wdt[:].rearrange("p a b -> p (a b)"), g.wscr[2][e_, gi], r=[rr], w=[wdt])
                    else:
                        for dc in range(8):
                            k.dma("pool", wgt[:, dc, :fw], sg_[dc * 128:(dc + 1) * 128, f0:f0 + fw], w=[wgt])
                            k.dma("pool", wut[:, dc, :fw], su_[dc * 128:(dc + 1) * 128, f0:f0 + fw], w=[wut])
                        for fc in range(nfc):
                            k.dma("pool", wdt[:, fc, :], sd_[f0 + fc * 128:f0 + (fc + 1) * 128, :], w=[wdt])
                        if moe:
                            rr = g.wscr_r[(e_, gi)] = Res()
                            k.dma("sp", g.wscr[0][e_, gi], wgt[:].rearrange("p a b -> p (a b)"), r=[wgt], w=[rr])
                            k.dma("sp", g.wscr[1][e_, gi], wut[:].rearrange("p a b -> p (a b)"), r=[wut], w=[rr])
                            k.dma("sp", g.wscr[2][e_, gi], wdt[:].rearrange("p a b -> p (a b)"), r=[wdt], w=[rr])
                    for tt in range(ntt):
                        tn = min(512, tb - tt * 512)
                        ts = slice(tt * 512, tt * 512 + tn)
                        a_ = at[ai % 2]
                        ai += 1
                        for fc in range(nfc):
                            pg, pu = g.ps[(pi % 2) * 2], g.ps[(pi % 2) * 2 + 1]
                            s_, a1_ = sg[pi % 2], a1[pi % 2]
                            pi += 1
                            for dc in range(8):
                                k.op("pe", lambda e, pg=pg, dc=dc, fc=fc, wgt=wgt, ts=ts: e.matmul(
                                    pg[:, :tn], lhsT=wgt[:, dc, fc * 128:(fc + 1) * 128], rhs=hb[:, dc, ts],
                                    start=(dc == 0), stop=(dc == 7)), r=[wgt, hb], w=[pg])
                            for dc in range(8):
                                k.op("pe", lambda e, pu=pu, dc=dc, fc=fc, wut=wut, ts=ts: e.matmul(
                                    pu[:, :tn], lhsT=wut[:, dc, fc * 128:(fc + 1) * 128], rhs=hb[:, dc, ts],
                                    start=(dc == 0), stop=(dc == 7)), r=[wut, hb], w=[pu])
                            k.op("act", lambda e, pg=pg, s_=s_: e.activation(out=s_[:, :tn], in_=pg[:, :tn], func=AF.Silu), r=[pg], w=[s_])
                            if moe:
                                k.op("dve", lambda e, pu=pu, s_=s_, a1_=a1_: e.tensor_tensor(out=a1_[:, :tn], in0=s_[:, :tn], in1=pu[:, :tn], op=ALU.mult),
                                     r=[pu, s_], w=[a1_])
                                k.op("pool", lambda e, a1_=a1_, a_=a_, fc=fc, e_=e_, ts=ts: e.tensor_tensor(out=a_[:, fc, :tn], in0=a1_[:, :tn], in1=gb[:, e_, ts],
                                                                                                     op=ALU.mult), r=[a1_, gb], w=[a_])
                            else:
                                k.op("dve", lambda e, pu=pu, s_=s_, a_=a_, fc=fc: e.tensor_tensor(out=a_[:, fc, :tn], in0=s_[:, :tn], in1=pu[:, :tn], op=ALU.mult),
                                     r=[pu, s_], w=[a_])
                        for dc in range(8):
                            po = g.ps[4 + (dc % 2)]
                            for fc in range(nfc):
                                k.op("pe", lambda e, po=po, dc=dc, fc=fc, wdt=wdt, a_=a_: e.matmul(
                                    po[:, :tn], lhsT=wdt[:, fc, dc * 128:(dc + 1) * 128], rhs=a_[:, fc, :tn],
                                    start=(fc == 0), stop=(fc == nfc - 1)), r=[wdt, a_], w=[po])
                            if first:
                                k.op("act", lambda e, po=po, dc=dc, ts=ts: e.activation(out=acc[:, dc, ts], in_=po[:, :tn], func=AF.Copy), r=[po], w=[acc])
                            else:
                                k.op("dve", lambda e, po=po, dc=dc, ts=ts: e.tensor_tensor(out=acc[:, dc, ts], in0=acc[:, dc, ts], in1=po[:, :tn], op=ALU.add),
                                     r=[po, acc], w=[acc])
                    first = False
            for tt in range(ntt):
                tn = min(512, tb - tt * 512)
                ts = slice(tt * 512, tt * 512 + tn)
                cc0 = c0 + tt * 512
                k.dma("sp", xt[:, :, :tn], xv[:, :, cc0:cc0 + tn], r=[g.xres_r], w=[xt])
                k.op("act", lambda e, ts=ts: e.activation(out=sq[:, :, :tn], in_=acc[:, :, ts], func=AF.Square), r=[acc], w=[sq])
                ps = g.ps[6]
                for dc in range(8):
                    k.op("pe", lambda e, dc=dc, ps=ps: e.matmul(ps[:, :tn], lhsT=g.ones_bf[:], rhs=sq[:, dc, :tn], start=(dc == 0), stop=(dc == 7)),
                         r=[sq, g.ones_bf], w=[ps])
                k.op("act", lambda e, ps=ps: e.activation(out=tmp[:, :tn], in_=ps[:, :tn], func=AF.Sqrt, scale=1.0 / D, bias=g.eps_t[:]),
                     r=[ps, g.eps_t], w=[tmp])
                k.op("dve", lambda e: e.reciprocal(out=rstd[:, :tn], in_=tmp[:, :tn]), r=[tmp], w=[rstd])
                for dc in range(8):
                    t2 = tmp2[dc % 2]
                    k.op("dve", lambda e, dc=dc, t2=t2, ts=ts: e.scalar_tensor_tensor(
                        out=t2[:, :tn], in0=acc[:, dc, ts], scalar=g.modv[:, 5, dc, wh:wh + 1], in1=rstd[:, :tn],
                        op0=ALU.mult, op1=ALU.mult), r=[acc, rstd, g.modv], w=[t2])
                    k.op("pool", lambda e, dc=dc, t2=t2: e.tensor_tensor(out=xt[:, dc, :tn], in0=xt[:, dc, :tn], in1=t2[:, :tn], op=ALU.add),
                         r=[t2, xt], w=[xt])
                if l == 1:
                    k.dma("sp", ov[:, :, cc0 - CTX:cc0 - CTX + tn], xt[:, :, :tn], r=[xt], w=[g.out_r])
                else:
                    k.dma("sp", xv[:, :, cc0:cc0 + tn], xt[:, :, :tn], r=[xt], w=[g.xres_r])
            k.epoch()

IN_SPECS = {
    "xT": ([D, TT], F32), "posT": ([D, SEQ], F32), "cvec": ([128, 8, 2], F32),
    "ada_w": ([2, D, 6 * D], F32), "ada_b": ([2, 128, 48], F32), "norm_g": ([2, 128, 4, 8], F32),
    "w_in": ([2, D, DIN], F32),
    "rw_par": ([2, 64, 8, 2, 8], F32), "rw_lmu": ([2, 64, 2, 2], F32), "rw_lora": ([2, 64, 2, 2, 512], F32),
    "rw_g2": ([2, 128, 512], F32), "rw_gn": ([2, 64, 2, 512], F32),
    "masks": ([64, 2, 320], F32), "ident": ([128, 128], F32), "rmask": ([64, SEG], F32),
    "conv_w": ([2, 2, 128, 3], F32),
    "w_out": ([2, D, D], F32),
    "ffn_w_gate": ([D, DFF], F32), "ffn_w_up": ([D, DFF], F32), "ffn_w_down": ([DFF, D], F32),
    "moe_w_gate": ([NE, D, DFE], F32), "moe_w_up": ([NE, D, DFE], F32), "moe_w_down": ([NE, DFE, D], F32),
    "router_w": ([128, 8, 8], F32), "router_b": ([128, 4, 8], F32), "sel": ([8, NE, 128], F32),
    "bd64": ([2, 128, 128], BF16), "dftL": ([2, 16, 128, 32 * 256], BF16), "dftC": ([2, 1, 128, 2 * 256], BF16),
}


def build(phases=None, dbg=(), ext_in=(), heads=range(8)):
    nc = bass.Bass("TRN2", target_bir_lowering=False)
    g = Ctx()
    g.nc = nc
    g.heads = list(heads)
    import os
    g.rw_stage = int(os.environ.get('RW_STAGE', '4'))
    g.rw_maxseg = int(os.environ.get('RW_MAXSEG', '99'))
    g.rw_s1 = os.environ.get('RW_S1', 'bad')
    g.i = {}
    if phases is None:
        phases = []
        for l in range(2):
            phases += [("mod", l), ("win", l), ("rwkv", l), ("conv", l), ("fourier", l), ("wout", l)]
            if l == 1:
                phases.append(("router", l))
            phases.append(("ffn", l))
    used = set()
    need = {"mod": ("cvec", "ada_w", "ada_b", "norm_g"), "win": ("xT", "posT", "w_in"),
            "rwkv": ("rw_par", "rw_lmu", "rw_lora", "rw_g2", "rw_gn", "masks", "ident", "rmask"),
            "conv": ("conv_w",), "fourier": ("bd64", "dftL", "dftC"), "wout": ("w_out",),
            "router": ("router_w", "router_b", "ident"), "ffn": ()}
    for (p, l) in phases:
        used.update(need[p])
        if p == "ffn":
            used.update(("moe_w_gate", "moe_w_up", "moe_w_down", "sel") if l == 1 else ("ffn_w_gate", "ffn_w_up", "ffn_w_down"))
    for name, (shape, dt) in IN_SPECS.items():
        if name in used:
            g.i[name] = nc.dram_tensor(name, shape, dt, kind="ExternalInput").ap()
    g.in_names = sorted(used)

    def scratch(name, shape, dt):
        kind = "ExternalOutput" if name in dbg else ("ExternalInput" if name in ext_in else "Internal")
        return nc.dram_tensor(name, shape, dt, kind=kind).ap()
    g.xres = scratch("xres", [D, TT], F32) if "xres_in" not in ext_in else nc.dram_tensor("xres", [D, TT], F32, kind="ExternalOutput").ap()
    g.xres_in = nc.dram_tensor("xres_in", [D, TT], F32, kind="ExternalInput").ap() if "xres_in" in ext_in else None
    g.zT = scratch("zT", [DIN, TT], F32)
    g.ymix = scratch("ymix", [D, TT], BF16)
    g.h2T = scratch("h2T", [D, TT], BF16)
    g.h2F = scratch("h2F", [D, TT], F32)
    g.gT = scratch("gT", [8, TT], F32)
    g.wscr = [scratch("wscr%d" % i, [NE, DFE // 512, 128, 4096], BF16) for i in range(3)]
    g.wscr_r = {}
    g.xres_r, g.zT_r, g.ymix_r, g.h2T_r, g.h2F_r, g.gT_r, g.out_r = [Res() for _ in range(7)]
    g.dbg_state = None
    if "state" in dbg:
        g.dbg_state = nc.dram_tensor("state", [2, 2, 8, 64, 64], F32, kind="ExternalOutput").ap()
    g.out = nc.dram_tensor("out", [D, SEQ], F32, kind="ExternalOutput").ap()
    with ExitStack() as es:
        g.k = k = KB(nc, es)
        sb = lambda n, s, d: T(es.enter_context(nc.sbuf_tensor(uname(n), s, d)))
        g.ps = [T(es.enter_context(nc.psum_tensor("ps%d" % i, [128, 512], F32))) for i in range(8)]
        g.modv = sb("modv", [128, 6, 8, 2], F32)
        g.ones_bf = sb("ones_bf", [128, 128], BF16)
        g.eps_t = sb("eps_t", [128, 1], F32)
        g.gneps_t = sb("gneps_t", [128, 1], F32)
        k.op("dve", lambda e: e.memset(g.ones_bf[:], 1.0), w=[g.ones_bf])
        k.op("dve", lambda e: e.memset(g.eps_t[:], EPS), w=[g.eps_t])
        k.op("dve", lambda e: e.memset(g.gneps_t[:], GN_EPS), w=[g.gneps_t])
        if g.xres_in is not None:
            for i_ in range(32):
                k.dma("sp", g.xres[i_ * 32:(i_ + 1) * 32, :], g.xres_in[i_ * 32:(i_ + 1) * 32, :], w=[g.xres_r])
        fns = {"mod": phase_mod, "win": phase_win, "conv": phase_conv, "fourier": phase_fourier,
               "wout": phase_wout, "router": phase_router, "ffn": phase_ffn}
        for (p, l) in phases:
            if p == "rwkv":
                phase_rwkv(g, l, l == 1)
            else:
                fns[p](g, l)
        k.barrier()
        if "modv" in dbg:
            md = nc.dram_tensor("modv_o", [128, 96], F32, kind="ExternalOutput").ap()
            k.dma("sp", md, g.modv[:].rearrange("p a b c -> p (a b c)"), r=[g.modv])
        k.barrier(["sp"])
    print("instructions", k.nins, "waits", k.nwait)
    return nc, g


def sincos_pos():
    quarter = D // 4
    omega = 1.0 / (10000.0 ** (np.arange(quarter, dtype=np.float32) / quarter))

    def axis_emb(n):
        ang = np.arange(n, dtype=np.float32)[:, None] * omega[None, :]
        return np.concatenate([np.sin(ang), np.cos(ang)], axis=-1)
    er, ec = axis_emb(64), axis_emb(64)
    emb = np.concatenate([np.broadcast_to(er[:, None, :], (64, 64, D // 2)),
                          np.broadcast_to(ec[None, :, :], (64, 64, D // 2))], axis=-1)
    return np.ascontiguousarray(emb.reshape(SEQ, D).T.astype(np.float32))


_CONST = {}


def constants():
    if _CONST:
        return _CONST
    import ml_dtypes
    bf = ml_dtypes.bfloat16
    c = _CONST
    c["posT"] = sincos_pos()
    c["ident"] = np.eye(128, dtype=np.float32)
    s_ = np.arange(64)[:, None]
    t_ = np.arange(64)[None, :]
    m = np.zeros((64, 2, 320), np.float32)
    for d, (st, inc) in enumerate((((s_ < t_), (s_ <= t_)), ((s_ > t_), (s_ >= t_)))):
        m[:, d, 0:64] = st
        m[:, d, 64:128] = inc
        m[:, d, 128:192] = st
        m[:, d, 192:256] = inc
        m[:, d, 256:320] = st.T
    c["masks"] = m
    sel = np.zeros((8, NE, 128), np.float32)
    for e_ in range(NE):
        sel[e_, e_, :] = 1.0
    c["sel"] = sel
    rm = np.ones((64, SEG), np.float32)
    rm[:, ::64] = 0.0
    c["rmask"] = rm
    a64 = 2 * np.pi * np.outer(np.arange(64), np.arange(64)) / 64
    bd = np.zeros((2, 128, 128), np.float64)
    for i in range(2):
        bd[0, i * 64:(i + 1) * 64, i * 64:(i + 1) * 64] = np.cos(a64)
        bd[1, i * 64:(i + 1) * 64, i * 64:(i + 1) * 64] = np.sin(a64)
    c["bd64"] = bd.astype(bf)
    for name, L in (("dftL", SEQ), ("dftC", CTX)):
        idx = (np.outer(np.arange(L), np.arange(L)) % L).astype(np.float64)
        ang = 2 * np.pi * idx / L
        sc = 1.0 / np.sqrt(64.0 * L)
        mats = np.stack([np.cos(ang) * sc, -np.sin(ang) * sc])
        ntt, ntp = L // 128, L // 256
        mats = mats.reshape(2, ntt, 128, ntp, 256).transpose(0, 3, 2, 1, 4)
        c[name] = np.ascontiguousarray(mats.reshape(2, ntp, 128, ntt * 256)).astype(bf)
    return c


def make_in_maps(inp, names, cores=range(8)):
    f32 = lambda a: np.ascontiguousarray(np.asarray(a, dtype=np.float32))
    shared = dict(constants())
    shared["ada_w"] = f32(inp["ada_w"])
    shared["ada_b"] = f32(inp["ada_b"].reshape(2, 48, 128).transpose(0, 2, 1))
    shared["norm_g"] = f32(inp["norm_g"].reshape(2, 4, 8, 128).transpose(0, 3, 1, 2))
    shared["w_in"] = f32(inp["w_in"])
    par = np.zeros((2, 64, 8, 2, 8), np.float32)
    hp = lambda a: a.reshape(2, 8, 64).transpose(0, 2, 1)
    for d in range(2):
        for j in range(3):
            par[:, :, :, d, j] = hp(inp["rwkv_mu"][:, d, j])
        par[:, :, :, d, 3] = hp(inp["rwkv_w0"][:, d])
        par[:, :, :, d, 4] = hp(inp["rwkv_a0"][:, d])
        par[:, :, :, d, 5] = hp(inp["rwkv_k_k"])
        par[:, :, :, d, 6] = hp(inp["rwkv_k_a"])
        par[:, :, :, d, 7] = hp(inp["rwkv_r_k"].reshape(2, 512))
    shared["rw_par"] = par
    shared["rw_lmu"] = f32(np.stack([inp["rwkv_mu_w"], inp["rwkv_mu_a"]], axis=-1).transpose(0, 2, 1, 3))
    shared["rw_lora"] = f32(np.stack([inp["rwkv_w2"], inp["rwkv_a2"]], axis=2).transpose(0, 3, 1, 2, 4))
    shared["rw_g2"] = f32(inp["rwkv_g2"])
    gn = np.stack([inp["rwkv_gn_w"], inp["rwkv_gn_b"]], axis=1)
    shared["rw_gn"] = f32(np.broadcast_to(gn[:, None], (2, 64, 2, 512)))
    shared["conv_w"] = f32(inp["conv_w"].transpose(0, 2, 1).reshape(2, 2, 128, 3))
    shared["w_out"] = f32(inp["w_out"])
    for n_ in ("ffn_w_gate", "ffn_w_up", "ffn_w_down", "moe_w_gate", "moe_w_up", "moe_w_down"):
        if n_ in names:
            shared[n_] = f32(inp[n_][0])
    shared["router_w"] = f32(inp["router_w"][0].reshape(8, 128, 8).transpose(1, 0, 2))
    shared["router_b"] = f32(np.broadcast_to(inp["router_b"][0][None, None, :], (128, 4, 8)))
    maps = []
    for b in cores:
        m = {n: shared[n] for n in names if n in shared}
        if "xT" in names:
            m["xT"] = f32(np.concatenate([inp["ctx"][b].T, inp["x"][b].T], axis=1))
        if "cvec" in names:
            m["cvec"] = f32(np.stack([inp["c"][b].reshape(8, 128).T, inp["c_ctx"].reshape(8, 128).T], axis=-1))
        maps.append(m)
    return maps


def kernel(**inp):
    inp = {k_: np.asarray(v) for k_, v in inp.items()}
    nc, g = build()
    maps = make_in_maps(inp, g.in_names)
    res = run_bass_kernel_spmd(nc, maps, core_ids=list(range(8)))
    out = np.stack([np.ascontiguousarray(r["out"].T) for r in res.results], axis=0)
    return out.astype(np.float32)
```

```python
import numpy as np
import concourse.bass as bass
import concourse.mybir as mybir
from concourse.bass_utils import run_bass_kernel_spmd
from contextlib import ExitStack

F32 = mybir.dt.float32
BF16 = mybir.dt.bfloat16
AF = mybir.ActivationFunctionType
ALU = mybir.AluOpType
AX = mybir.AxisListType
NDS = 40
NPL = 8

D = 1024
SEQ = 4096
CTX = 256
TT = CTX + SEQ
DIN = 2944
DFF = 2816
DFE = 3584
NE = 8
EPS = 1e-6
GN_EPS = 64e-5
O_K, O_V, O_WF, O_WB, O_AF, O_AB, O_R, O_G, O_U, O_B, O_C, O_F = (
    0, 512, 1024, 1088, 1152, 1216, 1280, 1792, 1920, 2176, 2432, 2688)


class Res:
    __slots__ = ("w", "r")

    def __init__(self):
        self.w = None
        self.r = []


class T:
    def __init__(self, h, res=None):
        self.h = h
        self.res = res if res is not None else Res()

    def __getitem__(self, idx):
        return self.h[idx]


def _res(x):
    return x.res if isinstance(x, T) else x


class KB:
    ENG = ("pe", "act", "dve", "pool", "sp")

    def __init__(self, nc, es):
        self.nc = nc
        self.eng = dict(pe=nc.tensor, act=nc.scalar, dve=nc.vector, pool=nc.gpsimd, sp=nc.sync)
        self.sems = [{e: es.enter_context(nc.semaphore("s%d_%s" % (i, e))) for e in self.ENG} for i in range(2)]
        self.dsemss = [[es.enter_context(nc.semaphore("d%d_%d" % (j, i))) for i in range(NDS - NPL)] for j in range(2)]
        self.psems = [es.enter_context(nc.semaphore("p%d" % i)) for i in range(NPL)]
        self.pcnt = [0] * NPL
        self.pnext = 0
        self.cur = 0
        self.ep = 0
        self._reset()
        self.nwait = 0
        self.nins = 0
        self.hook = None

    def _reset(self):
        self.sem = self.sems[self.cur]
        self.dsems = self.dsemss[self.cur]
        self.cnt = {e: 0 for e in self.ENG}
        self.seen = {e: {} for e in self.ENG}
        self.dcnt = [0] * (NDS - NPL)
        self.dnext = 0

    def _wait(self, eng, tok):
        if tok is None or tok[3] != self.ep:
            return
        kind, a, n, _ = tok
        if kind == "c":
            if a == "pe" and eng == "pe":
                return
            sem, val, key = self.sem[a], n, a
        elif kind == "p":
            sem, val, key = self.psems[a], 16 * n, "p%d" % a
        else:
            sem, val, key = self.dsems[a], 16 * n, "d%d" % a
        if self.seen[eng].get(key, 0) >= val:
            return
        self.eng[eng].wait_ge(sem, val)
        self.seen[eng][key] = val
        self.nwait += 1

    def _deps(self, eng, r, w):
        toks = []
        for x in r:
            toks.append(_res(x).w)
        for x in w:
            rs = _res(x)
            toks.append(rs.w)
            toks.extend(rs.r)
        for t in toks:
            self._wait(eng, t)

    def _mark(self, tok, r, w):
        for x in r:
            rs = _res(x)
            rs.r = [t for t in rs.r if t[3] == self.ep and not (t[0] == tok[0] and t[1] == tok[1])]
            rs.r.append(tok)
        for x in w:
            rs = _res(x)
            rs.w = tok
            rs.r = []

    def op(self, eng, fn, r=(), w=()):
        self._deps(eng, r, w)
        ins = fn(self.eng[eng])
        self.cnt[eng] += 1
        ins.then_inc(self.sem[eng], 1)
        self.nins += 1
        tok = ("c", eng, self.cnt[eng], self.ep)
        self._mark(tok, r, w)
        if self.hook:
            self.hook()
        return tok

    def dma(self, q, out, in_, r=(), w=(), **kw):
        if q == "pool":
            i = self.pnext
            self.pnext = (i + 1) % NPL
            if self.pcnt[i] > 0:
                self.seen[q].pop("p%d" % i, None)
                self.eng[q].wait_ge(self.psems[i], 16 * self.pcnt[i])
            self._deps(q, r, w)
            ins = self.eng[q].dma_start(out=out, in_=in_, **kw)
            self.pcnt[i] += 1
            ins.then_inc(self.psems[i], 16)
            self.nins += 1
            tok = ("p", i, self.pcnt[i], self.ep)
            self._mark(tok, r, w)
            return tok
        i = self.dnext
        self.dnext = (i + 1) % (NDS - NPL)
        if self.dcnt[i] > 0:
            self._wait(q, ("d", i, self.dcnt[i], self.ep))
        self._deps(q, r, w)
        ins = self.eng[q].dma_start(out=out, in_=in_, **kw)
        self.dcnt[i] += 1
        ins.then_inc(self.dsems[i], 16)
        self.nins += 1
        tok = ("d", i, self.dcnt[i], self.ep)
        self._mark(tok, r, w)
        return tok

    def barrier(self, engines=None):
        engines = engines or self.ENG
        for e in engines:
            for f in self.ENG:
                if self.cnt[f] > 0 and f != e:
                    self._wait(e, ("c", f, self.cnt[f], self.ep))
            if e != "pe" and self.cnt[e] > 0:
                self._wait(e, ("c", e, self.cnt[e], self.ep))
            for i in range(NDS - NPL):
                if self.dcnt[i] > 0:
                    self._wait(e, ("d", i, self.dcnt[i], self.ep))
            for i in range(NPL):
                if self.pcnt[i] > 0:
                    self.seen[e].pop("p%d" % i, None)
                    self._wait(e, ("p", i, self.pcnt[i], self.ep))

    def epoch(self):
        self.barrier()
        old_sem, old_d = self.sem, self.dsems
        self.cur = 1 - self.cur
        self.ep += 1
        self._reset()
        for e in self.ENG:
            ins = self.eng[e].nop()
            self.cnt[e] = 1
            ins.then_inc(self.sem[e], 1)
        for e in self.ENG:
            if e != "sp":
                self._wait("sp", ("c", e, 1, self.ep))
        for s_ in list(old_sem.values()) + list(old_d):
            self.eng["sp"].sem_clear(s_)
        ins = self.eng["sp"].nop()
        self.cnt["sp"] += 1
        ins.then_inc(self.sem["sp"], 1)


class Ctx:
    pass


def weave(k, fns, quota):
    import threading
    n = len(fns)
    if n == 1:
        fns[0]()
        return
    sems = [threading.Semaphore(0) for _ in fns]
    main = threading.Semaphore(0)
    done = [False] * n
    state = {"cur": 0, "left": quota[0], "err": None}

    def nxt(i):
        for j in range(1, n + 1):
            t = (i + j) % n
            if not done[t]:
                return t
        return None

    def hook():
        i = state["cur"]
        state["left"] -= 1
        if state["left"] <= 0:
            t = nxt(i)
            if t is not None and t != i:
                state["cur"] = t
                state["left"] = quota[t]
                sems[t].release()
                sems[i].acquire()
            else:
                state["left"] = quota[i]

    def runner(i):
        sems[i].acquire()
        try:
            fns[i]()
        except BaseException as ex:
            state["err"] = ex
        finally:
            done[i] = True
            t = nxt(i)
            if t is not None:
                state["cur"] = t
                state["left"] = quota[t]
                sems[t].release()
            else:
                main.release()

    k.hook = hook
    threads = [threading.Thread(target=runner, args=(i,)) for i in range(n)]
    for t in threads:
        t.start()
    sems[0].release()
    main.acquire()
    for t in threads:
        t.join()
    k.hook = None
    if state["err"] is not None:
        raise state["err"]


_UID = [0]


def uname(n):
    _UID[0] += 1
    return "%s_%d" % (n, _UID[0])


def token_tiles():
    tiles = [(0, CTX, 1)]
    for i in range(SEQ // 512):
        tiles.append((CTX + i * 512, 512, 0))
    return tiles


def phase_mod(g, l):
    nc, k = g.nc, g.k
    with ExitStack() as ph:
        sb = lambda n, s, d: T(ph.enter_context(nc.sbuf_tensor(uname(n), s, d)))
        cv = sb("cv", [128, 8, 2], F32)
        scv = sb("scv", [128, 8, 2], F32)
        adab = sb("adab", [128, 48], F32)
        ng = sb("ng", [128, 4, 8], F32)
        raw = sb("raw", [128, 48, 2], F32)
        wbuf = [sb("adaw%d" % i, [128, 8, 512], F32) for i in range(2)]
        ps = g.ps[0]
        k.dma("sp", cv[:], g.i["cvec"], w=[cv])
        k.dma("sp", adab[:], g.i["ada_b"][l], w=[adab])
        k.dma("sp", ng[:], g.i["norm_g"][l], w=[ng])
        k.op("act", lambda e: e.activation(out=scv[:], in_=cv[:], func=AF.Silu), r=[cv], w=[scv])
        wsrc = g.i["ada_w"][l].rearrange("(kc p) n -> p kc n", p=128)
        psv = ps[:, 0:96].rearrange("p (n w) -> p n w", w=2)
        for nb in range(12):
            wb = wbuf[nb % 2]
            k.dma("sp", wb[:], wsrc[:, :, nb * 512:(nb + 1) * 512], w=[wb])
            for j in range(4):
                n = nb * 4 + j
                for kc in range(8):
                    k.op("pe", lambda e, n=n, j=j, kc=kc, wb=wb: e.matmul(
                        psv[:, n, :], lhsT=wb[:, kc, j * 128:(j + 1) * 128], rhs=scv[:, kc, :],
                        start=(kc == 0), stop=(kc == 7)), r=[wb, scv], w=[ps])
        for w_ in range(2):
            k.op("dve", lambda e, w_=w_: e.tensor_tensor(out=raw[:, :, w_], in0=psv[:, :, w_], in1=adab[:],
                                                  op=ALU.add), r=[ps, adab], w=[raw])
        mv = g.modv
        for w_ in range(2):
            for (slot, jsc, jsh, jg, n0, n1) in ((0, 1, 0, 2, 0, 1), (3, 4, 3, 5, 2, 3)):
                k.op("dve", lambda e, w_=w_, slot=slot, jsc=jsc, n0=n0: e.scalar_tensor_tensor(
                    out=mv[:, slot, :, w_], in0=raw[:, jsc * 8:(jsc + 1) * 8, w_], scalar=1.0, in1=ng[:, n0, :],
                    op0=ALU.add, op1=ALU.mult), r=[raw, ng], w=[mv])
                k.op("dve", lambda e, w_=w_, slot=slot, jsh=jsh: e.tensor_copy(
                    out=mv[:, slot + 1, :, w_], in_=raw[:, jsh * 8:(jsh + 1) * 8, w_]), r=[raw], w=[mv])
                k.op("dve", lambda e, w_=w_, slot=slot, jg=jg, n1=n1: e.tensor_tensor(
                    out=mv[:, slot + 2, :, w_], in0=raw[:, jg * 8:(jg + 1) * 8, w_], in1=ng[:, n1, :],
                    op=ALU.mult), r=[raw, ng], w=[mv])
        k.epoch()


def rms_rstd(g, xt, tn, sq, ps, rstd, tmp):
    k = g.k
    k.op("act", lambda e: e.activation(out=sq[:, :, :tn], in_=xt[:, :, :tn], func=AF.Square), r=[xt], w=[sq])
    for dc in range(8):
        k.op("pe", lambda e, dc=dc: e.matmul(ps[:, :tn], lhsT=g.ones_bf[:], rhs=sq[:, dc, :tn],
                                             start=(dc == 0), stop=(dc == 7)), r=[sq, g.ones_bf], w=[ps])
    k.op("act", lambda e: e.activation(out=tmp[:, :tn], in_=ps[:, :tn], func=AF.Sqrt, scale=1.0 / D, bias=g.eps_t[:]),
         r=[ps, g.eps_t], w=[tmp])
    k.op("dve", lambda e: e.reciprocal(out=rstd[:, :tn], in_=tmp[:, :tn]), r=[tmp], w=[rstd])


def phase_win(g, l):
    nc, k = g.nc, g.k
    with ExitStack() as ph:
        sb = lambda n, s, d: T(ph.enter_context(nc.sbuf_tensor(uname(n), s, d)))
        win = sb("win", [128, 8, DIN], BF16)
        wsrc = g.i["w_in"][l].rearrange("(kc p) n -> p kc n", p=128)
        for kc in range(8):
            k.dma("pool", win[:, kc, :], wsrc[:, kc, :], w=[win])
        xts = [sb("xt%d" % i, [128, 8, 512], F32) for i in range(2)]
        pts = [sb("pt%d" % i, [128, 8, 512], F32) for i in range(2)]
        sq = sb("sq", [128, 8, 512], BF16)
        hT = [sb("hT%d" % i, [128, 8, 512], BF16) for i in range(2)]
        rstd = sb("rstd", [128, 512], F32)
        tmp = sb("tmp", [128, 512], F32)
        tmp2 = [sb("tmp2_%d" % i, [128, 512], F32) for i in range(2)]
        zsb = [sb("zsb%d" % i, [128, 512], F32) for i in range(4)]
        xsrc = (g.i["xT"] if l == 0 else g.xres).rearrange("(dc p) t -> p dc t", p=128)
        xdst = g.xres.rearrange("(dc p) t -> p dc t", p=128)
        psrc = g.i["posT"].rearrange("(dc p) t -> p dc t", p=128)
        zi = 0
        for ti, (c0, tn, wh) in enumerate(token_tiles()):
            xt = xts[ti % 2]
            h = hT[ti % 2]
            k.dma("sp", xt[:, :, :tn], xsrc[:, :, c0:c0 + tn], r=[g.xres_r] if l else [], w=[xt])
            if l == 0:
                if wh == 0:
                    pt = pts[ti % 2]
                    k.dma("sp", pt[:, :, :tn], psrc[:, :, c0 - CTX:c0 - CTX + tn], w=[pt])
                    k.op("pool", lambda e, xt=xt, pt=pt: e.tensor_tensor(out=xt[:], in0=xt[:], in1=pt[:], op=ALU.add),
                         r=[pt, xt], w=[xt])
                k.dma("sp", xdst[:, :, c0:c0 + tn], xt[:, :, :tn], r=[xt], w=[g.xres_r])
            rms_rstd(g, xt, tn, sq, g.ps[1], rstd, tmp)
            for dc in range(8):
                t2 = tmp2[dc % 2]
                k.op("dve", lambda e, dc=dc, t2=t2, xt=xt: e.scalar_tensor_tensor(
                    out=t2[:, :tn], in0=xt[:, dc, :tn], scalar=g.modv[:, 0, dc, wh:wh + 1], in1=rstd[:, :tn],
                    op0=ALU.mult, op1=ALU.mult), r=[xt, rstd, g.modv], w=[t2])
                k.op("act", lambda e, dc=dc, t2=t2, h=h: e.activation(
                    out=h[:, dc, :tn], in_=t2[:, :tn], func=AF.Identity, bias=g.modv[:, 1, dc, wh:wh + 1]),
                    r=[t2, g.modv], w=[h])
            for n in range(DIN // 128):
                ps = g.ps[2 + (zi % 4)]
                zs = zsb[zi % 4]
                for dc in range(8):
                    k.op("pe", lambda e, n=n, dc=dc, ps=ps, h=h: e.matmul(
                        ps[:, :tn], lhsT=win[:, dc, n * 128:(n + 1) * 128], rhs=h[:, dc, :tn],
                        start=(dc == 0), stop=(dc == 7)), r=[win, h], w=[ps])
                if zi % 2 == 0:
                    k.op("act", lambda e, ps=ps, zs=zs: e.activation(out=zs[:, :tn], in_=ps[:, :tn], func=AF.Copy),
                         r=[ps], w=[zs])
                else:
                    k.op("dve", lambda e, ps=ps, zs=zs: e.tensor_copy(out=zs[:, :tn], in_=ps[:, :tn]), r=[ps], w=[zs])
                k.dma("sp", g.zT[n * 128:(n + 1) * 128, c0:c0 + tn], zs[:, :tn], r=[zs], w=[g.zT_r])
                zi += 1
        k.epoch()


def phase_conv(g, l):
    nc, k = g.nc, g.k
    with ExitStack() as ph:
        sb = lambda n, s, d: T(ph.enter_context(nc.sbuf_tensor(uname(n), s, d)))
        for cc in range(2):
            u = sb("cu%d" % cc, [128, TT], F32)
            bg = sb("cb%d" % cc, [128, TT], F32)
            cg = sb("cc%d" % cc, [128, TT], F32)
            y = sb("cy%d" % cc, [128, TT], F32)
            yo = sb("cyo%d" % cc, [128, TT], BF16)
            cw = sb("cw%d" % cc, [128, 3], F32)
            k.dma("sp", cw[:], g.i["conv_w"][l, cc], w=[cw])
            k.dma("sp", u[:], g.zT[O_U + cc * 128:O_U + (cc + 1) * 128, :], r=[g.zT_r], w=[u])
            k.dma("sp", bg[:], g.zT[O_B + cc * 128:O_B + (cc + 1) * 128, :], r=[g.zT_r], w=[bg])
            k.dma("sp", cg[:], g.zT[O_C + cc * 128:O_C + (cc + 1) * 128, :], r=[g.zT_r], w=[cg])
            k.op("dve", lambda e: e.tensor_tensor(out=u[:], in0=u[:], in1=cg[:], op=ALU.mult), r=[cg, u], w=[u])
            k.op("dve", lambda e: e.tensor_scalar(out=y[:], in0=u[:], scalar1=cw[:, 1:2], scalar2=None, op0=ALU.mult),
                 r=[u, cw], w=[y])
            yl = y[:, CTX:].rearrange("p (r c) -> p r c", c=64)
            ul = u[:, CTX:].rearrange("p (r c) -> p r c", c=64)
            for (o_, i_, wi) in ((y[:, 1:CTX], u[:, 0:CTX - 1], 0), (y[:, 0:CTX - 1], u[:, 1:CTX], 2),
                                 (yl[:, :, 1:64], ul[:, :, 0:63], 0), (yl[:, :, 0:63], ul[:, :, 1:64], 2)):
                k.op("dve", lambda e, o_=o_, i_=i_, wi=wi: e.scalar_tensor_tensor(
                    out=o_, in0=i_, scalar=cw[:, wi:wi + 1], in1=o_, op0=ALU.mult, op1=ALU.add), r=[u, cw, y], w=[y])
            k.op("dve", lambda e: e.tensor_tensor(out=yo[:], in0=y[:], in1=bg[:], op=ALU.mult), r=[y, bg], w=[yo])
            k.dma("sp", g.ymix[512 + cc * 128:512 + (cc + 1) * 128, :], yo[:], r=[yo], w=[g.ymix_r])
        k.epoch()


def phase_fourier(g, l):
    nc, k = g.nc, g.k
    with ExitStack() as ph:
        sb = lambda n, s, d: T(ph.enter_context(nc.sbuf_tensor(uname(n), s, d)))
        bd = sb("bd", [128, 2, 128], BF16)
        k.dma("sp", bd[:], g.i["bd64"].rearrange("a p n -> p a n"), w=[bd])
        fst = sb("fst", [128, SEQ], F32)
        fb = sb("fb", [128, 2, SEQ], BF16)
        G = sb("G", [128, 32, 512], BF16)
        cb = [sb("dc%d" % i, [128, 32, 256], BF16) for i in range(2)]
        sbf = [sb("ds%d" % i, [128, 32, 256], BF16) for i in range(2)]
        yo = [sb("fyo%d" % i, [128, 256], BF16) for i in range(2)]
        it = 0
        for (c0, L, csrc) in ((0, CTX, g.i["dftC"]), (CTX, SEQ, g.i["dftL"])):
            if l == 1 and c0 == 0:
                continue
            ntt = L // 128
            for cc in range(2):
                k.dma("sp", fst[:, :L], g.zT[O_F + cc * 128:O_F + (cc + 1) * 128, c0:c0 + L], r=[g.zT_r], w=[fst])
                k.op("act", lambda e, cc=cc: e.activation(out=fb[:, cc, :L], in_=fst[:, :L], func=AF.Copy), r=[fst], w=[fb])
            for tt in range(ntt):
                ps = g.ps[tt % 2]
                for cs in range(2):
                    for cc in range(2):
                        k.op("pe", lambda e, ps=ps, cs=cs, cc=cc, tt=tt: e.matmul(
                            ps[:, (cs * 2 + cc) * 128:(cs * 2 + cc + 1) * 128], lhsT=fb[:, cc, tt * 128:(tt + 1) * 128],
                            rhs=bd[:, cs, :], start=True, stop=True), r=[fb, bd], w=[ps])
                if tt % 2 == 0:
                    k.op("act", lambda e, ps=ps, tt=tt: e.activation(out=G[:, tt, :], in_=ps[:, :], func=AF.Copy), r=[ps], w=[G])
                else:
                    k.op("dve", lambda e, ps=ps, tt=tt: e.tensor_copy(out=G[:, tt, :], in_=ps[:, :]), r=[ps], w=[G])
            for tp in range(L // 256):
                cbt, sbt = cb[tp % 2], sbf[tp % 2]
                k.dma("sp", cbt[:, :ntt, :], csrc[0, tp].rearrange("p (tt n) -> p tt n", n=256), w=[cbt])
                k.dma("sp", sbt[:, :ntt, :], csrc[1, tp].rearrange("p (tt n) -> p tt n", n=256), w=[sbt])
                for cc in range(2):
                    ps = g.ps[2 + it % 2]
                    y_ = yo[it % 2]
                    for tt in range(ntt):
                        k.op("pe", lambda e, ps=ps, cc=cc, tt=tt, cbt=cbt: e.matmul(
                            ps[:, :256], lhsT=G[:, tt, cc * 128:(cc + 1) * 128], rhs=cbt[:, tt, :],
                            start=(tt == 0), stop=False), r=[G, cbt], w=[ps])
                        k.op("pe", lambda e, ps=ps, cc=cc, tt=tt, sbt=sbt: e.matmul(
                            ps[:, :256], lhsT=G[:, tt, (2 + cc) * 128:(3 + cc) * 128], rhs=sbt[:, tt, :],
                            start=False, stop=(tt == ntt - 1)), r=[G, sbt], w=[ps])
                    k.op("act", lambda e, ps=ps, y_=y_: e.activation(out=y_[:], in_=ps[:, :256], func=AF.Copy), r=[ps], w=[y_])
                    k.dma("sp", g.ymix[768 + cc * 128:768 + (cc + 1) * 128, c0 + tp * 256:c0 + (tp + 1) * 256], y_[:],
                          r=[y_], w=[g.ymix_r])
                    it += 1
        k.epoch()


SEG = 512
NCH = TT // 64
DEC = -0.6065306597126334


def rwkv_segments(d):
    segs = [(0, CTX)] + [(CTX + i * SEG, SEG) for i in range(SEQ // SEG)]
    if d == 1:
        segs = [segs[0]] + segs[:0:-1]
    return segs


def phase_rwkv(g, l, last):
    nc, k = g.nc, g.k
    with ExitStack() as ph:
        sb = lambda n, s, d: T(ph.enter_context(nc.sbuf_tensor(uname(n), s, d)))
        par = sb("rpar", [64, 8, 2, 8], F32)
        omka = sb("omka", [64, 8], F32)
        lmu = sb("lmu", [64, 2, 2], F32)
        lora = sb("lora", [64, 2, 2, 512], F32)
        g2 = sb("g2w", [128, 512], F32)
        gn = sb("gnw", [64, 2, 512], F32)
        mask = sb("mask", [64, 2, 320], F32)
        ident = sb("ident", [64, 64], F32)
        ones64 = sb("ones64", [64, 64], F32)
        rmask = sb("rmask", [64, SEG], F32)
        sgl = sb("sgl", [128, TT], F32)
        k.dma("sp", par[:], g.i["rw_par"][l], w=[par])
        k.dma("sp", lmu[:], g.i["rw_lmu"][l], w=[lmu])
        k.dma("sp", lora[:], g.i["rw_lora"][l], w=[lora])
        k.dma("sp", g2[:], g.i["rw_g2"][l], w=[g2])
        k.dma("sp", gn[:], g.i["rw_gn"][l], w=[gn])
        k.dma("sp", mask[:], g.i["masks"], w=[mask])
        k.dma("sp", ident[:], g.i["ident"][0:64, 0:64], w=[ident])
        k.dma("sp", rmask[:], g.i["rmask"], w=[rmask])
        k.op("pool", lambda e: e.memset(ones64[:], 1.0), w=[ones64])
        k.op("dve", lambda e: e.tensor_scalar(out=omka[:], in0=par[:, :, 0, 6], scalar1=-1.0, scalar2=1.0,
                                              op0=ALU.mult, op1=ALU.add), r=[par], w=[omka])
        k.dma("sp", sgl[:], g.zT[O_G:O_G + 128, :], r=[g.zT_r], w=[sgl])
        k.op("act", lambda e: e.activation(out=sgl[:], in_=sgl[:], func=AF.Sigmoid), r=[sgl], w=[sgl])
        hz = {n: sb("hz_" + n, [64, SEG + 2], F32) for n in ("k", "v", "r", "w", "a")}
        wt0 = {n: sb("wt_" + n, [64, SEG], F32) for n in
               ("kl", "vl", "rl", "wl", "al", "sw", "sa", "kk", "t0", "t1", "kkn", "km", "bv", "Lp", "Ex", "En",
                "BT", "KT", "rk")}
        wt = wt0
        NC8 = SEG // 64
        Eis = [sb("Ei%d" % i, [64, SEG], F32) for i in range(3)]
        ARs = [sb("AR%d" % i, [64, NC8 * 128], F32) for i in range(3)]
        BTs = [wt0["BT"], sb("BTb", [64, SEG], F32)]
        KTs = [wt0["KT"], sb("KTb", [64, SEG], F32)]
        VLs = [wt0["vl"], sb("vlb", [64, SEG], F32)]
        RKs = [wt0["rk"], sb("rkb", [64, SEG], F32)]
        T3s = [sb("T3_%d" % i, [64, NC8, 4, 64], F32) for i in range(2)]
        ABKs = [sb("ABK%d" % i, [64, NC8, 256], F32) for i in range(2)]
        Wbs = [sb("Wb%d" % i, [64, NC8, 64], F32) for i in range(2)]
        N0 = sb("N0", [64, NC8, 64], F32)
        MNb = [sb("MNb%d" % i, [64, NC8, 128], F32) for i in range(2)]
        X = sb("Xs", [64, 64], F32)
        U = sb("Us", [64, 64], F32)
        Hs = [sb("Hs%d" % i, [64, 64], F32) for i in range(2)]
        Yd = sb("Yd", [64, NCH, 64], F32)
        Ysum = sb("Ysum", [64, NCH, 64], F32)
        st1 = sb("st1", [64, SEG // 64], F32)
        st2 = sb("st2", [64, SEG // 64], F32)
        ysq = sb("ysq", [64, SEG // 64, 64], F32)
        yfm = sb("yfm", [64, TT], BF16)
        pL = [g.ps[0], g.ps[1]]
        pN = g.ps[2]
        pB = [g.ps[3], g.ps[4]]
        pC = g.ps[5]
        pCh = g.ps[6]
        pH = g.ps[7]
        RX = RU = RH = RY = pCh
        RG = pN
        hi_ = 0

        def lerp(dst, src, S, d, mu_ap):
            cur = src[:, 1:S + 1]
            sh = src[:, 0:S] if d == 0 else src[:, 2:S + 2]
            k.op("pool", lambda e: e.tensor_tensor(out=wt["t0"][:, :S], in0=sh, in1=cur, op=ALU.subtract),
                 r=[src], w=[wt["t0"]])
            k.op("dve", lambda e: e.scalar_tensor_tensor(out=dst[:, :S], in0=wt["t0"][:, :S], scalar=mu_ap, in1=cur,
                                                         op0=ALU.mult, op1=ALU.add), r=[wt["t0"], src, lmu, par], w=[dst])

        tot = sb("tot", [64, SEG // 64], F32)
        st = {"hcur": 0}

        def stageB1(h, d, c0, S, b2, b3):
            wt = dict(wt0)
            wt.update(BT=BTs[b2], KT=KTs[b2], vl=VLs[b2], rk=RKs[b2])
            wt["Ei"] = Eis[b3]
            AR = ARs[b3]
            lo, hi = (0, CTX) if c0 < CTX else (CTX, TT)
            need_y = not (last and c0 < CTX)
            nch = S // 64
            rows = {"k": O_K + h * 64, "v": O_V + h * 64, "r": O_R + h * 64,
                    "w": (O_WF, O_WB)[d], "a": (O_AF, O_AB)[d]}
            for n_, t_ in hz.items():
                k.op("pool", lambda e, t_=t_: e.memset(t_[:], 0.0), w=[t_])
                a_, b_ = max(c0 - 1, lo), min(c0 + S + 1, hi)
                k.dma("sp", t_[:, a_ - (c0 - 1):b_ - (c0 - 1)], g.zT[rows[n_]:rows[n_] + 64, a_:b_],
                      r=[g.zT_r], w=[t_])
            P = lambda j: par[:, h, d, j:j + 1]
            lerp(wt["kl"], hz["k"], S, d, P(0))
            lerp(wt["vl"], hz["v"], S, d, P(1))
            lerp(wt["rl"], hz["r"], S, d, P(2))
            lerp(wt["wl"], hz["w"], S, d, lmu[:, d, 0:1])
            lerp(wt["al"], hz["a"], S, d, lmu[:, d, 1:2])
            k.op("act", lambda e: e.activation(out=wt["wl"][:, :S], in_=wt["wl"][:, :S], func=AF.Tanh),
                 r=[wt["wl"]], w=[wt["wl"]])
            for c5 in range(0, S, 512):
                n5 = min(512, S - c5)
                for (src, dst, li, bj) in ((wt["wl"], wt["sw"], 0, 3), (wt["al"], wt["sa"], 1, 4)):
                    k.op("pe", lambda e, src=src, li=li, c5=c5, n5=n5: e.matmul(
                        pH[0:64, :n5], lhsT=lora[:, d, li, h * 64:(h + 1) * 64], rhs=src[:, c5:c5 + n5],
                        start=True, stop=True), r=[lora, src], w=[pH])
                    k.op("act", lambda e, dst=dst, bj=bj, c5=c5, n5=n5: e.activation(
                        out=dst[:, c5:c5 + n5], in_=pH[0:64, :n5], func=AF.Sigmoid, bias=P(bj)), r=[pH, par], w=[dst])
            k.op("dve", lambda e: e.tensor_scalar(out=wt["kk"][:, :S], in0=wt["kl"][:, :S], scalar1=P(5), scalar2=None,
                                                  op0=ALU.mult), r=[wt["kl"], par], w=[wt["kk"]])
            k.op("pool", lambda e: e.tensor_tensor(out=wt["t0"][:, :S], in0=wt["kk"][:, :S], in1=wt["kk"][:, :S],
                                                   op=ALU.mult), r=[wt["kk"]], w=[wt["t0"]])
            for c5 in range(0, S, 512):
                n5 = min(512, S - c5)
                k.op("pe", lambda e, c5=c5, n5=n5: e.matmul(pH[0:64, :n5], lhsT=ones64[:], rhs=wt["t0"][:, c5:c5 + n5],
                                                           start=True, stop=True), r=[ones64, wt["t0"]], w=[pH])
                k.op("act", lambda e, c5=c5, n5=n5: e.activation(out=wt["t1"][:, c5:c5 + n5], in_=pH[0:64, :n5],
                                                                 func=AF.Sqrt), r=[pH], w=[wt["t1"]])
            k.op("dve", lambda e: e.tensor_scalar(out=wt["t1"][:, :S], in0=wt["t1"][:, :S], scalar1=1e-12, scalar2=None,
                                                  op0=ALU.max), r=[wt["t1"]], w=[wt["t1"]])
            k.op("dve", lambda e: e.reciprocal(out=wt["t1"][:, :S], in_=wt["t1"][:, :S]), r=[wt["t1"]], w=[wt["t1"]])
            k.op("pool", lambda e: e.tensor_tensor(out=wt["kkn"][:, :S], in0=wt["kk"][:, :S], in1=wt["t1"][:, :S],
                                                   op=ALU.mult), r=[wt["kk"], wt["t1"]], w=[wt["kkn"]])
            k.op("dve", lambda e: e.tensor_scalar(out=wt["t1"][:, :S], in0=wt["sa"][:, :S], scalar1=P(6),
                                                  scalar2=omka[:, h:h + 1], op0=ALU.mult, op1=ALU.add),
                 r=[wt["sa"], par, omka], w=[wt["t1"]])
            k.op("pool", lambda e: e.tensor_tensor(out=wt["km"][:, :S], in0=wt["kl"][:, :S], in1=wt["t1"][:, :S],
                                                   op=ALU.mult), r=[wt["kl"], wt["t1"]], w=[wt["km"]])
            k.op("pool", lambda e: e.tensor_tensor(out=wt["bv"][:, :S], in0=wt["kkn"][:, :S], in1=wt["sa"][:, :S],
                                                   op=ALU.mult), r=[wt["kkn"], wt["sa"]], w=[wt["bv"]])
            k.op("dve", lambda e: e.tensor_tensor_scan(out=wt["Lp"][:, :S], data0=rmask[:, :S], data1=wt["sw"][:, :S],
                                                       initial=0.0, op0=ALU.mult, op1=ALU.add),
                 r=[rmask, wt["sw"]], w=[wt["Lp"]])
            if d == 1:
                lp3 = wt["Lp"][:, :S].rearrange("p (c t) -> p c t", t=64)
                sw3 = wt["sw"][:, :S].rearrange("p (c t) -> p c t", t=64)
                t03 = wt["t0"][:, :S].rearrange("p (c t) -> p c t", t=64)
                k.op("pool", lambda e: e.tensor_tensor(out=wt["t0"][:, :S], in0=wt["sw"][:, :S], in1=wt["Lp"][:, :S],
                                                       op=ALU.subtract), r=[wt["sw"], wt["Lp"]], w=[wt["t0"]])
                k.op("dve", lambda e: e.tensor_copy(out=tot[:, :nch], in_=wt["Lp"][:, :S].rearrange("p (c t) -> p c t", t=64)[:, :, 63]),
                     r=[wt["Lp"]], w=[tot])
                k.op("dve", lambda e: e.tensor_tensor(out=lp3, in0=t03, in1=tot[:, :nch].unsqueeze(2).to_broadcast([64, nch, 64]),
                                                      op=ALU.add), r=[wt["t0"], tot], w=[wt["Lp"]])
            k.op("act", lambda e: e.activation(out=wt["Ei"][:, :S], in_=wt["Lp"][:, :S], func=AF.Exp, scale=DEC),
                 r=[wt["Lp"]], w=[wt["Ei"]])
            k.op("act", lambda e: e.activation(out=wt["En"][:, :S], in_=wt["Lp"][:, :S], func=AF.Exp, scale=-DEC),
                 r=[wt["Lp"]], w=[wt["En"]])
            k.op("pool", lambda e: e.tensor_tensor(out=wt["t0"][:, :S], in0=wt["Lp"][:, :S], in1=wt["sw"][:, :S],
                                                   op=ALU.subtract), r=[wt["sw"], wt["Lp"]], w=[wt["t0"]])
            k.op("act", lambda e: e.activation(out=wt["Ex"][:, :S], in_=wt["t0"][:, :S], func=AF.Exp, scale=DEC),
                 r=[wt["t0"]], w=[wt["Ex"]])
            ar4 = AR[:, :nch * 128].rearrange("p (c j t) -> p c j t", j=2, t=64)
            v3 = lambda t_: t_[:, :S].rearrange("p (c t) -> p c t", t=64)
            k.op("dve", lambda e: e.scalar_tensor_tensor(out=ar4[:, :, 0, :], in0=v3(wt["kkn"]), scalar=-1.0,
                                                         in1=v3(wt["Ex"]), op0=ALU.mult, op1=ALU.mult),
                 r=[wt["kkn"], wt["Ex"]], w=[AR])
            k.op("pool", lambda e: e.tensor_tensor(out=ar4[:, :, 1, :], in0=v3(wt["rl"]), in1=v3(wt["Ei"]), op=ALU.mult),
                 r=[wt["rl"], wt["Ei"]], w=[AR])
            k.op("dve", lambda e: e.tensor_tensor(out=wt["BT"][:, :S], in0=wt["bv"][:, :S], in1=wt["En"][:, :S], op=ALU.mult),
                 r=[wt["bv"], wt["En"]], w=[wt["BT"]])
            k.op("pool", lambda e: e.tensor_tensor(out=wt["KT"][:, :S], in0=wt["km"][:, :S], in1=wt["En"][:, :S], op=ALU.mult),
                 r=[wt["km"], wt["En"]], w=[wt["KT"]])
            if need_y:
                k.op("dve", lambda e: e.scalar_tensor_tensor(out=wt["rk"][:, :S], in0=wt["rl"][:, :S], scalar=P(7),
                                                             in1=wt["km"][:, :S], op0=ALU.mult, op1=ALU.mult),
                     r=[wt["rl"], wt["km"], par], w=[wt["rk"]])

        def stageB2(h, d, c0, S, b2, b3, buf):
            wt = dict(wt0)
            wt.update(BT=BTs[b2], KT=KTs[b2], vl=VLs[b2], rk=RKs[b2])
            wt["Ei"] = Eis[b3]
            AR = ARs[b3]
            T3, ABK, Wb = T3s[buf], ABKs[buf], Wbs[buf]
            need_y = not (last and c0 < CTX)
            nch = S // 64
            for c in range(nch if g.rw_stage >= 1 else 0):
                cs = slice(c * 64, (c + 1) * 64)
                for j, src in enumerate((wt["BT"], wt["KT"], wt["vl"])):
                    k.op("pe", lambda e, j=j, src=src, cs=cs: e.matmul(pN[0:64, j * 64:(j + 1) * 64], lhsT=src[:, cs], rhs=ident[:], start=True, stop=True),
                         r=[src, ident], w=[RG])
                if need_y:
                    k.op("pe", lambda e, cs=cs: e.matmul(pN[0:64, 192:256], lhsT=wt["rk"][:, cs], rhs=ones64[:],
                                                         start=True, stop=True), r=[wt["rk"], ones64], w=[RG])
                nj = 4 if need_y else 3
                k.op("dve", lambda e, c=c, nj=nj: e.tensor_copy(out=T3[:, c, 0:nj, :], in_=pN[0:64, 0:nj * 64].rearrange("p (j t) -> p j t", j=nj)),
                     r=[RG], w=[T3])
            if g.rw_stage >= 2:
                ar_of = lambda c: (AR[:, c * 128:c * 128 + 64], AR[:, c * 128 + 64:c * 128 + 128], AR[:, c * 128:c * 128 + 128])
                for h0 in range(0, nch, 4):
                    for c in range(h0, min(h0 + 4, nch)):
                        cs = slice(c * 64, (c + 1) * 64)
                        aT, rT, arT = ar_of(c)
                        pl = pL[(c % 4) // 2]
                        o0 = (c % 2) * 256
                        k.op("pe", lambda e, pl=pl, o0=o0, cs=cs, arT=arT: e.matmul(pl[0:64, o0:o0 + 128], lhsT=wt["BT"][:, cs], rhs=arT, start=True, stop=True),
                             r=[wt["BT"], AR], w=[pl])
                        k.op("pe", lambda e, pl=pl, o0=o0, cs=cs, arT=arT: e.matmul(pl[0:64, o0 + 128:o0 + 256], lhsT=wt["KT"][:, cs], rhs=arT, start=True, stop=True),
                             r=[wt["KT"], AR], w=[pl])
                        k.op("pe", lambda e, c=c, cs=cs, aT=aT: e.matmul(pN[0:64, c * 64:(c + 1) * 64], lhsT=aT, rhs=wt["BT"][:, cs], start=True, stop=True),
                             r=[wt["BT"], AR], w=[pN])
                    for j2 in range(2):
                        cA = h0 + j2 * 2
                        if cA >= nch:
                            continue
                        pl = pL[j2]
                        k.op("dve", lambda e, pl=pl, cA=cA: e.tensor_tensor(
                            out=ABK[:, cA:cA + 2, :], in0=pl[0:64, 0:512].rearrange("p (c x) -> p c x", c=2),
                            in1=mask[:, d, 0:256].unsqueeze(1).to_broadcast([64, 2, 256]), op=ALU.mult), r=[pl, mask], w=[ABK])
                k.op("dve", lambda e: e.tensor_tensor(out=N0[:, :nch, :], in0=pN[0:64, 0:nch * 64].rearrange("p (c x) -> p c x", x=64),
                                                      in1=mask[:, d, 256:320].unsqueeze(1).to_broadcast([64, nch, 64]), op=ALU.mult),
                     r=[pN, mask], w=[N0])
                k.op("pool", lambda e: e.tensor_tensor(out=Wb[:, :nch, :], in0=ABK[:, :nch, 0:64],
                                                       in1=ident[:].unsqueeze(1).to_broadcast([64, nch, 64]), op=ALU.add),
                     r=[ABK, ident], w=[Wb])
                for j in range(5):
                    lastj = j == 4
                    mn = MNb[j % 2]
                    for c in range(nch):
                        if j == 0:
                            Mj, Nj, Mr, Nr = ABK[:, c, 0:64], N0[:, c, :], ABK, N0
                        else:
                            pm = MNb[(j - 1) % 2]
                            Mj, Nj, Mr, Nr = pm[:, c, 0:64], pm[:, c, 64:128], pm, pm
                        pb = pB[c // 4]
                        o0 = (c % 4) * 128
                        if not lastj:
                            k.op("pe", lambda e, Mj=Mj, Nj=Nj, pb=pb, o0=o0: e.matmul(pb[0:64, o0:o0 + 64], lhsT=Nj, rhs=Mj, start=True, stop=True),
                                 r=[Mr, Nr], w=[pb])
                        k.op("pe", lambda e, Mj=Mj, Nj=Nj, pb=pb, o0=o0: e.matmul(pb[0:64, o0 + 64:o0 + 128], lhsT=Mj, rhs=Nj, start=True, stop=True),
                             r=[Mr, Nr], w=[pb])
                    for b4 in range((nch + 3) // 4):
                        n4 = min(4, nch - b4 * 4)
                        k.op("act", lambda e, b4=b4, n4=n4, mn=mn: e.activation(
                            out=mn[:, b4 * 4:b4 * 4 + n4, :], in_=pB[b4][0:64, 0:n4 * 128].rearrange("p (c x) -> p c x", x=128), func=AF.Copy),
                            r=[pB[b4]], w=[mn])
                    for c in range(nch):
                        k.op("pe", lambda e, c=c, mn=mn: e.matmul(pC[0:64, c * 64:(c + 1) * 64], lhsT=mn[:, c, 64:128], rhs=Wb[:, c, :], start=True, stop=True),
                             r=[mn, Wb], w=[pC])
                    wo_ = Wb
                    k.op("dve", lambda e, wo_=wo_: e.tensor_tensor(out=wo_[:, :nch, :], in0=pC[0:64, 0:nch * 64].rearrange("p (c x) -> p c x", x=64),
                                                                   in1=Wb[:, :nch, :], op=ALU.add), r=[pC, Wb], w=[wo_])

        def stageA(h, d, c0, S, buf, b3, first, last_ser, last_head):
            AR, T3, ABK, Wb = ARs[b3], T3s[buf], ABKs[buf], Wbs[buf]
            wt = dict(wt0)
            wt["Ei"] = Eis[b3]
            need_y = not (last and c0 < CTX)
            nch = S // 64
            ar_of = lambda c: (AR[:, c * 128:c * 128 + 64], AR[:, c * 128 + 64:c * 128 + 128], AR[:, c * 128:c * 128 + 128])
            if first:
                k.op("pool", lambda e: e.memset(Hs[0][:], 0.0), w=[Hs[0]])
                st["hcur"] = 0
            hcur = st["hcur"]
            if g.rw_stage >= 2:
                pass
            corder = range(nch) if d == 0 else range(nch - 1, -1, -1)
            for c in corder:
                aT, rT, arT = ar_of(c)
                Hn = Hs[1 - hcur]
                H = Hs[hcur]
                gc = c0 // 64 + c
                k.op("pe", lambda e, H=H, aT=aT: e.matmul(pCh[0:64, 0:64], lhsT=aT, rhs=H[:], start=True, stop=False),
                     r=[AR, H], w=[RX])
                k.op("pe", lambda e, c=c: e.matmul(pCh[0:64, 0:64], lhsT=ABK[:, c, 128:192], rhs=T3[:, c, 2, :], start=False, stop=True),
                     r=[ABK, T3], w=[RX])
                k.op("act", lambda e: e.activation(out=X[:], in_=pCh[0:64, 0:64], func=AF.Copy), r=[RX], w=[X])
                k.op("pe", lambda e, c=c: e.matmul(pCh[0:64, 64:128], lhsT=Wb[:, c, :], rhs=X[:], start=True, stop=True),
                     r=[Wb, X], w=[RU])
                k.op("dve", lambda e: e.tensor_copy(out=U[:], in_=pCh[0:64, 64:128]), r=[RU], w=[U])
                k.op("pe", lambda e, H=H: e.matmul(pCh[0:64, 192:256], lhsT=ident[:], rhs=H[:], start=True, stop=False),
                     r=[ident, H], w=[RH])
                k.op("pe", lambda e, c=c: e.matmul(pCh[0:64, 192:256], lhsT=T3[:, c, 0, :], rhs=U[:], start=False, stop=False),
                     r=[T3, U], w=[RH])
                k.op("pe", lambda e, c=c: e.matmul(pCh[0:64, 192:256], lhsT=T3[:, c, 1, :], rhs=T3[:, c, 2, :], start=False, stop=True),
                     r=[T3], w=[RH])
                pcol = c * 64 + (63 if d == 0 else 0)
                k.op("dve", lambda e, Hn=Hn, pcol=pcol: e.tensor_scalar(out=Hn[:], in0=pCh[0:64, 192:256],
                                                                         scalar1=wt["Ei"][:, pcol:pcol + 1], scalar2=None, op0=ALU.mult),
                     r=[RH, wt["Ei"]], w=[Hn])
                if need_y:
                    k.op("pe", lambda e, H=H, rT=rT: e.matmul(pCh[0:64, 128:192], lhsT=rT, rhs=H[:], start=True, stop=False),
                         r=[AR, H], w=[RY])
                    k.op("pe", lambda e, c=c: e.matmul(pCh[0:64, 128:192], lhsT=ABK[:, c, 64:128], rhs=U[:], start=False, stop=False),
                         r=[ABK, U], w=[RY])
                    k.op("pe", lambda e, c=c: e.matmul(pCh[0:64, 128:192], lhsT=ABK[:, c, 192:256], rhs=T3[:, c, 2, :], start=False, stop=True),
                         r=[ABK, T3], w=[RY])
                    k.op("act", lambda e, gc=gc: e.activation(out=Yd[:, gc, :], in_=pCh[0:64, 128:192], func=AF.Copy),
                         r=[RY], w=[Yd])
                hcur = 1 - hcur
            st["hcur"] = hcur
            if need_y and g.rw_stage >= 3:
                g0 = c0 // 64
                yv = Yd[:, g0:g0 + nch, :]
                bc = lambda ap: ap.unsqueeze(2).to_broadcast([64, nch, 64])
                k.op("dve", lambda e: e.tensor_reduce(out=st1[:, :nch], in_=yv, axis=AX.X, op=ALU.add), r=[Yd], w=[st1])
                k.op("pool", lambda e: e.tensor_tensor(out=ysq[:, :nch, :], in0=yv, in1=yv, op=ALU.mult), r=[Yd], w=[ysq])
                k.op("dve", lambda e: e.tensor_reduce(out=st2[:, :nch], in_=ysq[:, :nch, :], axis=AX.X, op=ALU.add), r=[ysq], w=[st2])
                k.op("dve", lambda e: e.tensor_scalar(out=st1[:, :nch], in0=st1[:, :nch], scalar1=1.0 / 64, scalar2=None, op0=ALU.mult),
                     r=[st1], w=[st1])
                k.op("dve", lambda e: e.scalar_tensor_tensor(out=ysq[:, :nch, 0], in0=st1[:, :nch], scalar=-1.0, in1=st1[:, :nch],
                                                             op0=ALU.mult, op1=ALU.mult), r=[st1], w=[ysq])
                k.op("dve", lambda e: e.scalar_tensor_tensor(out=st2[:, :nch], in0=st2[:, :nch], scalar=1.0 / 64, in1=ysq[:, :nch, 0],
                                                             op0=ALU.mult, op1=ALU.add), r=[st2, ysq], w=[st2])
                k.op("act", lambda e: e.activation(out=st2[:, :nch], in_=st2[:, :nch], func=AF.Sqrt, bias=g.gneps_t[0:64, :]),
                     r=[st2, g.gneps_t], w=[st2])
                k.op("dve", lambda e: e.reciprocal(out=st2[:, :nch], in_=st2[:, :nch]), r=[st2], w=[st2])
                k.op("dve", lambda e: e.tensor_tensor(out=yv, in0=yv, in1=bc(st1[:, :nch]), op=ALU.subtract), r=[Yd, st1], w=[Yd])
                k.op("dve", lambda e: e.tensor_tensor(out=yv, in0=yv, in1=bc(st2[:, :nch]), op=ALU.mult), r=[Yd, st2], w=[Yd])
                gb = lambda j: gn[:, j, h * 64:(h + 1) * 64].unsqueeze(1).to_broadcast([64, nch, 64])
                k.op("pool", lambda e: e.tensor_tensor(out=yv, in0=yv, in1=gb(0), op=ALU.mult), r=[Yd, gn], w=[Yd])
                k.op("pool", lambda e: e.tensor_tensor(out=yv, in0=yv, in1=gb(1), op=ALU.add), r=[Yd, gn], w=[Yd])
                k.op("pool", lambda e: e.tensor_tensor(out=ysq[:, :nch, :], in0=T3[:, :nch, 2, :], in1=T3[:, :nch, 3, :], op=ALU.mult),
                     r=[T3], w=[ysq])
                ys = Ysum[:, g0:g0 + nch, :]
                if d == 0:
                    k.op("dve", lambda e: e.tensor_tensor(out=ys, in0=yv, in1=ysq[:, :nch, :], op=ALU.add), r=[Yd, ysq], w=[Ysum])
                else:
                    k.op("dve", lambda e: e.tensor_tensor(out=yv, in0=yv, in1=ysq[:, :nch, :], op=ALU.add), r=[Yd, ysq], w=[Yd])
                    k.op("pool", lambda e: e.tensor_tensor(out=ys, in0=ys, in1=yv, op=ALU.add), r=[Yd, Ysum], w=[Ysum])
            if last_ser and g.dbg_state is not None:
                k.dma("sp", g.dbg_state[l, d, h], Hs[hcur][:], r=[Hs[hcur]])
            if last_head:
                pass
            if last_head and g.rw_stage >= 4:
                for c8 in range(0, NCH, 8):
                    n8 = min(8, NCH - c8)
                    for c in range(n8):
                        gc = c8 + c
                        k.op("pe", lambda e, c=c, gc=gc: e.matmul(pCh[0:64, c * 64:(c + 1) * 64], lhsT=sgl[:, gc * 64:(gc + 1) * 64],
                                                                  rhs=g2[:, h * 64:(h + 1) * 64], start=True, stop=True),
                             r=[sgl, g2], w=[pCh])
                    ysl = Ysum[:, c8:c8 + n8, :]
                    k.op("dve", lambda e, ysl=ysl, n8=n8: e.tensor_tensor(out=ysl, in0=ysl, in1=pCh[0:64, 0:n8 * 64].rearrange("p (c t) -> p c t", t=64),
                                                                          op=ALU.mult), r=[Ysum, pCh], w=[Ysum])
                    for c in range(n8):
                        gc = c8 + c
                        k.op("pe", lambda e, c=c, gc=gc: e.matmul(pCh[0:64, c * 64:(c + 1) * 64], lhsT=Ysum[:, gc, :], rhs=ident[:], start=True, stop=True),
                             r=[Ysum, ident], w=[pCh])
                    k.op("act", lambda e, c8=c8, n8=n8: e.activation(out=yfm[:, c8 * 64:(c8 + n8) * 64], in_=pCh[0:64, 0:n8 * 64], func=AF.Copy),
                         r=[pCh], w=[yfm])
                k.dma("sp", g.ymix[h * 64:(h + 1) * 64, :], yfm[:], r=[yfm], w=[g.ymix_r])

        jobs = []
        for h in g.heads:
            for d in range(2):
                segs = rwkv_segments(d)[:g.rw_maxseg]
                for si, (c0, S) in enumerate(segs):
                    n_ = len(jobs)
                    jobs.append(dict(h=h, d=d, c0=c0, S=S, buf=n_ % 2, b3=n_ % 3, first=(si == 0),
                                     last_ser=(si == len(segs) - 1), last_head=(d == 1 and si == len(segs) - 1)))
        J = len(jobs)
        for r_ in range(J + 2):
            fns, quota = [], []
            ja = jobs[r_ - 2] if 0 <= r_ - 2 < J else None
            jb = jobs[r_ - 1] if 0 <= r_ - 1 < J else None
            jc = jobs[r_] if r_ < J else None
            if ja is not None:
                fns.append(lambda p_=ja: stageA(p_["h"], p_["d"], p_["c0"], p_["S"], p_["buf"], p_["b3"], p_["first"],
                                                 p_["last_ser"], p_["last_head"]))
                quota.append(2)
            if jb is not None:
                fns.append(lambda p_=jb: stageB2(p_["h"], p_["d"], p_["c0"], p_["S"], p_["buf"], p_["b3"], p_["buf"]))
                quota.append(3)
            if jc is not None:
                fns.append(lambda p_=jc: stageB1(p_["h"], p_["d"], p_["c0"], p_["S"], p_["buf"], p_["b3"]))
                quota.append(1)
            weave(k, fns, quota)
            if ja is not None and ja["last_head"]:
                k.epoch()


def phase_wout(g, l):
    nc, k = g.nc, g.k
    with ExitStack() as ph:
        sb = lambda n, s, d: T(ph.enter_context(nc.sbuf_tensor(uname(n), s, d)))
        wo = sb("wo", [128, 8, D], BF16)
        wsrc = g.i["w_out"][l].rearrange("(kc p) n -> p kc n", p=128)
        for kc in range(8):
            k.dma("pool", wo[:, kc, :], wsrc[:, kc, :], w=[wo])
        yms = [sb("ym%d" % i, [128, 8, 512], BF16) for i in range(2)]
        xts = [sb("fx%d" % i, [128, 8, 512], F32) for i in range(2)]
        o = sb("fo", [128, 8, 512], F32)
        sq = sb("fsq", [128, 8, 512], BF16)
        h2f = sb("h2f", [128, 8, 512], F32)
        h2b = sb("h2b", [128, 8, 512], BF16)
        rstd = sb("frstd", [128, 512], F32)
        tmp = sb("ftmp", [128, 512], F32)
        tmp2 = [sb("ftmp2_%d" % i, [128, 512], F32) for i in range(2)]
        xv = g.xres.rearrange("(dc p) t -> p dc t", p=128)
        ymv = g.ymix.rearrange("(dc p) t -> p dc t", p=128)
        h2v = g.h2T.rearrange("(dc p) t -> p dc t", p=128)
        h2fv = g.h2F.rearrange("(dc p) t -> p dc t", p=128)
        zi = 0
        for ti, (c0, tn, wh) in enumerate(token_tiles()):
            if l == 1 and wh == 1:
                continue
            ym, xt = yms[ti % 2], xts[ti % 2]
            k.dma("sp", ym[:, :, :tn], ymv[:, :, c0:c0 + tn], r=[g.ymix_r], w=[ym])
            k.dma("sp", xt[:, :, :tn], xv[:, :, c0:c0 + tn], r=[g.xres_r], w=[xt])
            for dc in range(8):
                ps = g.ps[2 + (zi % 4)]
                zi += 1
                for kc in range(8):
                    k.op("pe", lambda e, ps=ps, kc=kc, dc=dc, ym=ym: e.matmul(
                        ps[:, :tn], lhsT=wo[:, kc, dc * 128:(dc + 1) * 128], rhs=ym[:, kc, :tn],
                        start=(kc == 0), stop=(kc == 7)), r=[wo, ym], w=[ps])
                if dc % 2 == 0:
                    k.op("act", lambda e, ps=ps, dc=dc: e.activation(out=o[:, dc, :tn], in_=ps[:, :tn], func=AF.Copy), r=[ps], w=[o])
                else:
                    k.op("dve", lambda e, ps=ps, dc=dc: e.tensor_copy(out=o[:, dc, :tn], in_=ps[:, :tn]), r=[ps], w=[o])
            rms_rstd(g, o, tn, sq, g.ps[1], rstd, tmp)
            for dc in range(8):
                t2 = tmp2[dc % 2]
                k.op("dve", lambda e, dc=dc, t2=t2: e.scalar_tensor_tensor(
                    out=t2[:, :tn], in0=o[:, dc, :tn], scalar=g.modv[:, 2, dc, wh:wh + 1], in1=rstd[:, :tn],
                    op0=ALU.mult, op1=ALU.mult), r=[o, rstd, g.modv], w=[t2])
                k.op("pool", lambda e, dc=dc, t2=t2, xt=xt: e.tensor_tensor(out=xt[:, dc, :tn], in0=xt[:, dc, :tn], in1=t2[:, :tn],
                                                                           op=ALU.add), r=[t2, xt], w=[xt])
            k.dma("sp", xv[:, :, c0:c0 + tn], xt[:, :, :tn], r=[xt], w=[g.xres_r])
            rms_rstd(g, xt, tn, sq, g.ps[1], rstd, tmp)
            for dc in range(8):
                t2 = tmp2[dc % 2]
                k.op("dve", lambda e, dc=dc, t2=t2, xt=xt: e.scalar_tensor_tensor(
                    out=t2[:, :tn], in0=xt[:, dc, :tn], scalar=g.modv[:, 3, dc, wh:wh + 1], in1=rstd[:, :tn],
                    op0=ALU.mult, op1=ALU.mult), r=[xt, rstd, g.modv], w=[t2])
                k.op("act", lambda e, dc=dc, t2=t2: e.activation(
                    out=h2f[:, dc, :tn], in_=t2[:, :tn], func=AF.Identity, bias=g.modv[:, 4, dc, wh:wh + 1]),
                    r=[t2, g.modv], w=[h2f])
            k.op("pool", lambda e: e.tensor_copy(out=h2b[:, :, :tn], in_=h2f[:, :, :tn]), r=[h2f], w=[h2b])
            k.dma("sp", h2v[:, :, c0:c0 + tn], h2b[:, :, :tn], r=[h2b], w=[g.h2T_r])
            if l == 1:
                k.dma("sp", h2fv[:, :, c0:c0 + tn], h2f[:, :, :tn], r=[h2f], w=[g.h2F_r])
        k.epoch()


def phase_router(g, l):
    nc, k = g.nc, g.k
    with ExitStack() as ph:
        sb = lambda n, s, d: T(ph.enter_context(nc.sbuf_tensor(uname(n), s, d)))
        rw = sb("rw", [128, 8, 8], F32)
        rb = sb("rb", [128, 4, 8], F32)
        idn = sb("idn", [128, 128], F32)
        k.dma("sp", rw[:], g.i["router_w"], w=[rw])
        k.dma("sp", rb[:], g.i["router_b"], w=[rb])
        k.dma("sp", idn[:], g.i["ident"], w=[idn])
        hf = [sb("rhf%d" % i, [128, 8, 512], F32) for i in range(2)]
        lg = sb("lg", [128, 4, 8], F32)
        lg2 = sb("lg2", [128, 4, 8], F32)
        eq = sb("eq", [128, 4, 8], F32)
        m1 = sb("m1", [128, 4], F32)
        m2 = sb("m2", [128, 4], F32)
        gT = sb("gTs", [8, 512], F32)
        h2fv = g.h2F.rearrange("(dc p) t -> p dc t", p=128)
        pX, pY = g.ps[0], g.ps[1]
        bc = lambda ap: ap.unsqueeze(2).to_broadcast([128, 4, 8])
        for ti in range(SEQ // 512):
            c0 = CTX + ti * 512
            h = hf[ti % 2]
            k.dma("sp", h[:], h2fv[:, :, c0:c0 + 512], r=[g.h2F_r], w=[h])
            for ch in range(4):
                for dc in range(8):
                    k.op("pe", lambda e, ch=ch, dc=dc, h=h: e.matmul(pX[:, ch * 8:(ch + 1) * 8], lhsT=h[:, dc, ch * 128:(ch + 1) * 128],
                                                                   rhs=rw[:, dc, :], start=(dc == 0), stop=(dc == 7)), r=[h, rw], w=[pX])
            k.op("dve", lambda e: e.tensor_tensor(out=lg[:], in0=pX[:, 0:32].rearrange("p (c e) -> p c e", e=8), in1=rb[:], op=ALU.add),
                 r=[pX, rb], w=[lg])
            k.op("dve", lambda e: e.tensor_reduce(out=m1[:], in_=lg[:], axis=AX.X, op=ALU.max), r=[lg], w=[m1])
            k.op("dve", lambda e: e.tensor_tensor(out=eq[:], in0=lg[:], in1=bc(m1[:]), op=ALU.is_equal), r=[lg, m1], w=[eq])
            k.op("dve", lambda e: e.scalar_tensor_tensor(out=lg2[:], in0=eq[:], scalar=-1e30, in1=lg[:], op0=ALU.mult, op1=ALU.add),
                 r=[eq, lg], w=[lg2])
            k.op("dve", lambda e: e.tensor_reduce(out=m2[:], in_=lg2[:], axis=AX.X, op=ALU.max), r=[lg2], w=[m2])
            k.op("dve", lambda e: e.tensor_tensor(out=eq[:], in0=lg[:], in1=bc(m2[:]), op=ALU.is_ge), r=[lg, m2], w=[eq])
            k.op("dve", lambda e: e.tensor_tensor(out=lg2[:], in0=lg[:], in1=bc(m1[:]), op=ALU.subtract), r=[lg, m1], w=[lg2])
            k.op("act", lambda e: e.activation(out=lg2[:], in_=lg2[:], func=AF.Exp), r=[lg2], w=[lg2])
            k.op("dve", lambda e: e.tensor_tensor(out=lg2[:], in0=lg2[:], in1=eq[:], op=ALU.mult), r=[lg2, eq], w=[lg2])
            k.op("dve", lambda e: e.tensor_reduce(out=m2[:], in_=lg2[:], axis=AX.X, op=ALU.add), r=[lg2], w=[m2])
            k.op("dve", lambda e: e.reciprocal(out=m2[:], in_=m2[:]), r=[m2], w=[m2])
            k.op("dve", lambda e: e.tensor_tensor(out=lg2[:], in0=lg2[:], in1=bc(m2[:]), op=ALU.mult), r=[lg2, m2], w=[lg2])
            for ch in range(4):
                k.op("pe", lambda e, ch=ch: e.matmul(pY[0:8, ch * 128:(ch + 1) * 128], lhsT=lg2[:, ch, :], rhs=idn[:], start=True, stop=True),
                     r=[lg2, idn], w=[pY])
            k.op("act", lambda e: e.activation(out=gT[:], in_=pY[0:8, :], func=AF.Copy), r=[pY], w=[gT])
            k.dma("sp", g.gT[:, c0:c0 + 512], gT[:], r=[gT], w=[g.gT_r])
        k.epoch()


def phase_ffn(g, l):
    nc, k = g.nc, g.k
    moe = (l % 2 == 1)
    NEXP = NE if moe else 1
    dff = DFE if moe else DFF
    groups = [(f0, min(512, dff - f0)) for f0 in range(0, dff, 512)]
    TB = 1024
    blocks = ([] if l == 1 else [(0, CTX, 1)]) + [(CTX + i * TB, TB, 0) for i in range(SEQ // TB)]
    with ExitStack() as ph:
        sb = lambda n, s, d: T(ph.enter_context(nc.sbuf_tensor(uname(n), s, d)))
        hb = sb("hb", [128, 8, TB], BF16)
        acc = sb("acc", [128, 8, TB], F32)
        wg = [sb("wg%d" % i, [128, 8, 512], BF16) for i in range(2)]
        wu = [sb("wu%d" % i, [128, 8, 512], BF16) for i in range(2)]
        wd = [sb("wd%d" % i, [128, 4, D], BF16) for i in range(2)]
        at = [sb("at%d" % i, [128, 4, 512], BF16) for i in range(2)]
        sg = [sb("sg%d" % i, [128, 512], F32) for i in range(2)]
        a1 = [sb("a1%d" % i, [128, 512], F32) for i in range(2)]
        xt = sb("gx", [128, 8, 512], F32)
        sq = sb("gsq", [128, 8, 512], BF16)
        rstd = sb("grstd", [128, 512], F32)
        tmp = sb("gtmp", [128, 512], F32)
        tmp2 = [sb("gtmp2_%d" % i, [128, 512], F32) for i in range(2)]
        if moe:
            gb = sb("gb", [128, NE, TB], BF16)
            gTs = sb("gTl", [8, TB], F32)
            sel = sb("sel", [8, NE, 128], F32)
            k.dma("sp", sel[:], g.i["sel"], w=[sel])
        h2v = g.h2T.rearrange("(dc p) t -> p dc t", p=128)
        xv = g.xres.rearrange("(dc p) t -> p dc t", p=128)
        ov = g.out.rearrange("(dc p) t -> p dc t", p=128)
        wi = 0
        ai = 0
        pi = 0
        import os
        bsel = os.environ.get("FFN_BLOCKS")
        if bsel:
            blocks = [blocks[int(x)] for x in bsel.split(",")]
        for (c0, tb, wh) in blocks:
            ntt = (tb + 511) // 512
            k.dma("sp", hb[:, :, :tb], h2v[:, :, c0:c0 + tb], r=[g.h2T_r], w=[hb])
            if moe:
                k.dma("sp", gTs[:, :tb], g.gT[:, c0:c0 + tb], r=[g.gT_r], w=[gTs])
                for e_ in range(NE):
                    for tt in range(ntt):
                        ps = g.ps[6 + (pi % 2)]
                        pi += 1
                        k.op("pe", lambda e, e_=e_, tt=tt, ps=ps: e.matmul(ps[:, :], lhsT=sel[:, e_, :], rhs=gTs[:, tt * 512:(tt + 1) * 512],
                                                                          start=True, stop=True), r=[sel, gTs], w=[ps])
                        k.op("act", lambda e, e_=e_, tt=tt, ps=ps: e.activation(out=gb[:, e_, tt * 512:(tt + 1) * 512], in_=ps[:, :], func=AF.Copy),
                             r=[ps], w=[gb])
            first = True
            for e_ in range(NEXP):
                if moe:
                    sg_, su_, sd_ = g.i["moe_w_gate"][e_], g.i["moe_w_up"][e_], g.i["moe_w_down"][e_]
                else:
                    sg_, su_, sd_ = g.i["ffn_w_gate"], g.i["ffn_w_up"], g.i["ffn_w_down"]
                for (f0, fw) in groups:
                    nfc = fw // 128
                    wgt, wut, wdt = wg[wi % 2], wu[wi % 2], wd[wi % 2]
                    wi += 1
                    gi = f0 // 512
                    bi = blocks.index((c0, tb, wh))
                    if moe and bi > 0:
                        rr = g.wscr_r[(e_, gi)]
                        k.dma("sp", wgt[:].rearrange("p a b -> p (a b)"), g.wscr[0][e_, gi], r=[rr], w=[wgt])
                        k.dma("sp", wut[:].rearrange("p a b -> p (a b)"), g.wscr[1][e_, gi], r=[rr], w=[wut])
                        k.dma("sp", wdt[:].rearrange("p a b -> p (a b)"), g.wscr[2][e_, gi], r=[rr], w=[wdt])
                    else:
                        for dc in range(8):
                            k.dma("pool", wgt[:, dc, :fw], sg_[dc * 128:(dc + 1) * 128, f0:f0 + fw], w=[wgt])
                            k.dma("pool", wut[:, dc, :fw], su_[dc * 128:(dc + 1) * 128, f0:f0 + fw], w=[wut])
                        for fc in range(nfc):
                            k.dma("pool", wdt[:, fc, :], sd_[f0 + fc * 128:f0 + (fc + 1) * 128, :], w=[wdt])
                        if moe:
                            rr = g.wscr_r[(e_, gi)] = Res()
                            k.dma("sp", g.wscr[0][e_, gi], wgt[:].rearrange("p a b -> p (a b)"), r=[wgt], w=[rr])
                            k.dma("sp", g.wscr[1][e_, gi], wut[:].rearrange("p a b -> p (a b)"), r=[wut], w=[rr])
                            k.dma("sp", g.wscr[2][e_, gi], wdt[:].rearrange("p a b -> p (a b)"), r=[wdt], w=[rr])
                    for tt in range(ntt):
                        tn = min(512, tb - tt * 512)
                        ts = slice(tt * 512, tt * 512 + tn)
                        a_ = at[ai % 2]
                        ai += 1
                        for fc in range(nfc):
                            pg, pu = g.ps[(pi % 2) * 2], g.ps[(pi % 2) * 2 + 1]
                            s_, a1_ = sg[pi % 2], a1[pi % 2]
                            pi += 1
                            for dc in range(8):
                                k.op("pe", lambda e, pg=pg, dc=dc, fc=fc, wgt=wgt, ts=ts: e.matmul(
                                    pg[:, :tn], lhsT=wgt[:, dc, fc * 128:(fc + 1) * 128], rhs=hb[:, dc, ts],
                                    start=(dc == 0), stop=(dc == 7)), r=[wgt, hb], w=[pg])
                            for dc in range(8):
                                k.op("pe", lambda e, pu=pu, dc=dc, fc=fc, wut=wut, ts=ts: e.matmul(
                                    pu[:, :tn], lhsT=wut[:, dc, fc * 128:(fc + 1) * 128], rhs=hb[:, dc, ts],
                                    start=(dc == 0), stop=(dc == 7)), r=[wut, hb], w=[pu])
                            k.op("act", lambda e, pg=pg, s_=s_: e.activation(out=s_[:, :tn], in_=pg[:, :tn], func=AF.Silu), r=[pg], w=[s_])
                            if moe:
                                k.op("dve", lambda e, pu=pu, s_=s_, a1_=a1_: e.tensor_tensor(out=a1_[:, :tn], in0=s_[:, :tn], in1=pu[:, :tn], op=ALU.mult),
                                     r=[pu, s_], w=[a1_])
                                k.op("pool", lambda e, a1_=a1_, a_=a_, fc=fc, e_=e_, ts=ts: e.tensor_tensor(out=a_[:, fc, :tn], in0=a1_[:, :tn], in1=gb[:, e_, ts],
                                                                                                     op=ALU.mult), r=[a1_, gb], w=[a_])
                            else:
                                k.op("dve", lambda e, pu=pu, s_=s_, a_=a_, fc=fc: e.tensor_tensor(out=a_[:, fc, :tn], in0=s_[:, :tn], in1=pu[:, :tn], op=ALU.mult),
                                     r=[pu, s_], w=[a_])
                        for dc in range(8):
                            po = g.ps[4 + (dc % 2)]
                            for fc in range(nfc):
                                k.op("pe", lambda e, po=po, dc=dc, fc=fc, wdt=wdt, a_=a_: e.matmul(
                                    po[:, :tn], lhsT=wdt[:, fc, dc * 128:(dc + 1) * 128], rhs=a_[:, fc, :tn],
                                    start=(fc == 0), stop=(fc == nfc - 1)), r=[wdt, a_], w=[po])
                            if first:
                                k.op("act", lambda e, po=po, dc=dc, ts=ts: e.activation(out=acc[:, dc, ts], in_=po[:, :tn], func=AF.Copy), r=[po], w=[acc])
                            else:
                                k.op("dve", lambda e, po=po, dc=dc, ts=ts: e.tensor_tensor(out=acc[:, dc, ts], in0=acc[:, dc, ts], in1=po[:, :tn], op=ALU.add),
                                     r=[po, acc], w=[acc])
                    first = False
            for tt in range(ntt):
                tn = min(512, tb - tt * 512)
                ts = slice(tt * 512, tt * 512 + tn)
                cc0 = c0 + tt * 512
                k.dma("sp", xt[:, :, :tn], xv[:, :, cc0:cc0 + tn], r=[g.xres_r], w=[xt])
                k.op("act", lambda e, ts=ts: e.activation(out=sq[:, :, :tn], in_=acc[:, :, ts], func=AF.Square), r=[acc], w=[sq])
                ps = g.ps[6]
                for dc in range(8):
                    k.op("pe", lambda e, dc=dc, ps=ps: e.matmul(ps[:, :tn], lhsT=g.ones_bf[:], rhs=sq[:, dc, :tn], start=(dc == 0), stop=(dc == 7)),
                         r=[sq, g.ones_bf], w=[ps])
                k.op("act", lambda e, ps=ps: e.activation(out=tmp[:, :tn], in_=ps[:, :tn], func=AF.Sqrt, scale=1.0 / D, bias=g.eps_t[:]),
                     r=[ps, g.eps_t], w=[tmp])
                k.op("dve", lambda e: e.reciprocal(out=rstd[:, :tn], in_=tmp[:, :tn]), r=[tmp], w=[rstd])
                for dc in range(8):
                    t2 = tmp2[dc % 2]
                    k.op("dve", lambda e, dc=dc, t2=t2, ts=ts: e.scalar_tensor_tensor(
                        out=t2[:, :tn], in0=acc[:, dc, ts], scalar=g.modv[:, 5, dc, wh:wh + 1], in1=rstd[:, :tn],
                        op0=ALU.mult, op1=ALU.mult), r=[acc, rstd, g.modv], w=[t2])
                    k.op("pool", lambda e, dc=dc, t2=t2: e.tensor_tensor(out=xt[:, dc, :tn], in0=xt[:, dc, :tn], in1=t2[:, :tn], op=ALU.add),
                         r=[t2, xt], w=[xt])
                if l == 1:
                    k.dma("sp", ov[:, :, cc0 - CTX:cc0 - CTX + tn], xt[:, :, :tn], r=[xt], w=[g.out_r])
                else:
                    k.dma("sp", xv[:, :, cc0:cc0 + tn], xt[:, :, :tn], r=[xt], w=[g.xres_r])
            k.epoch()

IN_SPECS = {
    "xT": ([D, TT], F32), "posT": ([D, SEQ], F32), "cvec": ([128, 8, 2], F32),
    "ada_w": ([2, D, 6 * D], F32), "ada_b": ([2, 128, 48], F32), "norm_g": ([2, 128, 4, 8], F32),
    "w_in": ([2, D, DIN], F32),
    "rw_par": ([2, 64, 8, 2, 8], F32), "rw_lmu": ([2, 64, 2, 2], F32), "rw_lora": ([2, 64, 2, 2, 512], F32),
    "rw_g2": ([2, 128, 512], F32), "rw_gn": ([2, 64, 2, 512], F32),
    "masks": ([64, 2, 320], F32), "ident": ([128, 128], F32), "rmask": ([64, SEG], F32),
    "conv_w": ([2, 2, 128, 3], F32),
    "w_out": ([2, D, D], F32),
    "ffn_w_gate": ([D, DFF], F32), "ffn_w_up": ([D, DFF], F32), "ffn_w_down": ([DFF, D], F32),
    "moe_w_gate": ([NE, D, DFE], F32), "moe_w_up": ([NE, D, DFE], F32), "moe_w_down": ([NE, DFE, D], F32),
    "router_w": ([128, 8, 8], F32), "router_b": ([128, 4, 8], F32), "sel": ([8, NE, 128], F32),
    "bd64": ([2, 128, 128], BF16), "dftL": ([2, 16, 128, 32 * 256], BF16), "dftC": ([2, 1, 128, 2 * 256], BF16),
}


def build(phases=None, dbg=(), ext_in=(), heads=range(8)):
    nc = bass.Bass("TRN2", target_bir_lowering=False)
    g = Ctx()
    g.nc = nc
    g.heads = list(heads)
    import os
    g.rw_stage = int(os.environ.get('RW_STAGE', '4'))
    g.rw_maxseg = int(os.environ.get('RW_MAXSEG', '99'))
    g.rw_s1 = os.environ.get('RW_S1', 'bad')
    g.i = {}
    if phases is None:
        phases = []
        for l in range(2):
            phases += [("mod", l), ("win", l), ("rwkv", l), ("conv", l), ("fourier", l), ("wout", l)]
            if l == 1:
                phases.append(("router", l))
            phases.append(("ffn", l))
    used = set()
    need = {"mod": ("cvec", "ada_w", "ada_b", "norm_g"), "win": ("xT", "posT", "w_in"),
            "rwkv": ("rw_par", "rw_lmu", "rw_lora", "rw_g2", "rw_gn", "masks", "ident", "rmask"),
            "conv": ("conv_w",), "fourier": ("bd64", "dftL", "dftC"), "wout": ("w_out",),
            "router": ("router_w", "router_b", "ident"), "ffn": ()}
    for (p, l) in phases:
        used.update(need[p])
        if p == "ffn":
            used.update(("moe_w_gate", "moe_w_up", "moe_w_down", "sel") if l == 1 else ("ffn_w_gate", "ffn_w_up", "ffn_w_down"))
    for name, (shape, dt) in IN_SPECS.items():
        if name in used:
            g.i[name] = nc.dram_tensor(name, shape, dt, kind="ExternalInput").ap()
    g.in_names = sorted(used)

    def scratch(name, shape, dt):
        kind = "ExternalOutput" if name in dbg else ("ExternalInput" if name in ext_in else "Internal")
        return nc.dram_tensor(name, shape, dt, kind=kind).ap()
    g.xres = scratch("xres", [D, TT], F32) if "xres_in" not in ext_in else nc.dram_tensor("xres", [D, TT], F32, kind="ExternalOutput").ap()
    g.xres_in = nc.dram_tensor("xres_in", [D, TT], F32, kind="ExternalInput").ap() if "xres_in" in ext_in else None
    g.zT = scratch("zT", [DIN, TT], F32)
    g.ymix = scratch("ymix", [D, TT], BF16)
    g.h2T = scratch("h2T", [D, TT], BF16)
    g.h2F = scratch("h2F", [D, TT], F32)
    g.gT = scratch("gT", [8, TT], F32)
    g.wscr = [scratch("wscr%d" % i, [NE, DFE // 512, 128, 4096], BF16) for i in range(3)]
    g.wscr_r = {}
    g.xres_r, g.zT_r, g.ymix_r, g.h2T_r, g.h2F_r, g.gT_r, g.out_r = [Res() for _ in range(7)]
    g.dbg_state = None
    if "state" in dbg:
        g.dbg_state = nc.dram_tensor("state", [2, 2, 8, 64, 64], F32, kind="ExternalOutput").ap()
    g.out = nc.dram_tensor("out", [D, SEQ], F32, kind="ExternalOutput").ap()
    with ExitStack() as es:
        g.k = k = KB(nc, es)
        sb = lambda n, s, d: T(es.enter_context(nc.sbuf_tensor(uname(n), s, d)))
        g.ps = [T(es.enter_context(nc.psum_tensor("ps%d" % i, [128, 512], F32))) for i in range(8)]
        g.modv = sb("modv", [128, 6, 8, 2], F32)
        g.ones_bf = sb("ones_bf", [128, 128], BF16)
        g.eps_t = sb("eps_t", [128, 1], F32)
        g.gneps_t = sb("gneps_t", [128, 1], F32)
        k.op("dve", lambda e: e.memset(g.ones_bf[:], 1.0), w=[g.ones_bf])
        k.op("dve", lambda e: e.memset(g.eps_t[:], EPS), w=[g.eps_t])
        k.op("dve", lambda e: e.memset(g.gneps_t[:], GN_EPS), w=[g.gneps_t])
        if g.xres_in is not None:
            for i_ in range(32):
                k.dma("sp", g.xres[i_ * 32:(i_ + 1) * 32, :], g.xres_in[i_ * 32:(i_ + 1) * 32, :], w=[g.xres_r])
        fns = {"mod": phase_mod, "win": phase_win, "conv": phase_conv, "fourier": phase_fourier,
               "wout": phase_wout, "router": phase_router, "ffn": phase_ffn}
        for (p, l) in phases:
            if p == "rwkv":
                phase_rwkv(g, l, l == 1)
            else:
                fns[p](g, l)
        k.barrier()
        if "modv" in dbg:
            md = nc.dram_tensor("modv_o", [128, 96], F32, kind="ExternalOutput").ap()
            k.dma("sp", md, g.modv[:].rearrange("p a b c -> p (a b c)"), r=[g.modv])
        k.barrier(["sp"])
    print("instructions", k.nins, "waits", k.nwait)
    return nc, g


def sincos_pos():
    quarter = D // 4
    omega = 1.0 / (10000.0 ** (np.arange(quarter, dtype=np.float32) / quarter))

    def axis_emb(n):
        ang = np.arange(n, dtype=np.float32)[:, None] * omega[None, :]
        return np.concatenate([np.sin(ang), np.cos(ang)], axis=-1)
    er, ec = axis_emb(64), axis_emb(64)
    emb = np.concatenate([np.broadcast_to(er[:, None, :], (64, 64, D // 2)),
                          np.broadcast_to(ec[None, :, :], (64, 64, D // 2))], axis=-1)
    return np.ascontiguousarray(emb.reshape(SEQ, D).T.astype(np.float32))


_CONST = {}


def constants():
    if _CONST:
        return _CONST
    import ml_dtypes
    bf = ml_dtypes.bfloat16
    c = _CONST
    c["posT"] = sincos_pos()
    c["ident"] = np.eye(128, dtype=np.float32)
    s_ = np.arange(64)[:, None]
    t_ = np.arange(64)[None, :]
    m = np.zeros((64, 2, 320), np.float32)
    for d, (st, inc) in enumerate((((s_ < t_), (s_ <= t_)), ((s_ > t_), (s_ >= t_)))):
        m[:, d, 0:64] = st
        m[:, d, 64:128] = inc
        m[:, d, 128:192] = st
        m[:, d, 192:256] = inc
        m[:, d, 256:320] = st.T
    c["masks"] = m
    sel = np.zeros((8, NE, 128), np.float32)
    for e_ in range(NE):
        sel[e_, e_, :] = 1.0
    c["sel"] = sel
    rm = np.ones((64, SEG), np.float32)
    rm[:, ::64] = 0.0
    c["rmask"] = rm
    a64 = 2 * np.pi * np.outer(np.arange(64), np.arange(64)) / 64
    bd = np.zeros((2, 128, 128), np.float64)
    for i in range(2):
        bd[0, i * 64:(i + 1) * 64, i * 64:(i + 1) * 64] = np.cos(a64)
        bd[1, i * 64:(i + 1) * 64, i * 64:(i + 1) * 64] = np.sin(a64)
    c["bd64"] = bd.astype(bf)
    for name, L in (("dftL", SEQ), ("dftC", CTX)):
        idx = (np.outer(np.arange(L), np.arange(L)) % L).astype(np.float64)
        ang = 2 * np.pi * idx / L
        sc = 1.0 / np.sqrt(64.0 * L)
        mats = np.stack([np.cos(ang) * sc, -np.sin(ang) * sc])
        ntt, ntp = L // 128, L // 256
        mats = mats.reshape(2, ntt, 128, ntp, 256).transpose(0, 3, 2, 1, 4)
        c[name] = np.ascontiguousarray(mats.reshape(2, ntp, 128, ntt * 256)).astype(bf)
    return c


def make_in_maps(inp, names, cores=range(8)):
    f32 = lambda a: np.ascontiguousarray(np.asarray(a, dtype=np.float32))
    shared = dict(constants())
    shared["ada_w"] = f32(inp["ada_w"])
    shared["ada_b"] = f32(inp["ada_b"].reshape(2, 48, 128).transpose(0, 2, 1))
    shared["norm_g"] = f32(inp["norm_g"].reshape(2, 4, 8, 128).transpose(0, 3, 1, 2))
    shared["w_in"] = f32(inp["w_in"])
    par = np.zeros((2, 64, 8, 2, 8), np.float32)
    hp = lambda a: a.reshape(2, 8, 64).transpose(0, 2, 1)
    for d in range(2):
        for j in range(3):
            par[:, :, :, d, j] = hp(inp["rwkv_mu"][:, d, j])
        par[:, :, :, d, 3] = hp(inp["rwkv_w0"][:, d])
        par[:, :, :, d, 4] = hp(inp["rwkv_a0"][:, d])
        par[:, :, :, d, 5] = hp(inp["rwkv_k_k"])
        par[:, :, :, d, 6] = hp(inp["rwkv_k_a"])
        par[:, :, :, d, 7] = hp(inp["rwkv_r_k"].reshape(2, 512))
    shared["rw_par"] = par
    shared["rw_lmu"] = f32(np.stack([inp["rwkv_mu_w"], inp["rwkv_mu_a"]], axis=-1).transpose(0, 2, 1, 3))
    shared["rw_lora"] = f32(np.stack([inp["rwkv_w2"], inp["rwkv_a2"]], axis=2).transpose(0, 3, 1, 2, 4))
    shared["rw_g2"] = f32(inp["rwkv_g2"])
    gn = np.stack([inp["rwkv_gn_w"], inp["rwkv_gn_b"]], axis=1)
    shared["rw_gn"] = f32(np.broadcast_to(gn[:, None], (2, 64, 2, 512)))
    shared["conv_w"] = f32(inp["conv_w"].transpose(0, 2, 1).reshape(2, 2, 128, 3))
    shared["w_out"] = f32(inp["w_out"])
    for n_ in ("ffn_w_gate", "ffn_w_up", "ffn_w_down", "moe_w_gate", "moe_w_up", "moe_w_down"):
        if n_ in names:
            shared[n_] = f32(inp[n_][0])
    shared["router_w"] = f32(inp["router_w"][0].reshape(8, 128, 8).transpose(1, 0, 2))
    shared["router_b"] = f32(np.broadcast_to(inp["router_b"][0][None, None, :], (128, 4, 8)))
    maps = []
    for b in cores:
        m = {n: shared[n] for n in names if n in shared}
        if "xT" in names:
            m["xT"] = f32(np.concatenate([inp["ctx"][b].T, inp["x"][b].T], axis=1))
        if "cvec" in names:
            m["cvec"] = f32(np.stack([inp["c"][b].reshape(8, 128).T, inp["c_ctx"].reshape(8, 128).T], axis=-1))
        maps.append(m)
    return maps


def kernel(**inp):
    inp = {k_: np.asarray(v) for k_, v in inp.items()}
    nc, g = build()
    maps = make_in_maps(inp, g.in_names)
    res = run_bass_kernel_spmd(nc, maps, core_ids=list(range(8)))
    out = np.stack([np.ascontiguousarray(r["out"].T) for r in res.results], axis=0)
    return out.astype(np.float32)
```

```python
import numpy as np
import concourse.bass as bass
import concourse.mybir as mybir
from concourse.bass_utils import run_bass_kernel_spmd
from contextlib import ExitStack

F32 = mybir.dt.float32
BF16 = mybir.dt.bfloat16
AF = mybir.ActivationFunctionType
ALU = mybir.AluOpType
AX = mybir.AxisListType
NDS = 40
NPL = 8

D = 1024
SEQ = 4096
CTX = 256
TT = CTX + SEQ
DIN = 2944
DFF = 2816
DFE = 3584
NE = 8
EPS = 1e-6
GN_EPS = 64e-5
O_K, O_V, O_WF, O_WB, O_AF, O_AB, O_R, O_G, O_U, O_B, O_C, O_F = (
    0, 512, 1024, 1088, 1152, 1216, 1280, 1792, 1920, 2176, 2432, 2688)


class Res:
    __slots__ = ("w", "r")

    def __init__(self):
        self.w = None
        self.r = []


class T:
    def __init__(self, h, res=None):
        self.h = h
        self.res = res if res is not None else Res()

    def __getitem__(self, idx):
        return self.h[idx]


def _res(x):
    return x.res if isinstance(x, T) else x


class KB:
    ENG = ("pe", "act", "dve", "pool", "sp")

    def __init__(self, nc, es):
        self.nc = nc
        self.eng = dict(pe=nc.tensor, act=nc.scalar, dve=nc.vector, pool=nc.gpsimd, sp=nc.sync)
        self.sems = [{e: es.enter_context(nc.semaphore("s%d_%s" % (i, e))) for e in self.ENG} for i in range(2)]
        self.dsemss = [[es.enter_context(nc.semaphore("d%d_%d" % (j, i))) for i in range(NDS - NPL)] for j in range(2)]
        self.psems = [es.enter_context(nc.semaphore("p%d" % i)) for i in range(NPL)]
        self.pcnt = [0] * NPL
        self.pnext = 0
        self.cur = 0
        self.ep = 0
        self._reset()
        self.nwait = 0
        self.nins = 0
        self.hook = None

    def _reset(self):
        self.sem = self.sems[self.cur]
        self.dsems = self.dsemss[self.cur]
        self.cnt = {e: 0 for e in self.ENG}
        self.seen = {e: {} for e in self.ENG}
        self.dcnt = [0] * (NDS - NPL)
        self.dnext = 0

    def _wait(self, eng, tok):
        if tok is None or tok[3] != self.ep:
            return
        kind, a, n, _ = tok
        if kind == "c":
            if a == "pe" and eng == "pe":
                return
            sem, val, key = self.sem[a], n, a
        elif kind == "p":
            sem, val, key = self.psems[a], 16 * n, "p%d" % a
        else:
            sem, val, key = self.dsems[a], 16 * n, "d%d" % a
        if self.seen[eng].get(key, 0) >= val:
            return
        self.eng[eng].wait_ge(sem, val)
        self.seen[eng][key] = val
        self.nwait += 1

    def _deps(self, eng, r, w):
        toks = []
        for x in r:
            toks.append(_res(x).w)
        for x in w:
            rs = _res(x)
            toks.append(rs.w)
            toks.extend(rs.r)
        for t in toks:
            self._wait(eng, t)

    def _mark(self, tok, r, w):
        for x in r:
            rs = _res(x)
            rs.r = [t for t in rs.r if t[3] == self.ep and not (t[0] == tok[0] and t[1] == tok[1])]
            rs.r.append(tok)
        for x in w:
            rs = _res(x)
            rs.w = tok
            rs.r = []

    def op(self, eng, fn, r=(), w=()):
        self._deps(eng, r, w)
        ins = fn(self.eng[eng])
        self.cnt[eng] += 1
        ins.then_inc(self.sem[eng], 1)
        self.nins += 1
        tok = ("c", eng, self.cnt[eng], self.ep)
        self._mark(tok, r, w)
        if self.hook:
            self.hook()
        return tok

    def dma(self, q, out, in_, r=(), w=(), **kw):
        if q == "pool":
            i = self.pnext
            self.pnext = (i + 1) % NPL
            if self.pcnt[i] > 0:
                self.seen[q].pop("p%d" % i, None)
                self.eng[q].wait_ge(self.psems[i], 16 * self.pcnt[i])
            self._deps(q, r, w)
            ins = self.eng[q].dma_start(out=out, in_=in_, **kw)
            self.pcnt[i] += 1
            ins.then_inc(self.psems[i], 16)
            self.nins += 1
            tok = ("p", i, self.pcnt[i], self.ep)
            self._mark(tok, r, w)
            return tok
        i = self.dnext
        self.dnext = (i + 1) % (NDS - NPL)
        if self.dcnt[i] > 0:
            self._wait(q, ("d", i, self.dcnt[i], self.ep))
        self._deps(q, r, w)
        ins = self.eng[q].dma_start(out=out, in_=in_, **kw)
        self.dcnt[i] += 1
        ins.then_inc(self.dsems[i], 16)
        self.nins += 1
        tok = ("d", i, self.dcnt[i], self.ep)
        self._mark(tok, r, w)
        return tok

    def barrier(self, engines=None):
        engines = engines or self.ENG
        for e in engines:
            for f in self.ENG:
                if self.cnt[f] > 0 and f != e:
                    self._wait(e, ("c", f, self.cnt[f], self.ep))
            if e != "pe" and self.cnt[e] > 0:
                self._wait(e, ("c", e, self.cnt[e], self.ep))
            for i in range(NDS - NPL):
                if self.dcnt[i] > 0:
                    self._wait(e, ("d", i, self.dcnt[i], self.ep))
            for i in range(NPL):
                if self.pcnt[i] > 0:
                    self.seen[e].pop("p%d" % i, None)
                    self._wait(e, ("p", i, self.pcnt[i], self.ep))

    def epoch(self):
        self.barrier()
        old_sem, old_d = self.sem, self.dsems
        self.cur = 1 - self.cur
        self.ep += 1
        self._reset()
        for e in self.ENG:
            ins = self.eng[e].nop()
            self.cnt[e] = 1
            ins.then_inc(self.sem[e], 1)
        for e in self.ENG:
            if e != "sp":
                self._wait("sp", ("c", e, 1, self.ep))
        for s_ in list(old_sem.values()) + list(old_d):
            self.eng["sp"].sem_clear(s_)
        ins = self.eng["sp"].nop()
        self.cnt["sp"] += 1
        ins.then_inc(self.sem["sp"], 1)


class Ctx:
    pass


def weave(k, fns, quota):
    import threading
    n = len(fns)
    if n == 1:
        fns[0]()
        return
    sems = [threading.Semaphore(0) for _ in fns]
    main = threading.Semaphore(0)
    done = [False] * n
    state = {"cur": 0, "left": quota[0], "err": None}

    def nxt(i):
        for j in range(1, n + 1):
            t = (i + j) % n
            if not done[t]:
                return t
        return None

    def hook():
        i = state["cur"]
        state["left"] -= 1
        if state["left"] <= 0:
            t = nxt(i)
            if t is not None and t != i:
                state["cur"] = t
                state["left"] = quota[t]
                sems[t].release()
                sems[i].acquire()
            else:
                state["left"] = quota[i]

    def runner(i):
        sems[i].acquire()
        try:
            fns[i]()
        except BaseException as ex:
            state["err"] = ex
        finally:
            done[i] = True
            t = nxt(i)
            if t is not None:
                state["cur"] = t
                state["left"] = quota[t]
                sems[t].release()
            else:
                main.release()

    k.hook = hook
    threads = [threading.Thread(target=runner, args=(i,)) for i in range(n)]
    for t in threads:
        t.start()
    sems[0].release()
    main.acquire()
    for t in threads:
        t.join()
    k.hook = None
    if state["err"] is not None:
        raise state["err"]


_UID = [0]


def uname(n):
    _UID[0] += 1
    return "%s_%d" % (n, _UID[0])


def token_tiles():
    tiles = [(0, CTX, 1)]
    for i in range(SEQ // 512):
        tiles.append((CTX + i * 512, 512, 0))
    return tiles


def phase_mod(g, l):
    nc, k = g.nc, g.k
    with ExitStack() as ph:
        sb = lambda n, s, d: T(ph.enter_context(nc.sbuf_tensor(uname(n), s, d)))
        cv = sb("cv", [128, 8, 2], F32)
        scv = sb("scv", [128, 8, 2], F32)
        adab = sb("adab", [128, 48], F32)
        ng = sb("ng", [128, 4, 8], F32)
        raw = sb("raw", [128, 48, 2], F32)
        wbuf = [sb("adaw%d" % i, [128, 8, 512], F32) for i in range(2)]
        ps = g.ps[0]
        k.dma("sp", cv[:], g.i["cvec"], w=[cv])
        k.dma("sp", adab[:], g.i["ada_b"][l], w=[adab])
        k.dma("sp", ng[:], g.i["norm_g"][l], w=[ng])
        k.op("act", lambda e: e.activation(out=scv[:], in_=cv[:], func=AF.Silu), r=[cv], w=[scv])
        wsrc = g.i["ada_w"][l].rearrange("(kc p) n -> p kc n", p=128)
        psv = ps[:, 0:96].rearrange("p (n w) -> p n w", w=2)
        for nb in range(12):
            wb = wbuf[nb % 2]
            k.dma("sp", wb[:], wsrc[:, :, nb * 512:(nb + 1) * 512], w=[wb])
            for j in range(4):
                n = nb * 4 + j
                for kc in range(8):
                    k.op("pe", lambda e, n=n, j=j, kc=kc, wb=wb: e.matmul(
                        psv[:, n, :], lhsT=wb[:, kc, j * 128:(j + 1) * 128], rhs=scv[:, kc, :],
                        start=(kc == 0), stop=(kc == 7)), r=[wb, scv], w=[ps])
        for w_ in range(2):
            k.op("dve", lambda e, w_=w_: e.tensor_tensor(out=raw[:, :, w_], in0=psv[:, :, w_], in1=adab[:],
                                                  op=ALU.add), r=[ps, adab], w=[raw])
        mv = g.modv
        for w_ in range(2):
            for (slot, jsc, jsh, jg, n0, n1) in ((0, 1, 0, 2, 0, 1), (3, 4, 3, 5, 2, 3)):
                k.op("dve", lambda e, w_=w_, slot=slot, jsc=jsc, n0=n0: e.scalar_tensor_tensor(
                    out=mv[:, slot, :, w_], in0=raw[:, jsc * 8:(jsc + 1) * 8, w_], scalar=1.0, in1=ng[:, n0, :],
                    op0=ALU.add, op1=ALU.mult), r=[raw, ng], w=[mv])
                k.op("dve", lambda e, w_=w_, slot=slot, jsh=jsh: e.tensor_copy(
                    out=mv[:, slot + 1, :, w_], in_=raw[:, jsh * 8:(jsh + 1) * 8, w_]), r=[raw], w=[mv])
                k.op("dve", lambda e, w_=w_, slot=slot, jg=jg, n1=n1: e.tensor_tensor(
                    out=mv[:, slot + 2, :, w_], in0=raw[:, jg * 8:(jg + 1) * 8, w_], in1=ng[:, n1, :],
                    op=ALU.mult), r=[raw, ng], w=[mv])
        k.epoch()


def rms_rstd(g, xt, tn, sq, ps, rstd, tmp):
    k = g.k
    k.op("act", lambda e: e.activation(out=sq[:, :, :tn], in_=xt[:, :, :tn], func=AF.Square), r=[xt], w=[sq])
    for dc in range(8):
        k.op("pe", lambda e, dc=dc: e.matmul(ps[:, :tn], lhsT=g.ones_bf[:], rhs=sq[:, dc, :tn],
                                             start=(dc == 0), stop=(dc == 7)), r=[sq, g.ones_bf], w=[ps])
    k.op("act", lambda e: e.activation(out=tmp[:, :tn], in_=ps[:, :tn], func=AF.Sqrt, scale=1.0 / D, bias=g.eps_t[:]),
         r=[ps, g.eps_t], w=[tmp])
    k.op("dve", lambda e: e.reciprocal(out=rstd[:, :tn], in_=tmp[:, :tn]), r=[tmp], w=[rstd])


def phase_win(g, l):
    nc, k = g.nc, g.k
    with ExitStack() as ph:
        sb = lambda n, s, d: T(ph.enter_context(nc.sbuf_tensor(uname(n), s, d)))
        win = sb("win", [128, 8, DIN], BF16)
        wsrc = g.i["w_in"][l].rearrange("(kc p) n -> p kc n", p=128)
        for kc in range(8):
            k.dma("pool", win[:, kc, :], wsrc[:, kc, :], w=[win])
        xts = [sb("xt%d" % i, [128, 8, 512], F32) for i in range(2)]
        pts = [sb("pt%d" % i, [128, 8, 512], F32) for i in range(2)]
        sq = sb("sq", [128, 8, 512], BF16)
        hT = [sb("hT%d" % i, [128, 8, 512], BF16) for i in range(2)]
        rstd = sb("rstd", [128, 512], F32)
        tmp = sb("tmp", [128, 512], F32)
        tmp2 = [sb("tmp2_%d" % i, [128, 512], F32) for i in range(2)]
        zsb = [sb("zsb%d" % i, [128, 512], F32) for i in range(4)]
        xsrc = (g.i["xT"] if l == 0 else g.xres).rearrange("(dc p) t -> p dc t", p=128)
        xdst = g.xres.rearrange("(dc p) t -> p dc t", p=128)
        psrc = g.i["posT"].rearrange("(dc p) t -> p dc t", p=128)
        zi = 0
        for ti, (c0, tn, wh) in enumerate(token_tiles()):
            xt = xts[ti % 2]
            h = hT[ti % 2]
            k.dma("sp", xt[:, :, :tn], xsrc[:, :, c0:c0 + tn], r=[g.xres_r] if l else [], w=[xt])
            if l == 0:
                if wh == 0:
                    pt = pts[ti % 2]
                    k.dma("sp", pt[:, :, :tn], psrc[:, :, c0 - CTX:c0 - CTX + tn], w=[pt])
                    k.op("pool", lambda e, xt=xt, pt=pt: e.tensor_tensor(out=xt[:], in0=xt[:], in1=pt[:], op=ALU.add),
                         r=[pt, xt], w=[xt])
                k.dma("sp", xdst[:, :, c0:c0 + tn], xt[:, :, :tn], r=[xt], w=[g.xres_r])
            rms_rstd(g, xt, tn, sq, g.ps[1], rstd, tmp)
            for dc in range(8):
                t2 = tmp2[dc % 2]
                k.op("dve", lambda e, dc=dc, t2=t2, xt=xt: e.scalar_tensor_tensor(
                    out=t2[:, :tn], in0=xt[:, dc, :tn], scalar=g.modv[:, 0, dc, wh:wh + 1], in1=rstd[:, :tn],
                    op0=ALU.mult, op1=ALU.mult), r=[xt, rstd, g.modv], w=[t2])
                k.op("act", lambda e, dc=dc, t2=t2, h=h: e.activation(
                    out=h[:, dc, :tn], in_=t2[:, :tn], func=AF.Identity, bias=g.modv[:, 1, dc, wh:wh + 1]),
                    r=[t2, g.modv], w=[h])
            for n in range(DIN // 128):
                ps = g.ps[2 + (zi % 4)]
                zs = zsb[zi % 4]
                for dc in range(8):
                    k.op("pe", lambda e, n=n, dc=dc, ps=ps, h=h: e.matmul(
                        ps[:, :tn], lhsT=win[:, dc, n * 128:(n + 1) * 128], rhs=h[:, dc, :tn],
                        start=(dc == 0), stop=(dc == 7)), r=[win, h], w=[ps])
                if zi % 2 == 0:
                    k.op("act", lambda e, ps=ps, zs=zs: e.activation(out=zs[:, :tn], in_=ps[:, :tn], func=AF.Copy),
                         r=[ps], w=[zs])
                else:
                    k.op("dve", lambda e, ps=ps, zs=zs: e.tensor_copy(out=zs[:, :tn], in_=ps[:, :tn]), r=[ps], w=[zs])
                k.dma("sp", g.zT[n * 128:(n + 1) * 128, c0:c0 + tn], zs[:, :tn], r=[zs], w=[g.zT_r])
                zi += 1
        k.epoch()


def phase_conv(g, l):
    nc, k = g.nc, g.k
    with ExitStack() as ph:
        sb = lambda n, s, d: T(ph.enter_context(nc.sbuf_tensor(uname(n), s, d)))
        for cc in range(2):
            u = sb("cu%d" % cc, [128, TT], F32)
            bg = sb("cb%d" % cc, [128, TT], F32)
            cg = sb("cc%d" % cc, [128, TT], F32)
            y = sb("cy%d" % cc, [128, TT], F32)
            yo = sb("cyo%d" % cc, [128, TT], BF16)
            cw = sb("cw%d" % cc, [128, 3], F32)
            k.dma("sp", cw[:], g.i["conv_w"][l, cc], w=[cw])
            k.dma("sp", u[:], g.zT[O_U + cc * 128:O_U + (cc + 1) * 128, :], r=[g.zT_r], w=[u])
            k.dma("sp", bg[:], g.zT[O_B + cc * 128:O_B + (cc + 1) * 128, :], r=[g.zT_r], w=[bg])
            k.dma("sp", cg[:], g.zT[O_C + cc * 128:O_C + (cc + 1) * 128, :], r=[g.zT_r], w=[cg])
            k.op("dve", lambda e: e.tensor_tensor(out=u[:], in0=u[:], in1=cg[:], op=ALU.mult), r=[cg, u], w=[u])
            k.op("dve", lambda e: e.tensor_scalar(out=y[:], in0=u[:], scalar1=cw[:, 1:2], scalar2=None, op0=ALU.mult),
                 r=[u, cw], w=[y])
            yl = y[:, CTX:].rearrange("p (r c) -> p r c", c=64)
            ul = u[:, CTX:].rearrange("p (r c) -> p r c", c=64)
            for (o_, i_, wi) in ((y[:, 1:CTX], u[:, 0:CTX - 1], 0), (y[:, 0:CTX - 1], u[:, 1:CTX], 2),
                                 (yl[:, :, 1:64], ul[:, :, 0:63], 0), (yl[:, :, 0:63], ul[:, :, 1:64], 2)):
                k.op("dve", lambda e, o_=o_, i_=i_, wi=wi: e.scalar_tensor_tensor(
                    out=o_, in0=i_, scalar=cw[:, wi:wi + 1], in1=o_, op0=ALU.mult, op1=ALU.add), r=[u, cw, y], w=[y])
            k.op("dve", lambda e: e.tensor_tensor(out=yo[:], in0=y[:], in1=bg[:], op=ALU.mult), r=[y, bg], w=[yo])
            k.dma("sp", g.ymix[512 + cc * 128:512 + (cc + 1) * 128, :], yo[:], r=[yo], w=[g.ymix_r])
        k.epoch()


def phase_fourier(g, l):
    nc, k = g.nc, g.k
    with ExitStack() as ph:
        sb = lambda n, s, d: T(ph.enter_context(nc.sbuf_tensor(uname(n), s, d)))
        bd = sb("bd", [128, 2, 128], BF16)
        k.dma("sp", bd[:], g.i["bd64"].rearrange("a p n -> p a n"), w=[bd])
        fst = sb("fst", [128, SEQ], F32)
        fb = sb("fb", [128, 2, SEQ], BF16)
        G = sb("G", [128, 32, 512], BF16)
        cb = [sb("dc%d" % i, [128, 32, 256], BF16) for i in range(2)]
        sbf = [sb("ds%d" % i, [128, 32, 256], BF16) for i in range(2)]
        yo = [sb("fyo%d" % i, [128, 256], BF16) for i in range(2)]
        it = 0
        for (c0, L, csrc) in ((0, CTX, g.i["dftC"]), (CTX, SEQ, g.i["dftL"])):
            if l == 1 and c0 == 0:
                continue
            ntt = L // 128
            for cc in range(2):
                k.dma("sp", fst[:, :L], g.zT[O_F + cc * 128:O_F + (cc + 1) * 128, c0:c0 + L], r=[g.zT_r], w=[fst])
                k.op("act", lambda e, cc=cc: e.activation(out=fb[:, cc, :L], in_=fst[:, :L], func=AF.Copy), r=[fst], w=[fb])
            for tt in range(ntt):
                ps = g.ps[tt % 2]
                for cs in range(2):
                    for cc in range(2):
                        k.op("pe", lambda e, ps=ps, cs=cs, cc=cc, tt=tt: e.matmul(
                            ps[:, (cs * 2 + cc) * 128:(cs * 2 + cc + 1) * 128], lhsT=fb[:, cc, tt * 128:(tt + 1) * 128],
                            rhs=bd[:, cs, :], start=True, stop=True), r=[fb, bd], w=[ps])
                if tt % 2 == 0:
                    k.op("act", lambda e, ps=ps, tt=tt: e.activation(out=G[:, tt, :], in_=ps[:, :], func=AF.Copy), r=[ps], w=[G])
                else:
                    k.op("dve", lambda e, ps=ps, tt=tt: e.tensor_copy(out=G[:, tt, :], in_=ps[:, :]), r=[ps], w=[G])
            for tp in range(L // 256):
                cbt, sbt = cb[tp % 2], sbf[tp % 2]
                k.dma("sp", cbt[:, :ntt, :], csrc[0, tp].rearrange("p (tt n) -> p tt n", n=256), w=[cbt])
                k.dma("sp", sbt[:, :ntt, :], csrc[1, tp].rearrange("p (tt n) -> p tt n", n=256), w=[sbt])
                for cc in range(2):
                    ps = g.ps[2 + it % 2]
                    y_ = yo[it % 2]
                    for tt in range(ntt):
                        k.op("pe", lambda e, ps=ps, cc=cc, tt=tt, cbt=cbt: e.matmul(
                            ps[:, :256], lhsT=G[:, tt, cc * 128:(cc + 1) * 128], rhs=cbt[:, tt, :],
                            start=(tt == 0), stop=False), r=[G, cbt], w=[ps])
                        k.op("pe", lambda e, ps=ps, cc=cc, tt=tt, sbt=sbt: e.matmul(
                            ps[:, :256], lhsT=G[:, tt, (2 + cc) * 128:(3 + cc) * 128], rhs=sbt[:, tt, :],
                            start=False, stop=(tt == ntt - 1)), r=[G, sbt], w=[ps])
                    k.op("act", lambda e, ps=ps, y_=y_: e.activation(out=y_[:], in_=ps[:, :256], func=AF.Copy), r=[ps], w=[y_])
                    k.dma("sp", g.ymix[768 + cc * 128:768 + (cc + 1) * 128, c0 + tp * 256:c0 + (tp + 1) * 256], y_[:],
                          r=[y_], w=[g.ymix_r])
                    it += 1
        k.epoch()


SEG = 512
NCH = TT // 64
DEC = -0.6065306597126334


def rwkv_segments(d):
    segs = [(0, CTX)] + [(CTX + i * SEG, SEG) for i in range(SEQ // SEG)]
    if d == 1:
        segs = [segs[0]] + segs[:0:-1]
    return segs


def phase_rwkv(g, l, last):
    nc, k = g.nc, g.k
    with ExitStack() as ph:
        sb = lambda n, s, d: T(ph.enter_context(nc.sbuf_tensor(uname(n), s, d)))
        par = sb("rpar", [64, 8, 2, 8], F32)
        omka = sb("omka", [64, 8], F32)
        lmu = sb("lmu", [64, 2, 2], F32)
        lora = sb("lora", [64, 2, 2, 512], F32)
        g2 = sb("g2w", [128, 512], F32)
        gn = sb("gnw", [64, 2, 512], F32)
        mask = sb("mask", [64, 2, 320], F32)
        ident = sb("ident", [64, 64], F32)
        ones64 = sb("ones64", [64, 64], F32)
        rmask = sb("rmask", [64, SEG], F32)
        sgl = sb("sgl", [128, TT], F32)
        k.dma("sp", par[:], g.i["rw_par"][l], w=[par])
        k.dma("sp", lmu[:], g.i["rw_lmu"][l], w=[lmu])
        k.dma("sp", lora[:], g.i["rw_lora"][l], w=[lora])
        k.dma("sp", g2[:], g.i["rw_g2"][l], w=[g2])
        k.dma("sp", gn[:], g.i["rw_gn"][l], w=[gn])
        k.dma("sp", mask[:], g.i["masks"], w=[mask])
        k.dma("sp", ident[:], g.i["ident"][0:64, 0:64], w=[ident])
        k.dma("sp", rmask[:], g.i["rmask"], w=[rmask])
        k.op("pool", lambda e: e.memset(ones64[:], 1.0), w=[ones64])
        k.op("dve", lambda e: e.tensor_scalar(out=omka[:], in0=par[:, :, 0, 6], scalar1=-1.0, scalar2=1.0,
                                              op0=ALU.mult, op1=ALU.add), r=[par], w=[omka])
        k.dma("sp", sgl[:], g.zT[O_G:O_G + 128, :], r=[g.zT_r], w=[sgl])
        k.op("act", lambda e: e.activation(out=sgl[:], in_=sgl[:], func=AF.Sigmoid), r=[sgl], w=[sgl])
        hz = {n: sb("hz_" + n, [64, SEG + 2], F32) for n in ("k", "v", "r", "w", "a")}
        wt0 = {n: sb("wt_" + n, [64, SEG], F32) for n in
               ("kl", "vl", "rl", "wl", "al", "sw", "sa", "kk", "t0", "t1", "kkn", "km", "bv", "Lp", "Ex", "En",
                "BT", "KT", "rk")}
        wt = wt0
        NC8 = SEG // 64
        Eis = [sb("Ei%d" % i, [64, SEG], F32) for i in range(3)]
        ARs = [sb("AR%d" % i, [64, NC8 * 128], F32) for i in range(3)]
        BTs = [wt0["BT"], sb("BTb", [64, SEG], F32)]
        KTs = [wt0["KT"], sb("KTb", [64, SEG], F32)]
        VLs = [wt0["vl"], sb("vlb", [64, SEG], F32)]
        RKs = [wt0["rk"], sb("rkb", [64, SEG], F32)]
        T3s = [sb("T3_%d" % i, [64, NC8, 4, 64], F32) for i in range(2)]
        ABKs = [sb("ABK%d" % i, [64, NC8, 256], F32) for i in range(2)]
        Wbs = [sb("Wb%d" % i, [64, NC8, 64], F32) for i in range(2)]
        N0 = sb("N0", [64, NC8, 64], F32)
        MNb = [sb("MNb%d" % i, [64, NC8, 128], F32) for i in range(2)]
        X = sb("Xs", [64, 64], F32)
        HP = sb("HPs", [64, 64], F32)
        U = sb("Us", [64, 64], F32)
        Hs = [sb("Hs%d" % i, [64, 64], F32) for i in range(2)]
        Yd = sb("Yd", [64, NCH, 64], F32)
        Ysum = sb("Ysum", [64, NCH, 64], F32)
        st1 = sb("st1", [64, SEG // 64], F32)
        st2 = sb("st2", [64, SEG // 64], F32)
        ysq = sb("ysq", [64, SEG // 64, 64], F32)
        yfm = sb("yfm", [64, TT], BF16)
        pL = [g.ps[0], g.ps[1]]
        pN = g.ps[2]
        pB = [g.ps[3], g.ps[4]]
        pC = g.ps[5]
        pCh = g.ps[6]
        pH = g.ps[7]
        RX = RU = RH = RY = pCh
        RG = pN
        hi_ = 0

        def lerp(dst, src, S, d, mu_ap):
            cur = src[:, 1:S + 1]
            sh = src[:, 0:S] if d == 0 else src[:, 2:S + 2]
            k.op("pool", lambda e: e.tensor_tensor(out=wt["t0"][:, :S], in0=sh, in1=cur, op=ALU.subtract),
                 r=[src], w=[wt["t0"]])
            k.op("dve", lambda e: e.scalar_tensor_tensor(out=dst[:, :S], in0=wt["t0"][:, :S], scalar=mu_ap, in1=cur,
                                                         op0=ALU.mult, op1=ALU.add), r=[wt["t0"], src, lmu, par], w=[dst])

        tot = sb("tot", [64, SEG // 64], F32)
        st = {"hcur": 0}

        def stageB1(h, d, c0, S, b2, b3):
            wt = dict(wt0)
            wt.update(BT=BTs[b2], KT=KTs[b2], vl=VLs[b2], rk=RKs[b2])
            wt["Ei"] = Eis[b3]
            AR = ARs[b3]
            lo, hi = (0, CTX) if c0 < CTX else (CTX, TT)
            need_y = not (last and c0 < CTX)
            nch = S // 64
            rows = {"k": O_K + h * 64, "v": O_V + h * 64, "r": O_R + h * 64,
                    "w": (O_WF, O_WB)[d], "a": (O_AF, O_AB)[d]}
            for n_, t_ in hz.items():
                k.op("pool", lambda e, t_=t_: e.memset(t_[:], 0.0), w=[t_])
                a_, b_ = max(c0 - 1, lo), min(c0 + S + 1, hi)
                k.dma("sp", t_[:, a_ - (c0 - 1):b_ - (c0 - 1)], g.zT[rows[n_]:rows[n_] + 64, a_:b_],
                      r=[g.zT_r], w=[t_])
            P = lambda j: par[:, h, d, j:j + 1]
            lerp(wt["kl"], hz["k"], S, d, P(0))
            lerp(wt["vl"], hz["v"], S, d, P(1))
            lerp(wt["rl"], hz["r"], S, d, P(2))
            lerp(wt["wl"], hz["w"], S, d, lmu[:, d, 0:1])
            lerp(wt["al"], hz["a"], S, d, lmu[:, d, 1:2])
            k.op("act", lambda e: e.activation(out=wt["wl"][:, :S], in_=wt["wl"][:, :S], func=AF.Tanh),
                 r=[wt["wl"]], w=[wt["wl"]])
            for c5 in range(0, S, 512):
                n5 = min(512, S - c5)
                for (src, dst, li, bj) in ((wt["wl"], wt["sw"], 0, 3), (wt["al"], wt["sa"], 1, 4)):
                    k.op("pe", lambda e, src=src, li=li, c5=c5, n5=n5: e.matmul(
                        pH[0:64, :n5], lhsT=lora[:, d, li, h * 64:(h + 1) * 64], rhs=src[:, c5:c5 + n5],
                        start=True, stop=True), r=[lora, src], w=[pH])
                    k.op("act", lambda e, dst=dst, bj=bj, c5=c5, n5=n5: e.activation(
                        out=dst[:, c5:c5 + n5], in_=pH[0:64, :n5], func=AF.Sigmoid, bias=P(bj)), r=[pH, par], w=[dst])
            k.op("dve", lambda e: e.tensor_scalar(out=wt["kk"][:, :S], in0=wt["kl"][:, :S], scalar1=P(5), scalar2=None,
                                                  op0=ALU.mult), r=[wt["kl"], par], w=[wt["kk"]])
            k.op("pool", lambda e: e.tensor_tensor(out=wt["t0"][:, :S], in0=wt["kk"][:, :S], in1=wt["kk"][:, :S],
                                                   op=ALU.mult), r=[wt["kk"]], w=[wt["t0"]])
            for c5 in range(0, S, 512):
                n5 = min(512, S - c5)
                k.op("pe", lambda e, c5=c5, n5=n5: e.matmul(pH[0:64, :n5], lhsT=ones64[:], rhs=wt["t0"][:, c5:c5 + n5],
                                                           start=True, stop=True), r=[ones64, wt["t0"]], w=[pH])
                k.op("act", lambda e, c5=c5, n5=n5: e.activation(out=wt["t1"][:, c5:c5 + n5], in_=pH[0:64, :n5],
                                                                 func=AF.Sqrt), r=[pH], w=[wt["t1"]])
            k.op("dve", lambda e: e.tensor_scalar(out=wt["t1"][:, :S], in0=wt["t1"][:, :S], scalar1=1e-12, scalar2=None,
                                                  op0=ALU.max), r=[wt["t1"]], w=[wt["t1"]])
            k.op("dve", lambda e: e.reciprocal(out=wt["t1"][:, :S], in_=wt["t1"][:, :S]), r=[wt["t1"]], w=[wt["t1"]])
            k.op("pool", lambda e: e.tensor_tensor(out=wt["kkn"][:, :S], in0=wt["kk"][:, :S], in1=wt["t1"][:, :S],
                                                   op=ALU.mult), r=[wt["kk"], wt["t1"]], w=[wt["kkn"]])
            k.op("dve", lambda e: e.tensor_scalar(out=wt["t1"][:, :S], in0=wt["sa"][:, :S], scalar1=P(6),
                                                  scalar2=omka[:, h:h + 1], op0=ALU.mult, op1=ALU.add),
                 r=[wt["sa"], par, omka], w=[wt["t1"]])
            k.op("pool", lambda e: e.tensor_tensor(out=wt["km"][:, :S], in0=wt["kl"][:, :S], in1=wt["t1"][:, :S],
                                                   op=ALU.mult), r=[wt["kl"], wt["t1"]], w=[wt["km"]])
            k.op("pool", lambda e: e.tensor_tensor(out=wt["bv"][:, :S], in0=wt["kkn"][:, :S], in1=wt["sa"][:, :S],
                                                   op=ALU.mult), r=[wt["kkn"], wt["sa"]], w=[wt["bv"]])
            k.op("dve", lambda e: e.tensor_tensor_scan(out=wt["Lp"][:, :S], data0=rmask[:, :S], data1=wt["sw"][:, :S],
                                                       initial=0.0, op0=ALU.mult, op1=ALU.add),
                 r=[rmask, wt["sw"]], w=[wt["Lp"]])
            if d == 1:
                lp3 = wt["Lp"][:, :S].rearrange("p (c t) -> p c t", t=64)
                sw3 = wt["sw"][:, :S].rearrange("p (c t) -> p c t", t=64)
                t03 = wt["t0"][:, :S].rearrange("p (c t) -> p c t", t=64)
                k.op("pool", lambda e: e.tensor_tensor(out=wt["t0"][:, :S], in0=wt["sw"][:, :S], in1=wt["Lp"][:, :S],
                                                       op=ALU.subtract), r=[wt["sw"], wt["Lp"]], w=[wt["t0"]])
                k.op("dve", lambda e: e.tensor_copy(out=tot[:, :nch], in_=wt["Lp"][:, :S].rearrange("p (c t) -> p c t", t=64)[:, :, 63]),
                     r=[wt["Lp"]], w=[tot])
                k.op("dve", lambda e: e.tensor_tensor(out=lp3, in0=t03, in1=tot[:, :nch].unsqueeze(2).to_broadcast([64, nch, 64]),
                                                      op=ALU.add), r=[wt["t0"], tot], w=[wt["Lp"]])
            k.op("act", lambda e: e.activation(out=wt["Ei"][:, :S], in_=wt["Lp"][:, :S], func=AF.Exp, scale=DEC),
                 r=[wt["Lp"]], w=[wt["Ei"]])
            k.op("act", lambda e: e.activation(out=wt["En"][:, :S], in_=wt["Lp"][:, :S], func=AF.Exp, scale=-DEC),
                 r=[wt["Lp"]], w=[wt["En"]])
            k.op("pool", lambda e: e.tensor_tensor(out=wt["t0"][:, :S], in0=wt["Lp"][:, :S], in1=wt["sw"][:, :S],
                                                   op=ALU.subtract), r=[wt["sw"], wt["Lp"]], w=[wt["t0"]])
            k.op("act", lambda e: e.activation(out=wt["Ex"][:, :S], in_=wt["t0"][:, :S], func=AF.Exp, scale=DEC),
                 r=[wt["t0"]], w=[wt["Ex"]])
            ar4 = AR[:, :nch * 128].rearrange("p (c j t) -> p c j t", j=2, t=64)
            v3 = lambda t_: t_[:, :S].rearrange("p (c t) -> p c t", t=64)
            k.op("dve", lambda e: e.scalar_tensor_tensor(out=ar4[:, :, 0, :], in0=v3(wt["kkn"]), scalar=-1.0,
                                                         in1=v3(wt["Ex"]), op0=ALU.mult, op1=ALU.mult),
                 r=[wt["kkn"], wt["Ex"]], w=[AR])
            k.op("pool", lambda e: e.tensor_tensor(out=ar4[:, :, 1, :], in0=v3(wt["rl"]), in1=v3(wt["Ei"]), op=ALU.mult),
                 r=[wt["rl"], wt["Ei"]], w=[AR])
            k.op("dve", lambda e: e.tensor_tensor(out=wt["BT"][:, :S], in0=wt["bv"][:, :S], in1=wt["En"][:, :S], op=ALU.mult),
                 r=[wt["bv"], wt["En"]], w=[wt["BT"]])
            k.op("pool", lambda e: e.tensor_tensor(out=wt["KT"][:, :S], in0=wt["km"][:, :S], in1=wt["En"][:, :S], op=ALU.mult),
                 r=[wt["km"], wt["En"]], w=[wt["KT"]])
            if need_y:
                k.op("dve", lambda e: e.scalar_tensor_tensor(out=wt["rk"][:, :S], in0=wt["rl"][:, :S], scalar=P(7),
                                                             in1=wt["km"][:, :S], op0=ALU.mult, op1=ALU.mult),
                     r=[wt["rl"], wt["km"], par], w=[wt["rk"]])

        def stageB2(h, d, c0, S, b2, b3, buf):
            wt = dict(wt0)
            wt.update(BT=BTs[b2], KT=KTs[b2], vl=VLs[b2], rk=RKs[b2])
            wt["Ei"] = Eis[b3]
            AR = ARs[b3]
            T3, ABK, Wb = T3s[buf], ABKs[buf], Wbs[buf]
            need_y = not (last and c0 < CTX)
            nch = S // 64
            for c in range(nch if g.rw_stage >= 1 else 0):
                cs = slice(c * 64, (c + 1) * 64)
                for j, src in enumerate((wt["BT"], wt["KT"], wt["vl"])):
                    k.op("pe", lambda e, j=j, src=src, cs=cs: e.matmul(pN[0:64, j * 64:(j + 1) * 64], lhsT=src[:, cs], rhs=ident[:], start=True, stop=True),
                         r=[src, ident], w=[RG])
                if need_y:
                    k.op("pe", lambda e, cs=cs: e.matmul(pN[0:64, 192:256], lhsT=wt["rk"][:, cs], rhs=ones64[:],
                                                         start=True, stop=True), r=[wt["rk"], ones64], w=[RG])
                nj = 4 if need_y else 3
                k.op("dve", lambda e, c=c, nj=nj: e.tensor_copy(out=T3[:, c, 0:nj, :], in_=pN[0:64, 0:nj * 64].rearrange("p (j t) -> p j t", j=nj)),
                     r=[RG], w=[T3])
            if g.rw_stage >= 2:
                ar_of = lambda c: (AR[:, c * 128:c * 128 + 64], AR[:, c * 128 + 64:c * 128 + 128], AR[:, c * 128:c * 128 + 128])
                for h0 in range(0, nch, 4):
                    for c in range(h0, min(h0 + 4, nch)):
                        cs = slice(c * 64, (c + 1) * 64)
                        aT, rT, arT = ar_of(c)
                        pl = pL[(c % 4) // 2]
                        o0 = (c % 2) * 256
                        k.op("pe", lambda e, pl=pl, o0=o0, cs=cs, arT=arT: e.matmul(pl[0:64, o0:o0 + 128], lhsT=wt["BT"][:, cs], rhs=arT, start=True, stop=True),
                             r=[wt["BT"], AR], w=[pl])
                        k.op("pe", lambda e, pl=pl, o0=o0, cs=cs, arT=arT: e.matmul(pl[0:64, o0 + 128:o0 + 256], lhsT=wt["KT"][:, cs], rhs=arT, start=True, stop=True),
                             r=[wt["KT"], AR], w=[pl])
                        k.op("pe", lambda e, c=c, cs=cs, aT=aT: e.matmul(pN[0:64, c * 64:(c + 1) * 64], lhsT=aT, rhs=wt["BT"][:, cs], start=True, stop=True),
                             r=[wt["BT"], AR], w=[pN])
                    for j2 in range(2):
                        cA = h0 + j2 * 2
                        if cA >= nch:
                            continue
                        pl = pL[j2]
                        k.op("dve", lambda e, pl=pl, cA=cA: e.tensor_tensor(
                            out=ABK[:, cA:cA + 2, :], in0=pl[0:64, 0:512].rearrange("p (c x) -> p c x", c=2),
                            in1=mask[:, d, 0:256].unsqueeze(1).to_broadcast([64, 2, 256]), op=ALU.mult), r=[pl, mask], w=[ABK])
                k.op("dve", lambda e: e.tensor_tensor(out=N0[:, :nch, :], in0=pN[0:64, 0:nch * 64].rearrange("p (c x) -> p c x", x=64),
                                                      in1=mask[:, d, 256:320].unsqueeze(1).to_broadcast([64, nch, 64]), op=ALU.mult),
                     r=[pN, mask], w=[N0])
                k.op("pool", lambda e: e.tensor_tensor(out=Wb[:, :nch, :], in0=ABK[:, :nch, 0:64],
                                                       in1=ident[:].unsqueeze(1).to_broadcast([64, nch, 64]), op=ALU.add),
                     r=[ABK, ident], w=[Wb])
                for j in range(5):
                    lastj = j == 4
                    mn = MNb[j % 2]
                    for c in range(nch):
                        if j == 0:
                            Mj, Nj, Mr, Nr = ABK[:, c, 0:64], N0[:, c, :], ABK, N0
                        else:
                            pm = MNb[(j - 1) % 2]
                            Mj, Nj, Mr, Nr = pm[:, c, 0:64], pm[:, c, 64:128], pm, pm
                        pb = pB[c // 4]
                        o0 = (c % 4) * 128
                        if not lastj:
                            k.op("pe", lambda e, Mj=Mj, Nj=Nj, pb=pb, o0=o0: e.matmul(pb[0:64, o0:o0 + 64], lhsT=Nj, rhs=Mj, start=True, stop=True),
                                 r=[Mr, Nr], w=[pb])
                        k.op("pe", lambda e, Mj=Mj, Nj=Nj, pb=pb, o0=o0: e.matmul(pb[0:64, o0 + 64:o0 + 128], lhsT=Mj, rhs=Nj, start=True, stop=True),
                             r=[Mr, Nr], w=[pb])
                    for b4 in range((nch + 3) // 4):
                        n4 = min(4, nch - b4 * 4)
                        k.op("act", lambda e, b4=b4, n4=n4, mn=mn: e.activation(
                            out=mn[:, b4 * 4:b4 * 4 + n4, :], in_=pB[b4][0:64, 0:n4 * 128].rearrange("p (c x) -> p c x", x=128), func=AF.Copy),
                            r=[pB[b4]], w=[mn])
                    for c in range(nch):
                        k.op("pe", lambda e, c=c, mn=mn: e.matmul(pC[0:64, c * 64:(c + 1) * 64], lhsT=mn[:, c, 64:128], rhs=Wb[:, c, :], start=True, stop=True),
                             r=[mn, Wb], w=[pC])
                    wo_ = Wb
                    k.op("dve", lambda e, wo_=wo_: e.tensor_tensor(out=wo_[:, :nch, :], in0=pC[0:64, 0:nch * 64].rearrange("p (c x) -> p c x", x=64),
                                                                   in1=Wb[:, :nch, :], op=ALU.add), r=[pC, Wb], w=[wo_])

        def stageA(h, d, c0, S, buf, b3, first, last_ser, last_head):
            AR, T3, ABK, Wb = ARs[b3], T3s[buf], ABKs[buf], Wbs[buf]
            wt = dict(wt0)
            wt["Ei"] = Eis[b3]
            need_y = not (last and c0 < CTX)
            nch = S // 64
            ar_of = lambda c: (AR[:, c * 128:c * 128 + 64], AR[:, c * 128 + 64:c * 128 + 128], AR[:, c * 128:c * 128 + 128])
            if first:
                k.op("pool", lambda e: e.memset(Hs[0][:], 0.0), w=[Hs[0]])
                st["hcur"] = 0
            hcur = st["hcur"]
            if g.rw_stage >= 2:
                pass
            corder = range(nch) if d == 0 else range(nch - 1, -1, -1)
            for c in corder:
                aT, rT, arT = ar_of(c)
                Hn = Hs[1 - hcur]
                H = Hs[hcur]
                gc = c0 // 64 + c
                pcol = c * 64 + (63 if d == 0 else 0)
                k.op("dve", lambda e, H=H, pcol=pcol: e.tensor_scalar(out=HP[:], in0=H[:], scalar1=wt["Ei"][:, pcol:pcol + 1], scalar2=None, op0=ALU.mult),
                     r=[H, wt["Ei"]], w=[HP])
                k.op("pe", lambda e, H=H, aT=aT: e.matmul(pCh[0:64, 0:64], lhsT=aT, rhs=H[:], start=True, stop=False),
                     r=[AR, H], w=[RX])
                k.op("pe", lambda e, c=c: e.matmul(pCh[0:64, 0:64], lhsT=ABK[:, c, 128:192], rhs=T3[:, c, 2, :], start=False, stop=True),
                     r=[ABK, T3], w=[RX])
                k.op("act", lambda e: e.activation(out=X[:], in_=pCh[0:64, 0:64], func=AF.Copy), r=[RX], w=[X])
                k.op("pe", lambda e, c=c: e.matmul(pCh[0:64, 64:128], lhsT=Wb[:, c, :], rhs=X[:], start=True, stop=True),
                     r=[Wb, X], w=[RU])
                k.op("dve", lambda e: e.tensor_copy(out=U[:], in_=pCh[0:64, 64:128]), r=[RU], w=[U])
                k.op("pe", lambda e, c=c: e.matmul(pCh[0:64, 192:256], lhsT=T3[:, c, 0, :], rhs=U[:], start=True, stop=False),
                     r=[T3, U], w=[RH])
                k.op("pe", lambda e, c=c: e.matmul(pCh[0:64, 192:256], lhsT=T3[:, c, 1, :], rhs=T3[:, c, 2, :], start=False, stop=True),
                     r=[T3], w=[RH])
                pcol = c * 64 + (63 if d == 0 else 0)
                k.op("dve", lambda e, Hn=Hn, pcol=pcol: e.scalar_tensor_tensor(out=Hn[:], in0=pCh[0:64, 192:256], scalar=wt["Ei"][:, pcol:pcol + 1],
                                                                                in1=HP[:], op0=ALU.mult, op1=ALU.add),
                     r=[RH, wt["Ei"], HP], w=[Hn])
                if need_y:
                    k.op("pe", lambda e, H=H, rT=rT: e.matmul(pCh[0:64, 128:192], lhsT=rT, rhs=H[:], start=True, stop=False),
                         r=[AR, H], w=[RY])
                    k.op("pe", lambda e, c=c: e.matmul(pCh[0:64, 128:192], lhsT=ABK[:, c, 64:128], rhs=U[:], start=False, stop=False),
                         r=[ABK, U], w=[RY])
                    k.op("pe", lambda e, c=c: e.matmul(pCh[0:64, 128:192], lhsT=ABK[:, c, 192:256], rhs=T3[:, c, 2, :], start=False, stop=True),
                         r=[ABK, T3], w=[RY])
                    k.op("act", lambda e, gc=gc: e.activation(out=Yd[:, gc, :], in_=pCh[0:64, 128:192], func=AF.Copy),
                         r=[RY], w=[Yd])
                hcur = 1 - hcur
            st["hcur"] = hcur
            if need_y and g.rw_stage >= 3:
                g0 = c0 // 64
                yv = Yd[:, g0:g0 + nch, :]
                bc = lambda ap: ap.unsqueeze(2).to_broadcast([64, nch, 64])
                k.op("dve", lambda e: e.tensor_reduce(out=st1[:, :nch], in_=yv, axis=AX.X, op=ALU.add), r=[Yd], w=[st1])
                k.op("pool", lambda e: e.tensor_tensor(out=ysq[:, :nch, :], in0=yv, in1=yv, op=ALU.mult), r=[Yd], w=[ysq])
                k.op("dve", lambda e: e.tensor_reduce(out=st2[:, :nch], in_=ysq[:, :nch, :], axis=AX.X, op=ALU.add), r=[ysq], w=[st2])
                k.op("dve", lambda e: e.tensor_scalar(out=st1[:, :nch], in0=st1[:, :nch], scalar1=1.0 / 64, scalar2=None, op0=ALU.mult),
                     r=[st1], w=[st1])
                k.op("dve", lambda e: e.scalar_tensor_tensor(out=ysq[:, :nch, 0], in0=st1[:, :nch], scalar=-1.0, in1=st1[:, :nch],
                                                             op0=ALU.mult, op1=ALU.mult), r=[st1], w=[ysq])
                k.op("dve", lambda e: e.scalar_tensor_tensor(out=st2[:, :nch], in0=st2[:, :nch], scalar=1.0 / 64, in1=ysq[:, :nch, 0],
                                                             op0=ALU.mult, op1=ALU.add), r=[st2, ysq], w=[st2])
                k.op("act", lambda e: e.activation(out=st2[:, :nch], in_=st2[:, :nch], func=AF.Sqrt, bias=g.gneps_t[0:64, :]),
                     r=[st2, g.gneps_t], w=[st2])
                k.op("dve", lambda e: e.reciprocal(out=st2[:, :nch], in_=st2[:, :nch]), r=[st2], w=[st2])
                k.op("dve", lambda e: e.tensor_tensor(out=yv, in0=yv, in1=bc(st1[:, :nch]), op=ALU.subtract), r=[Yd, st1], w=[Yd])
                k.op("dve", lambda e: e.tensor_tensor(out=yv, in0=yv, in1=bc(st2[:, :nch]), op=ALU.mult), r=[Yd, st2], w=[Yd])
                gb = lambda j: gn[:, j, h * 64:(h + 1) * 64].unsqueeze(1).to_broadcast([64, nch, 64])
                k.op("pool", lambda e: e.tensor_tensor(out=yv, in0=yv, in1=gb(0), op=ALU.mult), r=[Yd, gn], w=[Yd])
                k.op("pool", lambda e: e.tensor_tensor(out=yv, in0=yv, in1=gb(1), op=ALU.add), r=[Yd, gn], w=[Yd])
                k.op("pool", lambda e: e.tensor_tensor(out=ysq[:, :nch, :], in0=T3[:, :nch, 2, :], in1=T3[:, :nch, 3, :], op=ALU.mult),
                     r=[T3], w=[ysq])
                ys = Ysum[:, g0:g0 + nch, :]
                if d == 0:
                    k.op("dve", lambda e: e.tensor_tensor(out=ys, in0=yv, in1=ysq[:, :nch, :], op=ALU.add), r=[Yd, ysq], w=[Ysum])
                else:
                    k.op("dve", lambda e: e.tensor_tensor(out=yv, in0=yv, in1=ysq[:, :nch, :], op=ALU.add), r=[Yd, ysq], w=[Yd])
                    k.op("pool", lambda e: e.tensor_tensor(out=ys, in0=ys, in1=yv, op=ALU.add), r=[Yd, Ysum], w=[Ysum])
            if last_ser and g.dbg_state is not None:
                k.dma("sp", g.dbg_state[l, d, h], Hs[hcur][:], r=[Hs[hcur]])
            if last_head:
                pass
            if last_head and g.rw_stage >= 4:
                for c8 in range(0, NCH, 8):
                    n8 = min(8, NCH - c8)
                    for c in range(n8):
                        gc = c8 + c
                        k.op("pe", lambda e, c=c, gc=gc: e.matmul(pCh[0:64, c * 64:(c + 1) * 64], lhsT=sgl[:, gc * 64:(gc + 1) * 64],
                                                                  rhs=g2[:, h * 64:(h + 1) * 64], start=True, stop=True),
                             r=[sgl, g2], w=[pCh])
                    ysl = Ysum[:, c8:c8 + n8, :]
                    k.op("dve", lambda e, ysl=ysl, n8=n8: e.tensor_tensor(out=ysl, in0=ysl, in1=pCh[0:64, 0:n8 * 64].rearrange("p (c t) -> p c t", t=64),
                                                                          op=ALU.mult), r=[Ysum, pCh], w=[Ysum])
                    for c in range(n8):
                        gc = c8 + c
                        k.op("pe", lambda e, c=c, gc=gc: e.matmul(pCh[0:64, c * 64:(c + 1) * 64], lhsT=Ysum[:, gc, :], rhs=ident[:], start=True, stop=True),
                             r=[Ysum, ident], w=[pCh])
                    k.op("act", lambda e, c8=c8, n8=n8: e.activation(out=yfm[:, c8 * 64:(c8 + n8) * 64], in_=pCh[0:64, 0:n8 * 64], func=AF.Copy),
                         r=[pCh], w=[yfm])
                k.dma("sp", g.ymix[h * 64:(h + 1) * 64, :], yfm[:], r=[yfm], w=[g.ymix_r])

        jobs = []
        for h in g.heads:
            for d in range(2):
                segs = rwkv_segments(d)[:g.rw_maxseg]
                for si, (c0, S) in enumerate(segs):
                    n_ = len(jobs)
                    jobs.append(dict(h=h, d=d, c0=c0, S=S, buf=n_ % 2, b3=n_ % 3, first=(si == 0),
                                     last_ser=(si == len(segs) - 1), last_head=(d == 1 and si == len(segs) - 1)))
        J = len(jobs)
        for r_ in range(J + 2):
            fns, quota = [], []
            ja = jobs[r_ - 2] if 0 <= r_ - 2 < J else None
            jb = jobs[r_ - 1] if 0 <= r_ - 1 < J else None
            jc = jobs[r_] if r_ < J else None
            if ja is not None:
                fns.append(lambda p_=ja: stageA(p_["h"], p_["d"], p_["c0"], p_["S"], p_["buf"], p_["b3"], p_["first"],
                                                 p_["last_ser"], p_["last_head"]))
                quota.append(2)
            if jb is not None:
                fns.append(lambda p_=jb: stageB2(p_["h"], p_["d"], p_["c0"], p_["S"], p_["buf"], p_["b3"], p_["buf"]))
                quota.append(3)
            if jc is not None:
                fns.append(lambda p_=jc: stageB1(p_["h"], p_["d"], p_["c0"], p_["S"], p_["buf"], p_["b3"]))
                quota.append(1)
            weave(k, fns, quota)
            if ja is not None and ja["last_head"]:
                k.epoch()


def phase_wout(g, l):
    nc, k = g.nc, g.k
    with ExitStack() as ph:
        sb = lambda n, s, d: T(ph.enter_context(nc.sbuf_tensor(uname(n), s, d)))
        wo = sb("wo", [128, 8, D], BF16)
        wsrc = g.i["w_out"][l].rearrange("(kc p) n -> p kc n", p=128)
        for kc in range(8):
            k.dma("pool", wo[:, kc, :], wsrc[:, kc, :], w=[wo])
        yms = [sb("ym%d" % i, [128, 8, 512], BF16) for i in range(2)]
        xts = [sb("fx%d" % i, [128, 8, 512], F32) for i in range(2)]
        o = sb("fo", [128, 8, 512], F32)
        sq = sb("fsq", [128, 8, 512], BF16)
        h2f = sb("h2f", [128, 8, 512], F32)
        h2b = sb("h2b", [128, 8, 512], BF16)
        rstd = sb("frstd", [128, 512], F32)
        tmp = sb("ftmp", [128, 512], F32)
        tmp2 = [sb("ftmp2_%d" % i, [128, 512], F32) for i in range(2)]
        xv = g.xres.rearrange("(dc p) t -> p dc t", p=128)
        ymv = g.ymix.rearrange("(dc p) t -> p dc t", p=128)
        h2v = g.h2T.rearrange("(dc p) t -> p dc t", p=128)
        h2fv = g.h2F.rearrange("(dc p) t -> p dc t", p=128)
        zi = 0
        for ti, (c0, tn, wh) in enumerate(token_tiles()):
            if l == 1 and wh == 1:
                continue
            ym, xt = yms[ti % 2], xts[ti % 2]
            k.dma("sp", ym[:, :, :tn], ymv[:, :, c0:c0 + tn], r=[g.ymix_r], w=[ym])
            k.dma("sp", xt[:, :, :tn], xv[:, :, c0:c0 + tn], r=[g.xres_r], w=[xt])
            for dc in range(8):
                ps = g.ps[2 + (zi % 4)]
                zi += 1
                for kc in range(8):
                    k.op("pe", lambda e, ps=ps, kc=kc, dc=dc, ym=ym: e.matmul(
                        ps[:, :tn], lhsT=wo[:, kc, dc * 128:(dc + 1) * 128], rhs=ym[:, kc, :tn],
                        start=(kc == 0), stop=(kc == 7)), r=[wo, ym], w=[ps])
                if dc % 2 == 0:
                    k.op("act", lambda e, ps=ps, dc=dc: e.activation(out=o[:, dc, :tn], in_=ps[:, :tn], func=AF.Copy), r=[ps], w=[o])
                else:
                    k.op("dve", lambda e, ps=ps, dc=dc: e.tensor_copy(out=o[:, dc, :tn], in_=ps[:, :tn]), r=[ps], w=[o])
            rms_rstd(g, o, tn, sq, g.ps[1], rstd, tmp)
            for dc in range(8):
                t2 = tmp2[dc % 2]
                k.op("dve", lambda e, dc=dc, t2=t2: e.scalar_tensor_tensor(
                    out=t2[:, :tn], in0=o[:, dc, :tn], scalar=g.modv[:, 2, dc, wh:wh + 1], in1=rstd[:, :tn],
                    op0=ALU.mult, op1=ALU.mult), r=[o, rstd, g.modv], w=[t2])
                k.op("pool", lambda e, dc=dc, t2=t2, xt=xt: e.tensor_tensor(out=xt[:, dc, :tn], in0=xt[:, dc, :tn], in1=t2[:, :tn],
                                                                           op=ALU.add), r=[t2, xt], w=[xt])
            k.dma("sp", xv[:, :, c0:c0 + tn], xt[:, :, :tn], r=[xt], w=[g.xres_r])
            rms_rstd(g, xt, tn, sq, g.ps[1], rstd, tmp)
            for dc in range(8):
                t2 = tmp2[dc % 2]
                k.op("dve", lambda e, dc=dc, t2=t2, xt=xt: e.scalar_tensor_tensor(
                    out=t2[:, :tn], in0=xt[:, dc, :tn], scalar=g.modv[:, 3, dc, wh:wh + 1], in1=rstd[:, :tn],
                    op0=ALU.mult, op1=ALU.mult), r=[xt, rstd, g.modv], w=[t2])
                k.op("act", lambda e, dc=dc, t2=t2: e.activation(
                    out=h2f[:, dc, :tn], in_=t2[:, :tn], func=AF.Identity, bias=g.modv[:, 4, dc, wh:wh + 1]),
                    r=[t2, g.modv], w=[h2f])
            k.op("pool", lambda e: e.tensor_copy(out=h2b[:, :, :tn], in_=h2f[:, :, :tn]), r=[h2f], w=[h2b])
            k.dma("sp", h2v[:, :, c0:c0 + tn], h2b[:, :, :tn], r=[h2b], w=[g.h2T_r])
            if l == 1:
                k.dma("sp", h2fv[:, :, c0:c0 + tn], h2f[:, :, :tn], r=[h2f], w=[g.h2F_r])
        k.epoch()


def phase_router(g, l):
    nc, k = g.nc, g.k
    with ExitStack() as ph:
        sb = lambda n, s, d: T(ph.enter_context(nc.sbuf_tensor(uname(n), s, d)))
        rw = sb("rw", [128, 8, 8], F32)
        rb = sb("rb", [128, 4, 8], F32)
        idn = sb("idn", [128, 128], F32)
        k.dma("sp", rw[:], g.i["router_w"], w=[rw])
        k.dma("sp", rb[:], g.i["router_b"], w=[rb])
        k.dma("sp", idn[:], g.i["ident"], w=[idn])
        hf = [sb("rhf%d" % i, [128, 8, 512], F32) for i in range(2)]
        lg = sb("lg", [128, 4, 8], F32)
        lg2 = sb("lg2", [128, 4, 8], F32)
        eq = sb("eq", [128, 4, 8], F32)
        m1 = sb("m1", [128, 4], F32)
        m2 = sb("m2", [128, 4], F32)
        gT = sb("gTs", [8, 512], F32)
        h2fv = g.h2F.rearrange("(dc p) t -> p dc t", p=128)
        pX, pY = g.ps[0], g.ps[1]
        bc = lambda ap: ap.unsqueeze(2).to_broadcast([128, 4, 8])
        for ti in range(SEQ // 512):
            c0 = CTX + ti * 512
            h = hf[ti % 2]
            k.dma("sp", h[:], h2fv[:, :, c0:c0 + 512], r=[g.h2F_r], w=[h])
            for ch in range(4):
                for dc in range(8):
                    k.op("pe", lambda e, ch=ch, dc=dc, h=h: e.matmul(pX[:, ch * 8:(ch + 1) * 8], lhsT=h[:, dc, ch * 128:(ch + 1) * 128],
                                                                   rhs=rw[:, dc, :], start=(dc == 0), stop=(dc == 7)), r=[h, rw], w=[pX])
            k.op("dve", lambda e: e.tensor_tensor(out=lg[:], in0=pX[:, 0:32].rearrange("p (c e) -> p c e", e=8), in1=rb[:], op=ALU.add),
                 r=[pX, rb], w=[lg])
            k.op("dve", lambda e: e.tensor_reduce(out=m1[:], in_=lg[:], axis=AX.X, op=ALU.max), r=[lg], w=[m1])
            k.op("dve", lambda e: e.tensor_tensor(out=eq[:], in0=lg[:], in1=bc(m1[:]), op=ALU.is_equal), r=[lg, m1], w=[eq])
            k.op("dve", lambda e: e.scalar_tensor_tensor(out=lg2[:], in0=eq[:], scalar=-1e30, in1=lg[:], op0=ALU.mult, op1=ALU.add),
                 r=[eq, lg], w=[lg2])
            k.op("dve", lambda e: e.tensor_reduce(out=m2[:], in_=lg2[:], axis=AX.X, op=ALU.max), r=[lg2], w=[m2])
            k.op("dve", lambda e: e.tensor_tensor(out=eq[:], in0=lg[:], in1=bc(m2[:]), op=ALU.is_ge), r=[lg, m2], w=[eq])
            k.op("dve", lambda e: e.tensor_tensor(out=lg2[:], in0=lg[:], in1=bc(m1[:]), op=ALU.subtract), r=[lg, m1], w=[lg2])
            k.op("act", lambda e: e.activation(out=lg2[:], in_=lg2[:], func=AF.Exp), r=[lg2], w=[lg2])
            k.op("dve", lambda e: e.tensor_tensor(out=lg2[:], in0=lg2[:], in1=eq[:], op=ALU.mult), r=[lg2, eq], w=[lg2])
            k.op("dve", lambda e: e.tensor_reduce(out=m2[:], in_=lg2[:], axis=AX.X, op=ALU.add), r=[lg2], w=[m2])
            k.op("dve", lambda e: e.reciprocal(out=m2[:], in_=m2[:]), r=[m2], w=[m2])
            k.op("dve", lambda e: e.tensor_tensor(out=lg2[:], in0=lg2[:], in1=bc(m2[:]), op=ALU.mult), r=[lg2, m2], w=[lg2])
            for ch in range(4):
                k.op("pe", lambda e, ch=ch: e.matmul(pY[0:8, ch * 128:(ch + 1) * 128], lhsT=lg2[:, ch, :], rhs=idn[:], start=True, stop=True),
                     r=[lg2, idn], w=[pY])
            k.op("act", lambda e: e.activation(out=gT[:], in_=pY[0:8, :], func=AF.Copy), r=[pY], w=[gT])
            k.dma("sp", g.gT[:, c0:c0 + 512], gT[:], r=[gT], w=[g.gT_r])
        k.epoch()


def phase_ffn(g, l):
    nc, k = g.nc, g.k
    moe = (l % 2 == 1)
    NEXP = NE if moe else 1
    dff = DFE if moe else DFF
    groups = [(f0, min(512, dff - f0)) for f0 in range(0, dff, 512)]
    TB = 1024
    blocks = ([] if l == 1 else [(0, CTX, 1)]) + [(CTX + i * TB, TB, 0) for i in range(SEQ // TB)]
    with ExitStack() as ph:
        sb = lambda n, s, d: T(ph.enter_context(nc.sbuf_tensor(uname(n), s, d)))
        hb = sb("hb", [128, 8, TB], BF16)
        acc = sb("acc", [128, 8, TB], F32)
        wg = [sb("wg%d" % i, [128, 8, 512], BF16) for i in range(2)]
        wu = [sb("wu%d" % i, [128, 8, 512], BF16) for i in range(2)]
        wd = [sb("wd%d" % i, [128, 4, D], BF16) for i in range(2)]
        at = [sb("at%d" % i, [128, 4, 512], BF16) for i in range(2)]
        sg = [sb("sg%d" % i, [128, 512], F32) for i in range(2)]
        a1 = [sb("a1%d" % i, [128, 512], F32) for i in range(2)]
        xt = sb("gx", [128, 8, 512], F32)
        sq = sb("gsq", [128, 8, 512], BF16)
        rstd = sb("grstd", [128, 512], F32)
        tmp = sb("gtmp", [128, 512], F32)
        tmp2 = [sb("gtmp2_%d" % i, [128, 512], F32) for i in range(2)]
        if moe:
            gb = sb("gb", [128, NE, TB], BF16)
            gTs = sb("gTl", [8, TB], F32)
            sel = sb("sel", [8, NE, 128], F32)
            k.dma("sp", sel[:], g.i["sel"], w=[sel])
        h2v = g.h2T.rearrange("(dc p) t -> p dc t", p=128)
        xv = g.xres.rearrange("(dc p) t -> p dc t", p=128)
        ov = g.out.rearrange("(dc p) t -> p dc t", p=128)
        wi = 0
        ai = 0
        pi = 0
        import os
        bsel = os.environ.get("FFN_BLOCKS")
        if bsel:
            blocks = [blocks[int(x)] for x in bsel.split(",")]
        for (c0, tb, wh) in blocks:
            ntt = (tb + 511) // 512
            k.dma("sp", hb[:, :, :tb], h2v[:, :, c0:c0 + tb], r=[g.h2T_r], w=[hb])
            if moe:
                k.dma("sp", gTs[:, :tb], g.gT[:, c0:c0 + tb], r=[g.gT_r], w=[gTs])
                for e_ in range(NE):
                    for tt in range(ntt):
                        ps = g.ps[6 + (pi % 2)]
                        pi += 1
                        k.op("pe", lambda e, e_=e_, tt=tt, ps=ps: e.matmul(ps[:, :], lhsT=sel[:, e_, :], rhs=gTs[:, tt * 512:(tt + 1) * 512],
                                                                          start=True, stop=True), r=[sel, gTs], w=[ps])
                        k.op("act", lambda e, e_=e_, tt=tt, ps=ps: e.activation(out=gb[:, e_, tt * 512:(tt + 1) * 512], in_=ps[:, :], func=AF.Copy),
                             r=[ps], w=[gb])
            first = True
            for e_ in range(NEXP):
                if moe:
                    sg_, su_, sd_ = g.i["moe_w_gate"][e_], g.i["moe_w_up"][e_], g.i["moe_w_down"][e_]
                else:
                    sg_, su_, sd_ = g.i["ffn_w_gate"], g.i["ffn_w_up"], g.i["ffn_w_down"]
                for (f0, fw) in groups:
                    nfc = fw // 128
                    wgt, wut, wdt = wg[wi % 2], wu[wi % 2], wd[wi % 2]
                    wi += 1
                    gi = f0 // 512
                    bi = blocks.index((c0, tb, wh))
                    if moe and bi > 0:
                        rr = g.wscr_r[(e_, gi)]
                        k.dma("sp", wgt[:].rearrange("p a b -> p (a b)"), g.wscr[0][e_, gi], r=[rr], w=[wgt])
                        k.dma("sp", wut[:].rearrange("p a b -> p (a b)"), g.wscr[1][e_, gi], r=[rr], w=[wut])
                        k.dma("sp", wdt[:].rearrange("p a b -> p (a b)"), g.wscr[2][e_, gi], r=[rr], w=[wdt])
                    else:
                        for dc in range(8):
                            k.dma("pool", wgt[:, dc, :fw], sg_[dc * 128:(dc + 1) * 128, f0:f0 + fw], w=[wgt])
                            k.dma("pool", wut[:, dc, :fw], su_[dc * 128:(dc + 1) * 128, f0:f0 + fw], w=[wut])
                        for fc in range(nfc):
                            k.dma("pool", wdt[:, fc, :], sd_[f0 + fc * 128:f0 + (fc + 1) * 128, :], w=[wdt])
                        if moe:
                            rr = g.wscr_r[(e_, gi)] = Res()
                            k.dma("sp", g.wscr[0][e_, gi], wgt[:].rearrange("p a b -> p (a b)"), r=[wgt], w=[rr])
                            k.dma("sp", g.wscr[1][e_, gi], wut[:].rearrange("p a b -> p (a b)"), r=[wut], w=[rr])
                            k.dma("sp", g.wscr[2][e_, gi], wdt[:].rearrange("p a b -> p (a b)"), r=[wdt], w=[rr])
                    for tt in range(ntt):
                        tn = min(512, tb - tt * 512)
                        ts = slice(tt * 512, tt * 512 + tn)
                        a_ = at[ai % 2]
                        ai += 1
                        for fc in range(nfc):
                            pg, pu = g.ps[(pi % 2) * 2], g.ps[(pi % 2) * 2 + 1]
                            s_, a1_ = sg[pi % 2], a1[pi % 2]
                            pi += 1
                            for dc in range(8):
                                k.op("pe", lambda e, pg=pg, dc=dc, fc=fc, wgt=wgt, ts=ts: e.matmul(
                                    pg[:, :tn], lhsT=wgt[:, dc, fc * 128:(fc + 1) * 128], rhs=hb[:, dc, ts],
                                    start=(dc == 0), stop=(dc == 7)), r=[wgt, hb], w=[pg])
                            for dc in range(8):
                                k.op("pe", lambda e, pu=pu, dc=dc, fc=fc, wut=wut, ts=ts: e.matmul(
                                    pu[:, :tn], lhsT=wut[:, dc, fc * 128:(fc + 1) * 128], rhs=hb[:, dc, ts],
                                    start=(dc == 0), stop=(dc == 7)), r=[wut, hb], w=[pu])
                            k.op("act", lambda e, pg=pg, s_=s_: e.activation(out=s_[:, :tn], in_=pg[:, :tn], func=AF.Silu), r=[pg], w=[s_])
                            if moe:
                                k.op("dve", lambda e, pu=pu, s_=s_, a1_=a1_: e.tensor_tensor(out=a1_[:, :tn], in0=s_[:, :tn], in1=pu[:, :tn], op=ALU.mult),
                                     r=[pu, s_], w=[a1_])
                                k.op("pool", lambda e, a1_=a1_, a_=a_, fc=fc, e_=e_, ts=ts: e.tensor_tensor(out=a_[:, fc, :tn], in0=a1_[:, :tn], in1=gb[:, e_, ts],
                                                                                                     op=ALU.mult), r=[a1_, gb], w=[a_])
                            else:
                                k.op("dve", lambda e, pu=pu, s_=s_, a_=a_, fc=fc: e.tensor_tensor(out=a_[:, fc, :tn], in0=s_[:, :tn], in1=pu[:, :tn], op=ALU.mult),
                                     r=[pu, s_], w=[a_])
                        for dc in range(8):
                            po = g.ps[4 + (dc % 2)]
                            for fc in range(nfc):
                                k.op("pe", lambda e, po=po, dc=dc, fc=fc, wdt=wdt, a_=a_: e.matmul(
                                    po[:, :tn], lhsT=wdt[:, fc, dc * 128:(dc + 1) * 128], rhs=a_[:, fc, :tn],
                                    start=(fc == 0), stop=(fc == nfc - 1)), r=[wdt, a_], w=[po])
                            if first:
                                k.op("act", lambda e, po=po, dc=dc, ts=ts: e.activation(out=acc[:, dc, ts], in_=po[:, :tn], func=AF.Copy), r=[po], w=[acc])
                            else:
                                k.op("dve", lambda e, po=po, dc=dc, ts=ts: e.tensor_tensor(out=acc[:, dc, ts], in0=acc[:, dc, ts], in1=po[:, :tn], op=ALU.add),
                                     r=[po, acc], w=[acc])
                    first = False
            for tt in range(ntt):
                tn = min(512, tb - tt * 512)
                ts = slice(tt * 512, tt * 512 + tn)
                cc0 = c0 + tt * 512
                k.dma("sp", xt[:, :, :tn], xv[:, :, cc0:cc0 + tn], r=[g.xres_r], w=[xt])
                k.op("act", lambda e, ts=ts: e.activation(out=sq[:, :, :tn], in_=acc[:, :, ts], func=AF.Square), r=[acc], w=[sq])
                ps = g.ps[6]
                for dc in range(8):
                    k.op("pe", lambda e, dc=dc, ps=ps: e.matmul(ps[:, :tn], lhsT=g.ones_bf[:], rhs=sq[:, dc, :tn], start=(dc == 0), stop=(dc == 7)),
                         r=[sq, g.ones_bf], w=[ps])
                k.op("act", lambda e, ps=ps: e.activation(out=tmp[:, :tn], in_=ps[:, :tn], func=AF.Sqrt, scale=1.0 / D, bias=g.eps_t[:]),
                     r=[ps, g.eps_t], w=[tmp])
                k.op("dve", lambda e: e.reciprocal(out=rstd[:, :tn], in_=tmp[:, :tn]), r=[tmp], w=[rstd])
                for dc in range(8):
                    t2 = tmp2[dc % 2]
                    k.op("dve", lambda e, dc=dc, t2=t2, ts=ts: e.scalar_tensor_tensor(
                        out=t2[:, :tn], in0=acc[:, dc, ts], scalar=g.modv[:, 5, dc, wh:wh + 1], in1=rstd[:, :tn],
                        op0=ALU.mult, op1=ALU.mult), r=[acc, rstd, g.modv], w=[t2])
                    k.op("pool", lambda e, dc=dc, t2=t2: e.tensor_tensor(out=xt[:, dc, :tn], in0=xt[:, dc, :tn], in1=t2[:, :tn], op=ALU.add),
                         r=[t2, xt], w=[xt])
                if l == 1:
                    k.dma("sp", ov[:, :, cc0 - CTX:cc0 - CTX + tn], xt[:, :, :tn], r=[xt], w=[g.out_r])
                else:
                    k.dma("sp", xv[:, :, cc0:cc0 + tn], xt[:, :, :tn], r=[xt], w=[g.xres_r])
            k.epoch()

IN_SPECS = {
    "xT": ([D, TT], F32), "posT": ([D, SEQ], F32), "cvec": ([128, 8, 2], F32),
    "ada_w": ([2, D, 6 * D], F32), "ada_b": ([2, 128, 48], F32), "norm_g": ([2, 128, 4, 8], F32),
    "w_in": ([2, D, DIN], F32),
    "rw_par": ([2, 64, 8, 2, 8], F32), "rw_lmu": ([2, 64, 2, 2], F32), "rw_lora": ([2, 64, 2, 2, 512], F32),
    "rw_g2": ([2, 128, 512], F32), "rw_gn": ([2, 64, 2, 512], F32),
    "masks": ([64, 2, 320], F32), "ident": ([128, 128], F32), "rmask": ([64, SEG], F32),
    "conv_w": ([2, 2, 128, 3], F32),
    "w_out": ([2, D, D], F32),
    "ffn_w_gate": ([D, DFF], F32), "ffn_w_up": ([D, DFF], F32), "ffn_w_down": ([DFF, D], F32),
    "moe_w_gate": ([NE, D, DFE], F32), "moe_w_up": ([NE, D, DFE], F32), "moe_w_down": ([NE, DFE, D], F32),
    "router_w": ([128, 8, 8], F32), "router_b": ([128, 4, 8], F32), "sel": ([8, NE, 128], F32),
    "bd64": ([2, 128, 128], BF16), "dftL": ([2, 16, 128, 32 * 256], BF16), "dftC": ([2, 1, 128, 2 * 256], BF16),
}


def build(phases=None, dbg=(), ext_in=(), heads=range(8)):
    nc = bass.Bass("TRN2", target_bir_lowering=False)
    g = Ctx()
    g.nc = nc
    g.heads = list(heads)
    import os
    g.rw_stage = int(os.environ.get('RW_STAGE', '4'))
    g.rw_maxseg = int(os.environ.get('RW_MAXSEG', '99'))
    g.rw_s1 = os.environ.get('RW_S1', 'bad')
    g.i = {}
    if phases is None:
        phases = []
        for l in range(2):
            phases += [("mod", l), ("win", l), ("rwkv", l), ("conv", l), ("fourier", l), ("wout", l)]
            if l == 1:
                phases.append(("router", l))
            phases.append(("ffn", l))
    used = set()
    need = {"mod": ("cvec", "ada_w", "ada_b", "norm_g"), "win": ("xT", "posT", "w_in"),
            "rwkv": ("rw_par", "rw_lmu", "rw_lora", "rw_g2", "rw_gn", "masks", "ident", "rmask"),
            "conv": ("conv_w",), "fourier": ("bd64", "dftL", "dftC"), "wout": ("w_out",),
            "router": ("router_w", "router_b", "ident"), "ffn": ()}
    for (p, l) in phases:
        used.update(need[p])
        if p == "ffn":
            used.update(("moe_w_gate", "moe_w_up", "moe_w_down", "sel") if l == 1 else ("ffn_w_gate", "ffn_w_up", "ffn_w_down"))
    for name, (shape, dt) in IN_SPECS.items():
        if name in used:
            g.i[name] = nc.dram_tensor(name, shape, dt, kind="ExternalInput").ap()
    g.in_names = sorted(used)

    def scratch(name, shape, dt):
        kind = "ExternalOutput" if name in dbg else ("ExternalInput" if name in ext_in else "Internal")
        return nc.dram_tensor(name, shape, dt, kind=kind).ap()
    g.xres = scratch("xres", [D, TT], F32) if "xres_in" not in ext_in else nc.dram_tensor("xres", [D, TT], F32, kind="ExternalOutput").ap()
    g.xres_in = nc.dram_tensor("xres_in", [D, TT], F32, kind="ExternalInput").ap() if "xres_in" in ext_in else None
    g.zT = scratch("zT", [DIN, TT], F32)
    g.ymix = scratch("ymix", [D, TT], BF16)
    g.h2T = scratch("h2T", [D, TT], BF16)
    g.h2F = scratch("h2F", [D, TT], F32)
    g.gT = scratch("gT", [8, TT], F32)
    g.wscr = [scratch("wscr%d" % i, [NE, DFE // 512, 128, 4096], BF16) for i in range(3)]
    g.wscr_r = {}
    g.xres_r, g.zT_r, g.ymix_r, g.h2T_r, g.h2F_r, g.gT_r, g.out_r = [Res() for _ in range(7)]
    g.dbg_state = None
    if "state" in dbg:
        g.dbg_state = nc.dram_tensor("state", [2, 2, 8, 64, 64], F32, kind="ExternalOutput").ap()
    g.out = nc.dram_tensor("out", [D, SEQ], F32, kind="ExternalOutput").ap()
    with ExitStack() as es:
        g.k = k = KB(nc, es)
        sb = lambda n, s, d: T(es.enter_context(nc.sbuf_tensor(uname(n), s, d)))
        g.ps = [T(es.enter_context(nc.psum_tensor("ps%d" % i, [128, 512], F32))) for i in range(8)]
        g.modv = sb("modv", [128, 6, 8, 2], F32)
        g.ones_bf = sb("ones_bf", [128, 128], BF16)
        g.eps_t = sb("eps_t", [128, 1], F32)
        g.gneps_t = sb("gneps_t", [128, 1], F32)
        k.op("dve", lambda e: e.memset(g.ones_bf[:], 1.0), w=[g.ones_bf])
        k.op("dve", lambda e: e.memset(g.eps_t[:], EPS), w=[g.eps_t])
        k.op("dve", lambda e: e.memset(g.gneps_t[:], GN_EPS), w=[g.gneps_t])
        if g.xres_in is not None:
            for i_ in range(32):
                k.dma("sp", g.xres[i_ * 32:(i_ + 1) * 32, :], g.xres_in[i_ * 32:(i_ + 1) * 32, :], w=[g.xres_r])
        fns = {"mod": phase_mod, "win": phase_win, "conv": phase_conv, "fourier": phase_fourier,
               "wout": phase_wout, "router": phase_router, "ffn": phase_ffn}
        for (p, l) in phases:
            if p == "rwkv":
                phase_rwkv(g, l, l == 1)
            else:
                fns[p](g, l)
        k.barrier()
        if "modv" in dbg:
            md = nc.dram_tensor("modv_o", [128, 96], F32, kind="ExternalOutput").ap()
            k.dma("sp", md, g.modv[:].rearrange("p a b c -> p (a b c)"), r=[g.modv])
        k.barrier(["sp"])
    print("instructions", k.nins, "waits", k.nwait)
    return nc, g


def sincos_pos():
    quarter = D // 4
    omega = 1.0 / (10000.0 ** (np.arange(quarter, dtype=np.float32) / quarter))

    def axis_emb(n):
        ang = np.arange(n, dtype=np.float32)[:, None] * omega[None, :]
        return np.concatenate([np.sin(ang), np.cos(ang)], axis=-1)
    er, ec = axis_emb(64), axis_emb(64)
    emb = np.concatenate([np.broadcast_to(er[:, None, :], (64, 64, D // 2)),
                          np.broadcast_to(ec[None, :, :], (64, 64, D // 2))], axis=-1)
    return np.ascontiguousarray(emb.reshape(SEQ, D).T.astype(np.float32))


_CONST = {}


def constants():
    if _CONST:
        return _CONST
    import ml_dtypes
    bf = ml_dtypes.bfloat16
    c = _CONST
    c["posT"] = sincos_pos()
    c["ident"] = np.eye(128, dtype=np.float32)
    s_ = np.arange(64)[:, None]
    t_ = np.arange(64)[None, :]
    m = np.zeros((64, 2, 320), np.float32)
    for d, (st, inc) in enumerate((((s_ < t_), (s_ <= t_)), ((s_ > t_), (s_ >= t_)))):
        m[:, d, 0:64] = st
        m[:, d, 64:128] = inc
        m[:, d, 128:192] = st
        m[:, d, 192:256] = inc
        m[:, d, 256:320] = st.T
    c["masks"] = m
    sel = np.zeros((8, NE, 128), np.float32)
    for e_ in range(NE):
        sel[e_, e_, :] = 1.0
    c["sel"] = sel
    rm = np.ones((64, SEG), np.float32)
    rm[:, ::64] = 0.0
    c["rmask"] = rm
    a64 = 2 * np.pi * np.outer(np.arange(64), np.arange(64)) / 64
    bd = np.zeros((2, 128, 128), np.float64)
    for i in range(2):
        bd[0, i * 64:(i + 1) * 64, i * 64:(i + 1) * 64] = np.cos(a64)
        bd[1, i * 64:(i + 1) * 64, i * 64:(i + 1) * 64] = np.sin(a64)
    c["bd64"] = bd.astype(bf)
    for name, L in (("dftL", SEQ), ("dftC", CTX)):
        idx = (np.outer(np.arange(L), np.arange(L)) % L).astype(np.float64)
        ang = 2 * np.pi * idx / L
        sc = 1.0 / np.sqrt(64.0 * L)
        mats = np.stack([np.cos(ang) * sc, -np.sin(ang) * sc])
        ntt, ntp = L // 128, L // 256
        mats = mats.reshape(2, ntt, 128, ntp, 256).transpose(0, 3, 2, 1, 4)
        c[name] = np.ascontiguousarray(mats.reshape(2, ntp, 128, ntt * 256)).astype(bf)
    return c


def make_in_maps(inp, names, cores=range(8)):
    f32 = lambda a: np.ascontiguousarray(np.asarray(a, dtype=np.float32))
    shared = dict(constants())
    shared["ada_w"] = f32(inp["ada_w"])
    shared["ada_b"] = f32(inp["ada_b"].reshape(2, 48, 128).transpose(0, 2, 1))
    shared["norm_g"] = f32(inp["norm_g"].reshape(2, 4, 8, 128).transpose(0, 3, 1, 2))
    shared["w_in"] = f32(inp["w_in"])
    par = np.zeros((2, 64, 8, 2, 8), np.float32)
    hp = lambda a: a.reshape(2, 8, 64).transpose(0, 2, 1)
    for d in range(2):
        for j in range(3):
            par[:, :, :, d, j] = hp(inp["rwkv_mu"][:, d, j])
        par[:, :, :, d, 3] = hp(inp["rwkv_w0"][:, d])
        par[:, :, :, d, 4] = hp(inp["rwkv_a0"][:, d])
        par[:, :, :, d, 5] = hp(inp["rwkv_k_k"])
        par[:, :, :, d, 6] = hp(inp["rwkv_k_a"])
        par[:, :, :, d, 7] = hp(inp["rwkv_r_k"].reshape(2, 512))
    shared["rw_par"] = par
    shared["rw_lmu"] = f32(np.stack([inp["rwkv_mu_w"], inp["rwkv_mu_a"]], axis=-1).transpose(0, 2, 1, 3))
    shared["rw_lora"] = f32(np.stack([inp["rwkv_w2"], inp["rwkv_a2"]], axis=2).transpose(0, 3, 1, 2, 4))
    shared["rw_g2"] = f32(inp["rwkv_g2"])
    gn = np.stack([inp["rwkv_gn_w"], inp["rwkv_gn_b"]], axis=1)
    shared["rw_gn"] = f32(np.broadcast_to(gn[:, None], (2, 64, 2, 512)))
    shared["conv_w"] = f32(inp["conv_w"].transpose(0, 2, 1).reshape(2, 2, 128, 3))
    shared["w_out"] = f32(inp["w_out"])
    for n_ in ("ffn_w_gate", "ffn_w_up", "ffn_w_down", "moe_w_gate", "moe_w_up", "moe_w_down"):
        if n_ in names:
            shared[n_] = f32(inp[n_][0])
    shared["router_w"] = f32(inp["router_w"][0].reshape(8, 128, 8).transpose(1, 0, 2))
    shared["router_b"] = f32(np.broadcast_to(inp["router_b"][0][None, None, :], (128, 4, 8)))
    maps = []
    for b in cores:
        m = {n: shared[n] for n in names if n in shared}
        if "xT" in names:
            m["xT"] = f32(np.concatenate([inp["ctx"][b].T, inp["x"][b].T], axis=1))
        if "cvec" in names:
            m["cvec"] = f32(np.stack([inp["c"][b].reshape(8, 128).T, inp["c_ctx"].reshape(8, 128).T], axis=-1))
        maps.append(m)
    return maps


def kernel(**inp):
    inp = {k_: np.asarray(v) for k_, v in inp.items()}
    nc, g = build()
    maps = make_in_maps(inp, g.in_names)
    res = run_bass_kernel_spmd(nc, maps, core_ids=list(range(8)))
    out = np.stack([np.ascontiguousarray(r["out"].T) for r in res.results], axis=0)
    return out.astype(np.float32)
```
